# Optimizing a Trainium2 kernel written in Bass

```python
import jax, jax.numpy as jnp
from jax import lax
import numpy as np

D_MODEL = 1024
BATCH = 8
SEQ = 2048
DEPTH = 1

HEAD_DIM = 64
N_HEADS_A = 8
N_KV_A = 2
WINDOW_A = 128
N_HEADS_B = 8
DILATED_BRANCHES = ((128, 1), (512, 4), (2048, 16))
BLOCK = 128
ROT_DIM = HEAD_DIM // 4
ROPE_THETA = 500000.0
MIX_A = N_HEADS_A * HEAD_DIM
KV_A = N_KV_A * HEAD_DIM
MIX_B = N_HEADS_B * HEAD_DIM
MIX_WIDTH = MIX_A + MIX_B
IN_WIDTH = MIX_A + 2 * KV_A + 3 * MIX_B
N_EXPERTS = 256
TOP_K = 8
N_GROUPS = 8
TOPK_GROUPS = 4
EXPERT_DIM = 256
SHARED_DIM = 256
ROUTED_SCALE = 2.5
EXPERT_BLOCK = 128
EPS = 1e-6

kernel_name = "hybrid_swa_sink_dilated_moe_adaln"

F32 = jnp.float32


def rms_norm(t, g):
    tf = t.astype(F32)
    return tf * lax.rsqrt(jnp.mean(tf * tf, axis=-1, keepdims=True) + EPS) * g.astype(F32)


def rope_tables(positions):
    inv = ROPE_THETA ** (-jnp.arange(0, ROT_DIM, 2, dtype=F32) / ROT_DIM)
    ang = positions.astype(F32)[..., None] * inv
    return jnp.cos(ang)[:, :, None, :], jnp.sin(ang)[:, :, None, :]


def partial_rotary(t, cos, sin):
    half = ROT_DIM // 2
    t1, t2, rest = t[..., :half], t[..., half:ROT_DIM], t[..., ROT_DIM:]
    return jnp.concatenate([t1 * cos - t2 * sin, t2 * cos + t1 * sin, rest], axis=-1)


def banded_attention(q, k, v, max_dist, sinks=None):
    n, L, hk, g, hd = q.shape
    nb = L // BLOCK
    qb = q.reshape(n, nb, BLOCK, hk, g, hd).astype(F32)

    def with_prev(t):
        tb = t.reshape(n, nb, BLOCK, hk, hd).astype(F32)
        prev = jnp.pad(tb, ((0, 0), (1, 0), (0, 0), (0, 0), (0, 0)))[:, :-1]
        return jnp.concatenate([prev, tb], axis=2)

    kk, vv = with_prev(k), with_prev(v)
    s = jnp.einsum('nbqhgd,nbkhd->nbhgqk', qb, kk) * (hd ** -0.5)
    qpos = jnp.arange(BLOCK)[:, None] + BLOCK
    kpos = jnp.arange(2 * BLOCK)[None, :]
    dist = qpos - kpos
    band = (dist >= 0) & (dist <= max_dist)
    has_prev = (jnp.arange(nb)[:, None, None] > 0) | (kpos[None] >= BLOCK)
    mask = band[None] & has_prev
    s = jnp.where(mask[None, :, None, None], s, -jnp.inf)
    if sinks is None:
        lse = jax.nn.logsumexp(s, axis=-1)
    else:
        sink_col = jnp.broadcast_to(sinks.astype(F32)[None, None, :, :, None, None],
                                    s.shape[:-1] + (1,))
        lse = jax.nn.logsumexp(jnp.concatenate([s, sink_col], axis=-1), axis=-1)
    p = jnp.exp(s - lse[..., None])
    o = jnp.einsum('nbhgqk,nbkhd->nbqhgd', p, vv).reshape(n, L, hk, g, hd)
    lse = lse.transpose(0, 1, 4, 2, 3).reshape(n, L, hk, g)
    return o, lse


def dilated_attention(q, k, v):
    B, S, H, hd = q.shape
    outs, lses = [], []
    for window, dil in DILATED_BRANCHES:
        L = S // dil
        Lp = -(-L // BLOCK) * BLOCK

        def to_sub(t):
            t = t.reshape(B, L, dil, H, hd).transpose(0, 2, 1, 3, 4).reshape(B * dil, L, H, hd)
            return jnp.pad(t, ((0, 0), (0, Lp - L), (0, 0), (0, 0)))

        o, lse = banded_attention(to_sub(q)[:, :, :, None], to_sub(k), to_sub(v), window // dil)
        outs.append(o[:, :L, :, 0].reshape(B, dil, L, H, hd).transpose(0, 2, 1, 3, 4).reshape(B, S, H, hd))
        lses.append(lse[:, :L, :, 0].reshape(B, dil, L, H).transpose(0, 2, 1, 3).reshape(B, S, H))
    w = jax.nn.softmax(jnp.stack(lses), axis=0)
    return jnp.einsum('rbsh,rbshd->bshd', w, jnp.stack(outs))


def hybrid_mixer(h, cos, sin, w_in, g_q_a, g_k_a, sinks_a, g_q_b, g_k_b, g_out_a, g_out_b, w_out):
    B, S, _ = h.shape
    proj = h @ w_in
    cuts = [int(i) for i in np.cumsum([MIX_A, KV_A, KV_A, MIX_B, MIX_B])]
    q_a, k_a, v_a, q_b, k_b, v_b = jnp.split(proj, cuts, axis=-1)
    heads = lambda t: t.reshape(B, S, -1, HEAD_DIM)
    q_a = partial_rotary(rms_norm(heads(q_a), g_q_a), cos, sin)
    k_a = partial_rotary(rms_norm(heads(k_a), g_k_a), cos, sin)
    q_a = q_a.reshape(B, S, N_KV_A, N_HEADS_A // N_KV_A, HEAD_DIM)
    o_a, _ = banded_attention(q_a, k_a, heads(v_a), WINDOW_A - 1,
                              sinks_a.reshape(N_KV_A, N_HEADS_A // N_KV_A))
    o_a = rms_norm(o_a.reshape(B, S, MIX_A), g_out_a)
    q_b = partial_rotary(rms_norm(heads(q_b), g_q_b), cos, sin)
    k_b = partial_rotary(rms_norm(heads(k_b), g_k_b), cos, sin)
    o_b = dilated_attention(q_b, k_b, heads(v_b).astype(F32))
    o_b = rms_norm(o_b.reshape(B, S, MIX_B), g_out_b)
    return jnp.concatenate([o_a, o_b], axis=-1) @ w_out.astype(F32)


def swiglu(t, wg, wu, wd):
    return (jax.nn.silu(t @ wg) * (t @ wu)) @ wd


def routed_experts(xf, top_e, gates, w_gate_e, w_up_e, w_down_e):
    N, D = xf.shape
    e_flat = top_e.reshape(-1)
    NK = e_flat.shape[0]
    tok_flat = jnp.repeat(jnp.arange(N, dtype=jnp.int32), TOP_K)
    g_flat = gates.reshape(-1)
    order = jnp.argsort(e_flat)
    e_sorted = e_flat[order]
    counts = jnp.bincount(e_flat, length=N_EXPERTS)
    starts = jnp.cumsum(counts) - counts
    padded = (counts + EXPERT_BLOCK - 1) // EXPERT_BLOCK * EXPERT_BLOCK
    pends = jnp.cumsum(padded)
    pstarts = pends - padded
    dest = pstarts[e_sorted] + jnp.arange(NK) - starts[e_sorted]
    rows = -(-NK // EXPERT_BLOCK) * EXPERT_BLOCK + N_EXPERTS * EXPERT_BLOCK
    n_blocks = rows // EXPERT_BLOCK
    row_tok = jnp.zeros((rows,), jnp.int32).at[dest].set(tok_flat[order])
    row_gate = jnp.zeros((rows,), F32).at[dest].set(g_flat[order])
    block_e = jnp.minimum(
        jnp.searchsorted(pends, jnp.arange(n_blocks) * EXPERT_BLOCK, side='right'), N_EXPERTS - 1)

    def step(acc, blk):
        e, tok, g = blk
        yb = swiglu(xf[tok], w_gate_e[e], w_up_e[e], w_down_e[e])
        return acc.at[tok].add(yb.astype(F32) * g[:, None]), None

    acc, _ = lax.scan(step, jnp.zeros((N, D), F32),
                      (block_e, row_tok.reshape(n_blocks, EXPERT_BLOCK),
                       row_gate.reshape(n_blocks, EXPERT_BLOCK)))
    return acc


def moe_ffn(h, w_router, router_bias, w_gate_e, w_up_e, w_down_e, w_gate_s, w_up_s, w_down_s):
    B, S, D = h.shape
    N = B * S
    xf = h.reshape(N, D)
    scores = jax.nn.sigmoid(xf.astype(F32) @ w_router.astype(F32))
    biased = scores + router_bias.astype(F32)
    grp = lax.top_k(biased.reshape(N, N_GROUPS, N_EXPERTS // N_GROUPS), 2)[0].sum(-1)
    top_grp = lax.top_k(grp, TOPK_GROUPS)[1]
    gmask = jnp.any(top_grp[..., None] == jnp.arange(N_GROUPS), axis=1)
    emask = jnp.repeat(gmask, N_EXPERTS // N_GROUPS, axis=-1)
    _, top_e = lax.top_k(jnp.where(emask, biased, -jnp.inf), TOP_K)
    gates = jnp.take_along_axis(scores, top_e, axis=-1)
    gates = gates / jnp.sum(gates, axis=-1, keepdims=True) * ROUTED_SCALE
    routed = routed_experts(xf, top_e, gates, w_gate_e, w_up_e, w_down_e)
    shared = swiglu(xf, w_gate_s, w_up_s, w_down_s).astype(F32)
    return (routed + shared).reshape(B, S, D)


def setup_inputs(seed: int = 0) -> dict:
    key = jax.random.key(seed)
    ks = jax.random.split(key, 24)
    nrm = lambda k, shape, s: jax.random.normal(k, shape, F32) * s
    gain = lambda k, shape: 1.0 + 0.02 * jax.random.normal(k, shape, F32)
    x = jax.random.normal(ks[0], (BATCH, SEQ, D_MODEL), F32)
    c = jax.random.normal(ks[1], (BATCH, D_MODEL), F32)
    offset = jax.random.randint(ks[2], (BATCH, 1), 0, 4096, dtype=jnp.int32)
    positions = offset + jnp.arange(SEQ, dtype=jnp.int32)[None, :]
    return {
        "x": x,
        "c": c,
        "positions": positions,
        "w_ada": nrm(ks[3], (DEPTH, D_MODEL, 6 * D_MODEL), 0.25 * D_MODEL ** -0.5),
        "b_ada": nrm(ks[4], (DEPTH, 6 * D_MODEL), 0.02),
        "g_norm_mix": gain(ks[5], (DEPTH, D_MODEL)),
        "w_in": nrm(ks[6], (DEPTH, D_MODEL, IN_WIDTH), D_MODEL ** -0.5),
        "g_q_a": gain(ks[7], (DEPTH, HEAD_DIM)),
        "g_k_a": gain(ks[8], (DEPTH, HEAD_DIM)),
        "sinks_a": nrm(ks[9], (DEPTH, N_HEADS_A), 0.5),
        "g_q_b": gain(ks[10], (DEPTH, HEAD_DIM)),
        "g_k_b": gain(ks[11], (DEPTH, HEAD_DIM)),
        "g_out_a": gain(ks[12], (DEPTH, MIX_A)),
        "g_out_b": gain(ks[13], (DEPTH, MIX_B)),
        "w_out": nrm(ks[14], (DEPTH, MIX_WIDTH, D_MODEL), MIX_WIDTH ** -0.5),
        "g_norm_ffn": gain(ks[15], (DEPTH, D_MODEL)),
        "w_router": nrm(ks[16], (DEPTH, D_MODEL, N_EXPERTS), D_MODEL ** -0.5),
        "router_bias": nrm(ks[17], (DEPTH, N_EXPERTS), 0.01),
        "w_gate_e": nrm(ks[18], (DEPTH, N_EXPERTS, D_MODEL, EXPERT_DIM), D_MODEL ** -0.5),
        "w_up_e": nrm(ks[19], (DEPTH, N_EXPERTS, D_MODEL, EXPERT_DIM), D_MODEL ** -0.5),
        "w_down_e": nrm(ks[20], (DEPTH, N_EXPERTS, EXPERT_DIM, D_MODEL), EXPERT_DIM ** -0.5),
        "w_gate_s": nrm(ks[21], (DEPTH, D_MODEL, SHARED_DIM), D_MODEL ** -0.5),
        "w_up_s": nrm(ks[22], (DEPTH, D_MODEL, SHARED_DIM), D_MODEL ** -0.5),
        "w_down_s": nrm(ks[23], (DEPTH, SHARED_DIM, D_MODEL), SHARED_DIM ** -0.5),
    }


def reference(x, c, positions, w_ada, b_ada, g_norm_mix, w_in, g_q_a, g_k_a, sinks_a,
              g_q_b, g_k_b, g_out_a, g_out_b, w_out, g_norm_ffn, w_router, router_bias,
              w_gate_e, w_up_e, w_down_e, w_gate_s, w_up_s, w_down_s):
    B, S, D = x.shape
    cos, sin = rope_tables(positions)
    cond = jax.nn.silu(c.astype(F32))
    for l in range(DEPTH):
        mod = (cond @ w_ada[l].astype(F32) + b_ada[l].astype(F32)).reshape(B, 6, 1, D)
        shift_a, scale_a, gate_a = mod[:, 0], mod[:, 1], mod[:, 2]
        shift_m, scale_m, gate_m = mod[:, 3], mod[:, 4], mod[:, 5]
        h = (rms_norm(x, g_norm_mix[l]) * (1.0 + scale_a) + shift_a).astype(x.dtype)
        y = hybrid_mixer(h, cos, sin, w_in[l], g_q_a[l], g_k_a[l], sinks_a[l],
                         g_q_b[l], g_k_b[l], g_out_a[l], g_out_b[l], w_out[l])
        x = (x.astype(F32) + gate_a * y).astype(x.dtype)
        h = (rms_norm(x, g_norm_ffn[l]) * (1.0 + scale_m) + shift_m).astype(x.dtype)
        y = moe_ffn(h, w_router[l], router_bias[l], w_gate_e[l], w_up_e[l], w_down_e[l],
                    w_gate_s[l], w_up_s[l], w_down_s[l])
        x = (x.astype(F32) + gate_m * y).astype(x.dtype)
    return x
```

```python
import contextlib
import numpy as np
import concourse.bass as bass
import concourse.mybir as mybir
from concourse.bass_utils import run_bass_kernel_spmd

F32 = mybir.dt.float32
F32R = mybir.dt.float32r
BF16 = mybir.dt.bfloat16
I32 = mybir.dt.int32
U32 = mybir.dt.uint32
ALU = mybir.AluOpType
ACTF = mybir.ActivationFunctionType
AX = mybir.AxisListType

COMPUTE = ("pe", "act", "dve", "pool")
ENGS = ("pe", "act", "dve", "pool", "sp")
NT = 16
CAP = 128
NE = 256
EPS = 1e-6
NCONST = 1048 + 256
NOV = 24


class Sched:
    def __init__(self, nc, stack, same_engine_sync=True):
        self.nc = nc
        self.stack = stack
        self.same = same_engine_sync
        self.ops = {e: [] for e in ENGS}
        self.cnt = {}
        self.sems = {}
        self.recs = {}
        self.bank_last = {}
        self.waited = {e: {} for e in ENGS}
        for e in COMPUTE:
            self._sem(e)

    def _sem(self, key):
        if key not in self.sems:
            self.sems[key] = self.stack.enter_context(self.nc.semaphore("s_" + key))
            self.cnt[key] = 0
        return self.sems[key]

    def _deps(self, reads, writes):
        deps = []
        for (sp, lo, hi) in reads:
            if sp.startswith("bank"):
                continue
            for r in self.recs.get(sp, ()):
                if r[2] == "w" and r[0] < hi and lo < r[1]:
                    deps.append(r[3])
        for (sp, lo, hi) in writes:
            if sp.startswith("bank"):
                continue
            for r in self.recs.get(sp, ()):
                if r[0] < hi and lo < r[1]:
                    deps.append(r[3])
        for (sp, lo, hi) in list(reads) + list(writes):
            if sp.startswith("bank") and sp in self.bank_last:
                deps.append(self.bank_last[sp])
        return deps

    def _record(self, reads, writes, tok):
        for (sp, lo, hi) in list(reads) + list(writes):
            if sp.startswith("bank"):
                self.bank_last[sp] = tok
        for (sp, lo, hi) in writes:
            if sp.startswith("bank"):
                continue
            lst = self.recs.setdefault(sp, [])
            lst[:] = [r for r in lst if not (lo <= r[0] and r[1] <= hi)]
            lst.append([lo, hi, "w", tok])
        for (sp, lo, hi) in reads:
            if sp.startswith("bank"):
                continue
            lst = self.recs.setdefault(sp, [])
            lst[:] = [r for r in lst if not (r[2] == "r" and r[3][0] == tok[0]
                                             and lo <= r[0] and r[1] <= hi)]
            lst.append([lo, hi, "r", tok])

    def op(self, eng, emit, reads=(), writes=(), dsem=None):
        deps = self._deps(reads, writes)
        if dsem is None:
            key, amt = eng, 1
        else:
            key, amt = dsem, 16
            self._sem(key)
        waits = {}
        for (k, v) in deps:
            if k == eng and dsem is None and (eng == "pe" or not self.same):
                continue
            if self.waited[eng].get(k, 0) >= v:
                continue
            waits[k] = max(waits.get(k, 0), v)
        for k, v in waits.items():
            self.waited[eng][k] = v
        self.cnt[key] += amt
        tok = (key, self.cnt[key])
        self.ops[eng].append((sorted(waits.items()), emit, key, amt))
        self._record(reads, writes, tok)
        return tok

    def wait_all(self, eng, toks):
        waits = {}
        for (k, v) in toks:
            waits[k] = max(waits.get(k, 0), v)
        self.ops[eng].append((sorted(waits.items()), None, None, 0))

    def emit(self):
        nc = self.nc
        with nc.Block() as block:
            def run(engname):
                def body(eng):
                    for waits, emit, key, amt in self.ops[engname]:
                        for (k, v) in waits:
                            eng.wait_ge(self.sems[k], v)
                        if emit is not None:
                            ins = emit(eng)
                            ins.then_inc(self.sems[key], amt)
                return body
            block.tensor(run("pe"))
            block.scalar(run("act"))
            block.vector(run("dve"))
            block.gpsimd(run("pool"))
            block.sync(run("sp"))


def R(name, lo=0, hi=1):
    return (name, lo, hi)


def make_consts():
    c = np.zeros((128, NCONST), np.float32)
    p = np.arange(128)
    c[:, 0:128] = np.eye(128, dtype=np.float32)
    c[:, 128:256] = (p[:, None] < p[None, :]).astype(np.float32)
    c[:, 256:384] = 1.0
    c[:, 384:640] = np.arange(256, dtype=np.float32)[None, :]
    c[:, 640:768] = (p[:, None] <= p[None, :]).astype(np.float32)
    c[:, 768:896] = (p[:, None] > p[None, :]).astype(np.float32)
    c[:, 896:1024] = (p[:, None] >= p[None, :]).astype(np.float32)
    inv = (np.float32(500000.0) ** (-np.arange(0, 16, 2, dtype=np.float32) / np.float32(16))).astype(np.float32)
    c[:, 1024:1032] = inv[None, :]
    c[:, 1032:1048] = (128 * np.arange(16)[None, :] + p[:, None]).astype(np.float32)
    c[:, 1048:1176] = p[:, None].astype(np.float32)
    c[:, 1176:1304] = (128 + p[:, None]).astype(np.float32)
    return c


class _Stop(Exception):
    pass


def build_nc(debug=False, stop_at=None):
    nc = bass.Bass("TRN2", target_bir_lowering=False)

    def din(name, shape, dt=F32):
        return nc.dram_tensor(name, shape, dt, kind="ExternalInput").ap()

    x_d = din("x", [2048, 1024])
    cT_d = din("cT", [128, 8])
    pos_d = din("posT", [128, 16], I32)
    w_ada_d = din("w_ada", [1024, 6144])
    b_ada_d = din("b_ada", [1, 6144])
    gmix_d = din("g_mix", [1, 1024])
    gffn_d = din("g_ffn", [1, 1024])
    w_in_d = din("w_in", [1024, 2304])
    gqk_d = din("gqk", [1, 256])
    sinks_d = din("sinks", [1, 8])
    gout_d = din("g_out", [1, 1024])
    w_out_d = din("w_out", [1024, 1024])
    wr_d = din("w_router", [1024, 256])
    rb_d = din("rbias", [1, 256])
    wge_d = din("w_gate_e", [256, 1024, 256])
    wue_d = din("w_up_e", [256, 1024, 256])
    wde_d = din("w_down_e", [256, 256, 1024])
    wgs_d = din("w_gate_s", [1024, 256])
    wus_d = din("w_up_s", [1024, 256])
    wds_d = din("w_down_s", [256, 1024])
    consts_d = din("consts", [128, NCONST])
    out_d = nc.dram_tensor("out", [2048, 1024], F32, kind="ExternalOutput").ap()

    mods_dram = nc.dram_tensor("mods_scr", [128, 6144], F32).ap()
    v_dram = nc.dram_tensor("v_scr", [2048, 520], BF16).ap()
    o4_dram = nc.dram_tensor("o4_scr", [2048, 520], F32).ap()
    o16_dram = nc.dram_tensor("o16_scr", [2048, 520], F32).ap()
    x1_dram = nc.dram_tensor("x1_scr", [2048, 1024], F32).ap()
    base_dram = nc.dram_tensor("base_scr", [2048, 1024], F32).ap()
    h2_dram = nc.dram_tensor("h2_scr", [2049, 1024], F32).ap()
    list_dram = nc.dram_tensor("list_scr", [(NE + NOV) * CAP, 2], I32).ap()
    y_dram = nc.dram_tensor("y_scr", [(NE + NOV) * CAP, 1024], F32).ap()

    dbg = {}
    if debug:
        dbg["x1"] = nc.dram_tensor("dbg_x1", [2048, 1024], F32, kind="ExternalOutput").ap()
        dbg["h2"] = nc.dram_tensor("dbg_h2", [2048, 1024], F32, kind="ExternalOutput").ap()
        dbg["off8"] = nc.dram_tensor("dbg_off8", [128, 128], I32, kind="ExternalOutput").ap()
        dbg["gate8"] = nc.dram_tensor("dbg_gate8", [128, 128], F32, kind="ExternalOutput").ap()
        dbg["base"] = nc.dram_tensor("dbg_base", [2048, 1024], F32, kind="ExternalOutput").ap()
        dbg["mods"] = nc.dram_tensor("dbg_mods", [128, 6144], F32, kind="ExternalOutput").ap()
        dbg["ocat"] = nc.dram_tensor("dbg_ocat", [2048, 1024], F32, kind="ExternalOutput").ap()
        dbg["qk"] = nc.dram_tensor("dbg_qk", [2048, 1664], BF16, kind="ExternalOutput").ap()
        dbg["kTa"] = nc.dram_tensor("dbg_kTa", [128, 2048], BF16, kind="ExternalOutput").ap()
        dbg["kTb"] = nc.dram_tensor("dbg_kTb", [128, 4 * 2048], BF16, kind="ExternalOutput").ap()
        dbg["qTa"] = nc.dram_tensor("dbg_qTa", [128, 4 * 2048], BF16, kind="ExternalOutput").ap()
        dbg["qTb"] = nc.dram_tensor("dbg_qTb", [128, 4 * 2048], BF16, kind="ExternalOutput").ap()
        dbg["v"] = nc.dram_tensor("dbg_v", [2048, 650], BF16, kind="ExternalOutput").ap()

    with contextlib.ExitStack() as st:
        S = Sched(nc, st)
        ARENA_F = 52100
        consts = nc.alloc_sbuf_tensor_at("consts_sb", [128, NCONST], F32, offset=16512)
        banks = [st.enter_context(nc.psum_tensor(f"bank{i}", [128, 512], F32)) for i in range(8)]

        uid = [0]
        ABASE = 21760

        class Buf:
            def __init__(self, name, off_b, nbytes, dt, shape_str=None, **kw):
                self.name = name
                self.off = off_b
                self.nbytes = nbytes
                isz = 2 if dt == BF16 else 4
                uid[0] += 1
                t = nc.alloc_sbuf_tensor_at("%s_%d" % (name, uid[0]), [128, nbytes // isz], dt, offset=ABASE + off_b)
                ap = t[:]
                if shape_str:
                    ap = ap.rearrange(shape_str, **kw)
                self.ap = ap

            def reg(self, lo=None, hi=None):
                if lo is None:
                    return ("arena", self.off, self.off + self.nbytes)
                return ("arena", self.off + lo, self.off + hi)

        class Alloc:
            def __init__(self):
                self.top = 0

            def get(self, name, nbytes, dt=F32, shape_str=None, **kw):
                nbytes = (nbytes + 31) // 32 * 32
                b = Buf(name, self.top, nbytes, dt, shape_str, **kw)
                self.top += nbytes
                assert self.top <= ARENA_F * 4, (name, self.top)
                return b

        def checkpoint(n):
            if stop_at == n:
                raise _Stop()

        last_tok = None
        try:
            A = Alloc()
            ident = consts[:, 0:128]
            tri = consts[:, 128:256]
            ones = consts[:, 256:384]
            iota_e = consts[:, 384:640]
            invf = consts[:, 1024:1032]
            tokf = consts[:, 1032:1048]
            eid0 = consts[:, 1048:1176]
            eid1 = consts[:, 1176:1304]
            ident_b = A.get("ident_b", 256, BF16)
            masks_b = A.get("masks_b", 3 * 256, BF16, "p (m k) -> p m k", m=3)
            maskA2 = A.get("maskA2", 2 * 4 * 256, BF16, "p (j h k) -> p j h k", j=2, h=4)
            maskB4 = A.get("maskB4", 4 * 256, BF16, "p (b k) -> p b k", b=4)
            cosb = A.get("cos", 16 * 8 * 4, F32, "p (j f) -> p j f", j=16)
            sinb = A.get("sin", 16 * 8 * 4, F32, "p (j f) -> p j f", j=16)
            off8 = A.get("off8", 128 * 4, I32, "p (j k) -> p j k", j=16)
            gate8 = A.get("gate8", 128 * 4, F32, "p (j k) -> p j k", j=16)
            tok_i = A.get("tok_i", 32 * 4, I32, "p (j t) -> p j t", t=2)
            small = A.get("small", 64 * 4, F32)
            neghalf = A.get("neghalf", 32 * 4, F32)
            esink = A.get("esink", 8 * 4, F32)
            junk = A.get("junk", 4096, F32)
            PERSIST_TOP = A.top

            ld = [0]

            def _autosem(reads, writes, sem):
                if sem not in (None, "st"):
                    return sem
                regs = reads if sem == "st" else writes
                r0 = regs[0]
                return ("s" if sem == "st" else "l") + "_%s_%s" % (r0[0], r0[1])

            def dma_sp(out, in_, reads, writes, sem=None):
                return S.op("sp", lambda e: e.dma_start(out=out, in_=in_), reads=reads, writes=writes, dsem=_autosem(reads, writes, sem))

            def dma_pool(out, in_, reads, writes, sem=None):
                return S.op("pool", lambda e: e.dma_start(out=out, in_=in_), reads=reads, writes=writes, dsem=_autosem(reads, writes, sem))

            _regs = {}

            def breg(e, val):
                if val not in _regs:
                    _regs[val] = e.to_reg(val)
                return _regs[val]

            def bank_reg(i, lo=0, hi=2048):
                return ("bank%d" % i, lo, hi)

            dma_sp(consts[:], consts_d, [], [R("consts")])
            S.op("dve", lambda e: e.tensor_copy(out=ident_b.ap, in_=ident), reads=[R("consts")], writes=[ident_b.reg()])
            for m in range(3):
                S.op("dve", lambda e, m=m: e.tensor_copy(out=masks_b.ap[:, m, :], in_=consts[:, 640 + 128 * m:768 + 128 * m]),
                     reads=[R("consts")], writes=[masks_b.reg()])
            for h in range(4):
                S.op("dve", lambda e, h=h: e.tensor_copy(out=maskA2.ap[:, 0, h, :], in_=consts[:, 768:896]),
                     reads=[R("consts")], writes=[maskA2.reg()])
                S.op("dve", lambda e, h=h: e.tensor_copy(out=maskA2.ap[:, 1, h, :], in_=consts[:, 640:768]),
                     reads=[R("consts")], writes=[maskA2.reg()])
                S.op("dve", lambda e, h=h: e.tensor_copy(out=maskB4.ap[:, h, :], in_=(consts[:, 896:1024] if h % 2 == 0 else consts[:, 640:768])),
                     reads=[R("consts")], writes=[maskB4.reg()])
            S.op("pool", lambda e: e.memset(neghalf.ap, -0.5), writes=[neghalf.reg()])
            S.op("dve", lambda e: e.tensor_copy(out=tok_i.ap, in_=tokf.unsqueeze(2).to_broadcast([128, 16, 2])), reads=[R("consts")], writes=[tok_i.reg()])

            TWO_PI = float(2 * np.pi)
            pos_i = A.get("pos_i", 64, I32)
            pos_f = A.get("pos_f", 64, F32)
            ang = A.get("ang", 512, F32, "p (j f) -> p j f", j=16)
            nrot = A.get("nrot", 512, F32, "p (j f) -> p j f", j=16)
            nrot_i = A.get("nrot_i", 512, I32, "p (j f) -> p j f", j=16)
            tmp_r = A.get("tmp_r", 512, F32, "p (j f) -> p j f", j=16)
            dma_sp(pos_i.ap, pos_d, [], [pos_i.reg()])
            S.op("dve", lambda e: e.tensor_copy(out=pos_f.ap, in_=pos_i.ap), reads=[pos_i.reg()], writes=[pos_f.reg()])
            S.op("dve", lambda e: e.tensor_tensor(out=ang.ap, in0=pos_f.ap.unsqueeze(2).to_broadcast([128, 16, 8]),
                                                  in1=invf.unsqueeze(1).to_broadcast([128, 16, 8]), op=ALU.mult),
                 reads=[pos_f.reg(), R("consts")], writes=[ang.reg()])
            S.op("dve", lambda e: e.tensor_scalar(out=nrot.ap, in0=ang.ap, scalar1=1.0 / TWO_PI, scalar2=None, op0=ALU.mult),
                 reads=[ang.reg()], writes=[nrot.reg()])
            S.op("dve", lambda e: e.tensor_copy(out=nrot_i.ap, in_=nrot.ap), reads=[nrot.reg()], writes=[nrot_i.reg()])
            S.op("dve", lambda e: e.tensor_copy(out=nrot.ap, in_=nrot_i.ap), reads=[nrot_i.reg()], writes=[nrot.reg()])
            S.op("dve", lambda e: e.scalar_tensor_tensor(out=ang.ap, in0=nrot.ap, scalar=-6.28125, in1=ang.ap, op0=ALU.mult, op1=ALU.add),
                 reads=[nrot.reg(), ang.reg()], writes=[ang.reg()])
            S.op("dve", lambda e: e.scalar_tensor_tensor(out=ang.ap, in0=nrot.ap, scalar=-(TWO_PI - 6.28125), in1=ang.ap, op0=ALU.mult, op1=ALU.add),
                 reads=[nrot.reg(), ang.reg()], writes=[ang.reg()])
            PI = float(np.pi)
            S.op("dve", lambda e: e.tensor_scalar(out=tmp_r.ap, in0=ang.ap, scalar1=PI, scalar2=-TWO_PI, op0=ALU.is_gt, op1=ALU.mult),
                 reads=[ang.reg()], writes=[tmp_r.reg()])
            S.op("dve", lambda e: e.tensor_tensor(out=ang.ap, in0=ang.ap, in1=tmp_r.ap, op=ALU.add),
                 reads=[ang.reg(), tmp_r.reg()], writes=[ang.reg()])
            S.op("dve", lambda e: e.tensor_scalar(out=tmp_r.ap, in0=ang.ap, scalar1=-PI, scalar2=TWO_PI, op0=ALU.is_lt, op1=ALU.mult),
                 reads=[ang.reg()], writes=[tmp_r.reg()])
            S.op("dve", lambda e: e.tensor_tensor(out=ang.ap, in0=ang.ap, in1=tmp_r.ap, op=ALU.add),
                 reads=[ang.reg(), tmp_r.reg()], writes=[ang.reg()])
            S.op("act", lambda e: e.activation(out=sinb.ap, in_=ang.ap, func=ACTF.Sin), reads=[ang.reg()], writes=[sinb.reg()])
            S.op("dve", lambda e: e.tensor_scalar(out=tmp_r.ap, in0=ang.ap, scalar1=-1.0, scalar2=None, op0=ALU.mult),
                 reads=[ang.reg()], writes=[tmp_r.reg()])
            S.op("dve", lambda e: e.tensor_tensor(out=tmp_r.ap, in0=tmp_r.ap, in1=ang.ap, op=ALU.max),
                 reads=[ang.reg(), tmp_r.reg()], writes=[tmp_r.reg()])
            S.op("dve", lambda e: e.tensor_scalar(out=tmp_r.ap, in0=tmp_r.ap, scalar1=-1.0, scalar2=PI / 2, op0=ALU.mult, op1=ALU.add),
                 reads=[tmp_r.reg()], writes=[tmp_r.reg()])
            S.op("act", lambda e: e.activation(out=cosb.ap, in_=tmp_r.ap, func=ACTF.Sin), reads=[tmp_r.reg()], writes=[cosb.reg()])
            A.top = PERSIST_TOP

            wada = [A.get(f"wada{i}", 8 * 512 * 4, F32, "p (k n) -> p k n", k=8) for i in range(2)]
            csb = A.get("csb", 8 * 128 * 4, F32, "p (k m) -> p k m", k=8)
            cs = A.get("cs", 32, F32)
            bbc = A.get("bbc", 6144 * 4, F32)
            gmix = A.get("gmix", 4096, F32)
            gffn = A.get("gffn", 4096, F32)
            mods = A.get("mods", 6144 * 4, F32)
            dma_sp(cs.ap, cT_d, [], [cs.reg()])
            dma_pool(bbc.ap, b_ada_d.partition_broadcast(128), [], [bbc.reg()])
            dma_pool(gmix.ap, gmix_d.partition_broadcast(128), [], [gmix.reg()])
            dma_pool(gffn.ap, gffn_d.partition_broadcast(128), [], [gffn.reg()])
            S.op("act", lambda e: e.activation(out=cs.ap, in_=cs.ap, func=ACTF.Silu), reads=[cs.reg()], writes=[cs.reg()])
            S.op("dve", lambda e: e.tensor_copy(out=csb.ap.bitcast(F32R), in_=cs.ap.unsqueeze(2).to_broadcast([128, 8, 128])),
                 reads=[cs.reg()], writes=[csb.reg()])
            wada_v = w_ada_d.rearrange("(p k) n -> p k n", k=8)
            for c in range(12):
                wb = wada[c % 2]
                dma_pool(wb.ap.bitcast(F32R), wada_v[:, :, c * 512:(c + 1) * 512], [], [wb.reg()], sem="wada%d" % (c % 2))
                bk = c % 2
                for k in range(8):
                    S.op("pe", lambda e, k=k, wb=wb, bk=bk: e.matmul(banks[bk][:], lhsT=csb.ap[:, k, :].bitcast(F32R),
                                                                   rhs=wb.ap[:, k, :].bitcast(F32R), start=(k == 0), stop=(k == 7)),
                         reads=[csb.reg(), wb.reg()], writes=[bank_reg(bk)])
                mi = c // 2
                cols = slice(c * 512, (c + 1) * 512)
                S.op("dve", lambda e, bk=bk, cols=cols: e.tensor_tensor(out=mods.ap[:, cols], in0=banks[bk][:], in1=bbc.ap[:, cols], op=ALU.add),
                     reads=[bank_reg(bk), bbc.reg()], writes=[mods.reg(c * 2048, (c + 1) * 2048)])
                if mi in (1, 4):
                    g = gmix if mi == 1 else gffn
                    gc = slice((c % 2) * 512, (c % 2) * 512 + 512)
                    S.op("dve", lambda e, cols=cols, g=g, gc=gc: e.scalar_tensor_tensor(out=mods.ap[:, cols], in0=mods.ap[:, cols], scalar=1.0,
                                                                                      in1=g.ap[:, gc], op0=ALU.add, op1=ALU.mult),
                         reads=[mods.reg(c * 2048, (c + 1) * 2048), g.reg()], writes=[mods.reg(c * 2048, (c + 1) * 2048)])
            dma_sp(mods_dram, mods.ap, [mods.reg()], [R("mods_dram")], sem="st")
            if debug:
                dma_sp(dbg["mods"], mods.ap, [mods.reg()], [R("dbg_mods")], sem="st")
            A.top = PERSIST_TOP

            checkpoint(1)
            modsB = A.get("modsB", 2048 * 4, F32)
            gqk = A.get("gqk", 26 * 64 * 4, F32, "p (h d) -> p h d", h=26)
            qTa = A.get("qTa", 4 * 2048 * 2, BF16, "p (i t) -> p i t", i=4)
            kTa = A.get("kTa", 2048 * 2, BF16)
            qTb = A.get("qTb", 4 * 2048 * 2, BF16, "p (i t) -> p i t", i=4)
            kTb = A.get("kTb", 4 * 2048 * 2, BF16, "p (i t) -> p i t", i=4)
            vnat = A.get("vnat", 16 * 650 * 2, BF16, "p (j h d) -> p j h d", j=16, h=10)
            BWIDE_TOP = A.top
            dma_sp(modsB.ap, mods_dram[:, 0:2048], [R("mods_dram")], [modsB.reg()])
            gq_tmp = A.get("gq_tmp", 1024, F32)
            dma_pool(gq_tmp.ap, gqk_d.partition_broadcast(128), [], [gq_tmp.reg()])
            for h in range(26):
                if h < 8:
                    src, sc = 0, 0.125
                elif h < 10:
                    src, sc = 1, 1.0
                elif h < 18:
                    src, sc = 2, 0.125
                else:
                    src, sc = 3, 1.0
                S.op("dve", lambda e, h=h, src=src, sc=sc: e.tensor_scalar(out=gqk.ap[:, h, :], in0=gq_tmp.ap[:, src * 64:src * 64 + 64],
                                                                            scalar1=sc, scalar2=None, op0=ALU.mult),
                     reads=[gq_tmp.reg()], writes=[gqk.reg()])
            S.op("pool", lambda e: e.memset(vnat.ap, 1.0), writes=[vnat.reg()])

            w_in = A.get("w_in", 8 * 2304 * 4, F32, "p (k n) -> p k n", k=8)
            xt = [A.get(f"xt{i}", 4096, F32) for i in range(1)]
            hbuf = A.get("h", 4096, F32)
            hT = [A.get(f"hT{i}", 4096, F32, "p (k t) -> p k t", k=8) for i in range(1)]
            sq = [A.get(f"sq{i}", 2048, F32) for i in range(2)]
            qkf = [A.get(f"qkf{i}", 2048, F32) for i in range(2)]
            qkb = A.get("qkb", 1664 * 2, BF16)
            rope_t = A.get("rope_t", 4 * 8 * 8 * 4, F32, "p (a h f) -> p a h f", a=4, h=8)
            st_small = A.get("st_small", 64 * 4, F32)
            dma_pool(w_in.ap.bitcast(F32R), w_in_d.rearrange("(p k) n -> p k n", k=8), [], [w_in.reg()], sem="win")

            def rmsnorm_mod(xin, xin_reg, out_ap, out_reg, a_ap, s_ap, mod_reg, scr, tagsem):
                ss = scr.ap[:, 0:1]
                S.op("act", lambda e: e.activation(out=junk.ap,
                                                   in_=xin, func=ACTF.Square, accum_out=ss),
                     reads=[xin_reg], writes=[junk.reg(), scr.reg()])
                S.op("dve", lambda e: e.tensor_scalar(out=scr.ap[:, 1:2], in0=ss, scalar1=1.0 / 1024, scalar2=EPS, op0=ALU.mult, op1=ALU.add),
                     reads=[scr.reg()], writes=[scr.reg()])
                S.op("pool", lambda e: e.tensor_tensor(out=scr.ap[:, 2:3], in0=scr.ap[:, 1:2], in1=neghalf.ap[:, 0:1], op=ALU.pow),
                     reads=[scr.reg(), neghalf.reg()], writes=[scr.reg()])
                S.op("dve", lambda e: e.scalar_tensor_tensor(out=out_ap, in0=xin, scalar=scr.ap[:, 2:3], in1=a_ap, op0=ALU.mult, op1=ALU.mult),
                     reads=[xin_reg, scr.reg(), mod_reg], writes=[out_reg])
                S.op("dve", lambda e: e.tensor_tensor(out=out_ap, in0=out_ap, in1=s_ap, op=ALU.add),
                     reads=[out_reg, mod_reg], writes=[out_reg])

            def transpose8(src_ap, src_reg, dst, bk0=0, bk1=1):
                v = src_ap.rearrange("t (p k) -> t k p", k=8)
                for k in range(8):
                    bk = bk0 if k < 4 else bk1
                    S.op("pe", lambda e, k=k, bk=bk: e.transpose(banks[bk][:, (k % 4) * 128:(k % 4) * 128 + 128], v[:, k, :], ident),
                         reads=[src_reg, R("consts")], writes=[bank_reg(bk, (k % 4) * 512, (k % 4) * 512 + 512)])
                S.op("act", lambda e: e.activation(out=dst.ap[:, 0:4, :].bitcast(F32R), in_=banks[bk0][:].rearrange("p (k t) -> p k t", k=4), func=ACTF.Copy),
                     reads=[bank_reg(bk0)], writes=[dst.reg(0, 2048)])
                S.op("act", lambda e: e.activation(out=dst.ap[:, 4:8, :].bitcast(F32R), in_=banks[bk1][:].rearrange("p (k t) -> p k t", k=4), func=ACTF.Copy),
                     reads=[bank_reg(bk1)], writes=[dst.reg(2048, 4096)])

            chunks = [(0, 512, 8, 0), (512, 256, 2, 8), (768, 512, 8, 10), (1280, 512, 8, 18), (1792, 512, 0, 0)]
            for j in range(NT):
                xb = xt[0]
                hTb = hT[0]
                dma_sp(xb.ap, x_d[j * 128:(j + 1) * 128, :], [], [xb.reg()], sem="x0")
                rmsnorm_mod(xb.ap, xb.reg(), hbuf.ap, hbuf.reg(), modsB.ap[:, 1024:2048], modsB.ap[:, 0:1024], modsB.reg(), st_small, "b1")
                transpose8(hbuf.ap, hbuf.reg(), hTb)
                for ci, (c0, ncol, nh, h0) in enumerate(chunks):
                    bk = 2 + ci
                    for k in range(8):
                        S.op("pe", lambda e, k=k, bk=bk, c0=c0, ncol=ncol, hTb=hTb: e.matmul(
                            banks[bk][:, 0:ncol], lhsT=hTb.ap[:, k, :].bitcast(F32R), rhs=w_in.ap[:, k, c0:c0 + ncol].bitcast(F32R),
                            start=(k == 0), stop=(k == 7)),
                            reads=[hTb.reg(), w_in.reg()], writes=[bank_reg(bk)])
                for ci, (c0, ncol, nh, h0) in enumerate(chunks):
                    bk = 2 + ci
                    if nh > 0:
                        sqb = sq[ci % 2]
                        qf = qkf[ci % 2]
                        nq = nh * 64
                        S.op("act", lambda e, bk=bk, sqb=sqb, nq=nq: e.activation(out=sqb.ap[:, 0:nq], in_=banks[bk][:, 0:nq], func=ACTF.Square),
                             reads=[bank_reg(bk)], writes=[sqb.reg()])
                        ssum = st_small.ap[:, 8:8 + nh]
                        S.op("dve", lambda e, sqb=sqb, nh=nh, nq=nq, ssum=ssum: e.tensor_reduce(out=ssum, in_=sqb.ap[:, 0:nq].rearrange("p (h d) -> p h d", h=nh),
                                                                                             axis=AX.X, op=ALU.add),
                             reads=[sqb.reg()], writes=[st_small.reg()])
                        S.op("dve", lambda e, ssum=ssum: e.tensor_scalar(out=ssum, in0=ssum, scalar1=1.0 / 64, scalar2=EPS, op0=ALU.mult, op1=ALU.add),
                             reads=[st_small.reg()], writes=[st_small.reg()])
                        rstd = st_small.ap[:, 16:16 + nh]
                        S.op("pool", lambda e, ssum=ssum, rstd=rstd, nh=nh: e.tensor_tensor(out=rstd, in0=ssum, in1=neghalf.ap[:, 0:nh], op=ALU.pow),
                             reads=[st_small.reg(), neghalf.reg()], writes=[st_small.reg()])
                        qv = qf.ap[:, 0:nq].rearrange("p (h d) -> p h d", h=nh)
                        S.op("dve", lambda e, bk=bk, qv=qv, rstd=rstd, nh=nh, nq=nq: e.tensor_tensor(
                            out=qv, in0=banks[bk][:, 0:nq].rearrange("p (h d) -> p h d", h=nh),
                            in1=rstd.unsqueeze(2).to_broadcast([128, nh, 64]), op=ALU.mult),
                            reads=[bank_reg(bk), st_small.reg()], writes=[qf.reg()])
                        S.op("dve", lambda e, qv=qv, h0=h0, nh=nh: e.tensor_tensor(out=qv, in0=qv, in1=gqk.ap[:, h0:h0 + nh, :], op=ALU.mult),
                             reads=[qf.reg(), gqk.reg()], writes=[qf.reg()])
                        cb = cosb.ap[:, j, :].unsqueeze(1).to_broadcast([128, nh, 8])
                        sb_ = sinb.ap[:, j, :].unsqueeze(1).to_broadcast([128, nh, 8])
                        t1 = qv[:, :, 0:8]
                        t2 = qv[:, :, 8:16]
                        ra, rb_, rc, rd = (rope_t.ap[:, a, 0:nh, :] for a in range(4))
                        for (o_, i0, i1) in ((ra, t1, cb), (rb_, t2, sb_), (rc, t2, cb), (rd, t1, sb_)):
                            S.op("dve", lambda e, o_=o_, i0=i0, i1=i1: e.tensor_tensor(out=o_, in0=i0, in1=i1, op=ALU.mult),
                                 reads=[qf.reg(), cosb.reg(), sinb.reg()], writes=[rope_t.reg()])
                        S.op("dve", lambda e, t1=t1, ra=ra, rb_=rb_: e.tensor_tensor(out=t1, in0=ra, in1=rb_, op=ALU.subtract),
                             reads=[rope_t.reg()], writes=[qf.reg()])
                        S.op("dve", lambda e, t2=t2, rc=rc, rd=rd: e.tensor_tensor(out=t2, in0=rc, in1=rd, op=ALU.add),
                             reads=[rope_t.reg()], writes=[qf.reg()])
                        qoff = {0: 0, 1: 512, 2: 640, 3: 1152}[ci]
                        if ci == 0:
                            S.op("act", lambda e, qf=qf: e.activation(out=qkb.ap[:, 0:512].rearrange("p (i g d) -> p g i d", i=4, g=2),
                                                                      in_=qf.ap[:, 0:512].rearrange("p (g i d) -> p g i d", g=2, i=4), func=ACTF.Copy),
                                 reads=[qf.reg()], writes=[qkb.reg(0, 1024)])
                        else:
                            S.op("act", lambda e, qf=qf, nq=nq, qoff=qoff: e.activation(out=qkb.ap[:, qoff:qoff + nq], in_=qf.ap[:, 0:nq], func=ACTF.Copy),
                                 reads=[qf.reg()], writes=[qkb.reg(qoff * 2, (qoff + nq) * 2)])
                    if ci == 1:
                        S.op("act", lambda e, bk=bk, j=j: e.activation(out=vnat.ap[:, j, 0:2, 0:64], in_=banks[bk][:, 128:256].rearrange("p (h d) -> p h d", h=2), func=ACTF.Copy),
                             reads=[bank_reg(bk)], writes=[vnat.reg(j * 1300, j * 1300 + 260)])
                    if ci == 4:
                        S.op("act", lambda e, bk=bk, j=j: e.activation(out=vnat.ap[:, j, 2:10, 0:64], in_=banks[bk][:, 0:512].rearrange("p (h d) -> p h d", h=8), func=ACTF.Copy),
                             reads=[bank_reg(bk)], writes=[vnat.reg(j * 1300 + 260, (j + 1) * 1300)])
                        dma_sp(v_dram[j * 128:(j + 1) * 128, :], vnat.ap[:, j, 2:10, :].rearrange("p h d -> p (h d)"),
                               [vnat.reg(j * 1300 + 260, (j + 1) * 1300)], [R("v_dram", j, j + 1)], sem="st_vnat")
                if debug:
                    dma_sp(dbg["qk"][j * 128:(j + 1) * 128, :], qkb.ap, [qkb.reg()], [R("dbg_qk", j, j + 1)], sem="st")
                    dma_sp(dbg["v"][j * 128:(j + 1) * 128, :], vnat.ap[:, j, :, :].rearrange("p h d -> p (h d)"), [vnat.reg(j * 1300, (j + 1) * 1300)], [R("dbg_v", j, j + 1)], sem="st_dbgv")
                pb7 = banks[7][:].bitcast(BF16).rearrange("p (s t) -> p s t", s=8)
                pb0 = banks[0][:].bitcast(BF16).rearrange("p (s t) -> p s t", s=8)
                tsl = slice(j * 128, (j + 1) * 128)
                for i in range(4):
                    S.op("pe", lambda e, i=i: e.transpose(pb7[:, i, :], qkb.ap[:, i * 128:(i + 1) * 128], ident_b.ap),
                         reads=[qkb.reg(0, 1024), ident_b.reg()], writes=[bank_reg(7, i * 256, i * 256 + 256)])
                for i in range(4):
                    S.op("pe", lambda e, i=i: e.transpose(pb7[:, 4 + i, :], qkb.ap[:, 640 + i * 128:640 + i * 128 + 128], ident_b.ap),
                         reads=[qkb.reg(1280, 2304), ident_b.reg()], writes=[bank_reg(7, (4 + i) * 256, (4 + i) * 256 + 256)])
                S.op("act", lambda e, tsl=tsl: e.activation(out=qTa.ap[:, :, tsl], in_=pb7[:, 0:4, :], func=ACTF.Copy),
                     reads=[bank_reg(7)], writes=[qTa.reg()])
                S.op("dve", lambda e, tsl=tsl: e.tensor_copy(out=qTb.ap[:, :, tsl], in_=pb7[:, 4:8, :]),
                     reads=[bank_reg(7)], writes=[qTb.reg()])
                for i in range(4):
                    S.op("pe", lambda e, i=i: e.transpose(pb7[:, i, :], qkb.ap[:, 1152 + i * 128:1152 + i * 128 + 128], ident_b.ap),
                         reads=[qkb.reg(2304, 3328), ident_b.reg()], writes=[bank_reg(7, i * 256, i * 256 + 256)])
                S.op("pe", lambda e: e.transpose(pb7[:, 4, :], qkb.ap[:, 512:640], ident_b.ap),
                     reads=[qkb.reg(1024, 1280), ident_b.reg()], writes=[bank_reg(7, 1024, 1280)])
                S.op("act", lambda e, tsl=tsl: e.activation(out=kTb.ap[:, :, tsl], in_=pb7[:, 0:4, :], func=ACTF.Copy),
                     reads=[bank_reg(7)], writes=[kTb.reg()])
                S.op("dve", lambda e, tsl=tsl: e.tensor_copy(out=kTa.ap[:, tsl], in_=pb7[:, 4, :]),
                     reads=[bank_reg(7)], writes=[kTa.reg()])
            A.top = BWIDE_TOP
            if debug:
                dma_sp(dbg["kTa"], kTa.ap, [kTa.reg()], [R("dbg_kTa")], sem="st_d1")
                dma_sp(dbg["kTb"], kTb.ap.rearrange("p i t -> p (i t)"), [kTb.reg()], [R("dbg_kTb")], sem="st_d2")
                dma_sp(dbg["qTa"], qTa.ap.rearrange("p i t -> p (i t)"), [qTa.reg()], [R("dbg_qTa")], sem="st_d3")
                dma_sp(dbg["qTb"], qTb.ap.rearrange("p i t -> p (i t)"), [qTb.reg()], [R("dbg_qTb")], sem="st_d4")

            checkpoint(2)
            w_out = A.get("w_out", 8 * 1024 * 4, F32, "p (k n) -> p k n", k=8)
            v4 = A.get("v4", 16 * 520 * 2, BF16, "p (j h d) -> p j h d", j=16, h=8)
            v16 = A.get("v16", 16 * 520 * 2, BF16, "p (j h d) -> p j h d", j=16, h=8)
            modg = A.get("modg", 4096, F32)
            goutb = A.get("goutb", 4096, F32)
            pT = [A.get(f"pT{i}", 512 * 2, BF16) for i in range(4)]
            osb = [A.get(f"osb{i}", 520 * 4, F32, "p (h d) -> p h d", h=8) for i in range(2)]
            o4n = A.get("o4n", 520 * 4, F32, "p (h d) -> p h d", h=8)
            o16n = A.get("o16n", 520 * 4, F32, "p (h d) -> p h d", h=8)
            ocat = A.get("ocat", 4096, F32)
            ocT = A.get("ocT", 4096, F32, "p (k t) -> p k t", k=8)
            xb2 = A.get("xb2", 4096, F32)
            x1b = ocat
            att_s = A.get("att_s", 64 * 4, F32)
            dma_pool(w_out.ap.bitcast(F32R), w_out_d.rearrange("(p k) n -> p k n", k=8), [], [w_out.reg()], sem="win")
            dma_sp(modg.ap, mods_dram[:, 2048:3072], [R("mods_dram")], [modg.reg()])
            dma_pool(goutb.ap, gout_d.partition_broadcast(128), [], [goutb.reg()])
            dma_pool(esink.ap, sinks_d.partition_broadcast(128), [], [esink.reg()])
            S.op("act", lambda e: e.activation(out=esink.ap, in_=esink.ap, func=ACTF.Exp), reads=[esink.reg()], writes=[esink.reg()])
            v4d = v_dram.rearrange("(i d) c -> d i c", d=4)
            v16d = v_dram.rearrange("(i d) c -> d i c", d=16)
            for r in range(4):
                for m in range(4):
                    dma_sp(v4.ap[:, r * 4 + m, :, :].rearrange("p h d -> p (h d)"), v4d[r, m * 128:(m + 1) * 128, :],
                           [R("v_dram", 0, 16)], [v4.reg((r * 4 + m) * 1040, (r * 4 + m + 1) * 1040)], sem="l_v4")
            for r in range(16):
                dma_sp(v16.ap[:, r, :, :].rearrange("p h d -> p (h d)"), v16d[r, 0:128, :],
                       [R("v_dram", 0, 16)], [v16.reg(r * 1040, (r + 1) * 1040)], sem="l_v16")

            pcount = [0]

            def attn_b_tile(q_sel, k_sels, vbuf, vidx, out_banks):
                nk = len(k_sels)
                for hp2 in range(2):
                    pts = [pT[(pcount[0]) % 4], pT[(pcount[0] + 1) % 4]]
                    pcount[0] += 2
                    blocks = []
                    for hp in (2 * hp2, 2 * hp2 + 1):
                        for (ks, vt, isprev) in k_sels:
                            blocks.append((hp, ks, vt))
                    nb = len(blocks)
                    for s in range(2):
                        sbk = s
                        pt = pts[s]
                        for bi, (hp, ks, vt) in enumerate(blocks):
                            S.op("pe", lambda e, s=s, ks=ks, hp=hp, bi=bi, sbk=sbk: e.matmul(
                                banks[sbk][:, bi * 128:(bi + 1) * 128], lhsT=kTb.ap[64 * s:64 * s + 64, hp, ks],
                                rhs=qTb.ap[64 * s:64 * s + 64, hp, q_sel], start=True, stop=True),
                                reads=[kTb.reg(), qTb.reg()], writes=[bank_reg(sbk, bi * 512, (bi + 1) * 512)])
                        S.op("act", lambda e, pt=pt, sbk=sbk, nb=nb: e.activation(out=pt.ap[:, 0:nb * 128], in_=banks[sbk][:, 0:nb * 128], func=ACTF.Exp),
                             reads=[bank_reg(sbk, 0, nb * 512)], writes=[pt.reg()])
                        if nk == 2:
                            S.op("dve", lambda e, pt=pt: e.tensor_tensor(out=pt.ap[:, 0:512], in0=pt.ap[:, 0:512],
                                                                        in1=maskB4.ap[:, :, :].rearrange("p b k -> p (b k)"), op=ALU.mult),
                                 reads=[pt.reg(), maskB4.reg()], writes=[pt.reg()])
                        else:
                            S.op("dve", lambda e, pt=pt, nb=nb: e.tensor_tensor(out=pt.ap[:, 0:nb * 128].rearrange("p (b k) -> p b k", b=nb),
                                                                               in0=pt.ap[:, 0:nb * 128].rearrange("p (b k) -> p b k", b=nb),
                                                                               in1=masks_b.ap[:, 0:1, :].to_broadcast([128, nb, 128]), op=ALU.mult),
                                 reads=[pt.reg(), masks_b.reg()], writes=[pt.reg()])
                    for s in range(2):
                        pt = pts[s]
                        for hp in (2 * hp2, 2 * hp2 + 1):
                            hh = 2 * hp + s
                            ob = out_banks[hh // 4]
                            col = (hh % 4) * 65
                            mine = [(bi, b) for bi, b in enumerate(blocks) if b[0] == hp]
                            for n_, (bi, (hp_, ks, vt)) in enumerate(mine):
                                S.op("pe", lambda e, pt=pt, bi=bi, vt=vt, hh=hh, ob=ob, col=col, n_=n_, last=(n_ == len(mine) - 1): e.matmul(
                                    banks[ob][:, col:col + 65], lhsT=pt.ap[:, bi * 128:(bi + 1) * 128], rhs=vbuf.ap[:, vt, vidx + hh, :],
                                    start=(n_ == 0), stop=last),
                                    reads=[pt.reg(), vbuf.reg()], writes=[bank_reg(ob, col * 4, col * 4 + 260)])

            o4d = o4_dram.rearrange("(i d) c -> d i c", d=4)
            o16d = o16_dram.rearrange("(i d) c -> d i c", d=16)
            oi = [0]

            def evac_store(dst_ap, dst_reg):
                ob = osb[oi[0] % 2]
                oi[0] += 1
                S.op("act", lambda e, ob=ob: e.activation(out=ob.ap[:, 0:4, :], in_=banks[2][:, 0:260].rearrange("p (h d) -> p h d", h=4), func=ACTF.Copy),
                     reads=[bank_reg(2)], writes=[ob.reg(0, 1040)])
                S.op("dve", lambda e, ob=ob: e.tensor_copy(out=ob.ap[:, 4:8, :], in_=banks[3][:, 0:260].rearrange("p (h d) -> p h d", h=4)),
                     reads=[bank_reg(3)], writes=[ob.reg(1040, 2080)])
                dma_sp(dst_ap, ob.ap.rearrange("p h d -> p (h d)"), [ob.reg()], [dst_reg], sem="st")

            for r in range(4):
                for m in range(4):
                    def sel(mm, r=r):
                        st_ = r + 512 * mm
                        return slice(st_, st_ + 4 * 127 + 1, 4)
                    ks = []
                    if m > 0:
                        ks.append((sel(m - 1), r * 4 + m - 1, True))
                    ks.append((sel(m), r * 4 + m, False))
                    attn_b_tile(sel(m), ks, v4, 0, (2, 3))
                    evac_store(o4d[r, m * 128:(m + 1) * 128, :], R("o4_dram", r * 4 + m, r * 4 + m + 1))
            for r in range(16):
                sl = slice(r, r + 16 * 127 + 1, 16)
                attn_b_tile(sl, [(sl, r, False)], v16, 0, (2, 3))
                evac_store(o16d[r, 0:128, :], R("o16_dram", r, r + 1))

            checkpoint(3)
            for j in range(NT):
                tsl = slice(j * 128, (j + 1) * 128)
                dma_sp(xb2.ap, x_d[tsl, :], [], [xb2.reg()], sem="x2")
                dma_sp(o4n.ap.rearrange("p h d -> p (h d)"), o4_dram[tsl, :], [R("o4_dram", 0, 16)], [o4n.reg()], sem="on")
                dma_sp(o16n.ap.rearrange("p h d -> p (h d)"), o16_dram[tsl, :], [R("o16_dram", 0, 16)], [o16n.reg()], sem="on")
                for kvh in range(2):
                    jks = ([j - 1] if j > 0 else []) + [j]
                    pa = pT[2 * kvh].ap if False else None
                    pt0 = pT[(pcount[0]) % 4]
                    pt1 = pT[(pcount[0] + 1) % 4]
                    pcount[0] += 2
                    pts = [pt0, pt1]
                    for n_, jk in enumerate(jks):
                        sbk = 4 + n_
                        S.op("pe", lambda e, kvh=kvh, jk=jk, sbk=sbk, tsl=tsl: e.matmul(
                            banks[sbk][:].rearrange("p (i t) -> p i t", i=4), lhsT=kTa.ap[64 * kvh:64 * kvh + 64, jk * 128:(jk + 1) * 128],
                            rhs=qTa.ap[64 * kvh:64 * kvh + 64, :, tsl], start=True, stop=True),
                            reads=[kTa.reg(), qTa.reg()], writes=[bank_reg(sbk)])
                        pt = pts[n_]
                        S.op("act", lambda e, pt=pt, sbk=sbk: e.activation(out=pt.ap, in_=banks[sbk][:], func=ACTF.Exp),
                             reads=[bank_reg(sbk)], writes=[pt.reg()])
                        midx = 1 if jk == j else 0
                        S.op("dve", lambda e, pt=pt, midx=midx: e.tensor_tensor(out=pt.ap, in0=pt.ap, in1=maskA2.ap[:, midx, :, :].rearrange("p h k -> p (h k)"), op=ALU.mult),
                             reads=[pt.reg(), maskA2.reg()], writes=[pt.reg()])
                    ob = 6 + kvh
                    for i in range(4):
                        for n_, jk in enumerate(jks):
                            pt = pts[n_]
                            S.op("pe", lambda e, pt=pt, i=i, jk=jk, kvh=kvh, ob=ob, n_=n_, last=(n_ == len(jks) - 1): e.matmul(
                                banks[ob][:, i * 65:(i + 1) * 65], lhsT=pt.ap[:, i * 128:(i + 1) * 128], rhs=vnat.ap[:, jk, kvh, :],
                                start=(n_ == 0), stop=last),
                                reads=[pt.reg(), vnat.reg()], writes=[bank_reg(ob, i * 260, (i + 1) * 260)])
                ks = ([(slice((j - 1) * 128, j * 128), j - 1, True)] if j > 0 else []) + [(tsl, j, False)]
                if j == 0:
                    attn_b_tile(tsl, ks, vnat, 2, (2, 3))
                else:
                    attn_b_tile(tsl, ks, vnat, 2, (2, 3))
                den = att_s.ap[:, 0:8]
                for kvh in range(2):
                    S.op("dve", lambda e, kvh=kvh: e.tensor_tensor(out=att_s.ap[:, kvh * 4:kvh * 4 + 4], in0=banks[6 + kvh][:, 0:260].rearrange("p (h d) -> p h d", h=4)[:, :, 64],
                                                                  in1=esink.ap[:, kvh * 4:kvh * 4 + 4], op=ALU.add),
                         reads=[bank_reg(6 + kvh), esink.reg()], writes=[att_s.reg()])
                S.op("dve", lambda e: e.reciprocal(out=att_s.ap[:, 8:16], in_=att_s.ap[:, 0:8]), reads=[att_s.reg()], writes=[att_s.reg()])
                for kvh in range(2):
                    S.op("dve", lambda e, kvh=kvh: e.tensor_tensor(out=ocat.ap[:, kvh * 256:(kvh + 1) * 256].rearrange("p (h d) -> p h d", h=4),
                                                                  in0=banks[6 + kvh][:, 0:260].rearrange("p (h d) -> p h d", h=4)[:, :, 0:64],
                                                                  in1=att_s.ap[:, 8 + kvh * 4:12 + kvh * 4].unsqueeze(2).to_broadcast([128, 4, 64]), op=ALU.mult),
                         reads=[bank_reg(6 + kvh), att_s.reg()], writes=[ocat.reg(kvh * 1024, (kvh + 1) * 1024)])
                S.op("dve", lambda e: e.tensor_tensor(out=o4n.ap, in0=o4n.ap, in1=o16n.ap, op=ALU.add), reads=[o4n.reg(), o16n.reg()], writes=[o4n.reg()])
                for half in range(2):
                    S.op("dve", lambda e, half=half: e.tensor_tensor(out=o4n.ap[:, half * 4:half * 4 + 4, :], in0=o4n.ap[:, half * 4:half * 4 + 4, :],
                                                                    in1=banks[2 + half][:, 0:260].rearrange("p (h d) -> p h d", h=4), op=ALU.add),
                         reads=[o4n.reg(), bank_reg(2 + half)], writes=[o4n.reg()])
                S.op("dve", lambda e: e.reciprocal(out=att_s.ap[:, 16:24], in_=o4n.ap[:, :, 64]), reads=[o4n.reg()], writes=[att_s.reg()])
                S.op("dve", lambda e: e.tensor_tensor(out=ocat.ap[:, 512:1024].rearrange("p (h d) -> p h d", h=8), in0=o4n.ap[:, :, 0:64],
                                                      in1=att_s.ap[:, 16:24].unsqueeze(2).to_broadcast([128, 8, 64]), op=ALU.mult),
                     reads=[o4n.reg(), att_s.reg()], writes=[ocat.reg(2048, 4096)])
                for gi in range(2):
                    gsl = slice(gi * 512, (gi + 1) * 512)
                    S.op("act", lambda e, gsl=gsl, gi=gi: e.activation(out=junk.ap[:, 0:512], in_=ocat.ap[:, gsl], func=ACTF.Square, accum_out=att_s.ap[:, 24 + gi:25 + gi]),
                         reads=[ocat.reg(gi * 2048, (gi + 1) * 2048)], writes=[junk.reg(), att_s.reg()])
                S.op("dve", lambda e: e.tensor_scalar(out=att_s.ap[:, 26:28], in0=att_s.ap[:, 24:26], scalar1=1.0 / 512, scalar2=EPS, op0=ALU.mult, op1=ALU.add),
                     reads=[att_s.reg()], writes=[att_s.reg()])
                S.op("pool", lambda e: e.tensor_tensor(out=att_s.ap[:, 28:30], in0=att_s.ap[:, 26:28], in1=neghalf.ap[:, 0:2], op=ALU.pow),
                     reads=[att_s.reg(), neghalf.reg()], writes=[att_s.reg()])
                for gi in range(2):
                    gsl = slice(gi * 512, (gi + 1) * 512)
                    S.op("dve", lambda e, gsl=gsl, gi=gi: e.scalar_tensor_tensor(out=ocat.ap[:, gsl], in0=ocat.ap[:, gsl], scalar=att_s.ap[:, 28 + gi:29 + gi],
                                                                               in1=goutb.ap[:, gsl], op0=ALU.mult, op1=ALU.mult),
                         reads=[ocat.reg(gi * 2048, (gi + 1) * 2048), att_s.reg(), goutb.reg()], writes=[ocat.reg(gi * 2048, (gi + 1) * 2048)])
                if debug:
                    dma_sp(dbg["ocat"][tsl, :], ocat.ap, [ocat.reg()], [R("dbg_ocat", j, j + 1)], sem="st")
                transpose8(ocat.ap, ocat.reg(), ocT, 0, 1)
                for nh_ in range(2):
                    bk = 4 + nh_
                    for k in range(8):
                        S.op("pe", lambda e, k=k, bk=bk, nh_=nh_: e.matmul(banks[bk][:], lhsT=ocT.ap[:, k, :].bitcast(F32R),
                                                                         rhs=w_out.ap[:, k, nh_ * 512:(nh_ + 1) * 512].bitcast(F32R), start=(k == 0), stop=(k == 7)),
                             reads=[ocT.reg(), w_out.reg()], writes=[bank_reg(bk)])
                for nh_ in range(2):
                    csl = slice(nh_ * 512, (nh_ + 1) * 512)
                    S.op("dve", lambda e, nh_=nh_, csl=csl: e.tensor_tensor(out=x1b.ap[:, csl], in0=banks[4 + nh_][:], in1=modg.ap[:, csl], op=ALU.mult),
                         reads=[bank_reg(4 + nh_), modg.reg()], writes=[x1b.reg(nh_ * 2048, (nh_ + 1) * 2048)])
                S.op("dve", lambda e: e.tensor_tensor(out=x1b.ap, in0=x1b.ap, in1=xb2.ap, op=ALU.add), reads=[x1b.reg(), xb2.reg()], writes=[x1b.reg()])
                dma_sp(x1_dram[tsl, :], x1b.ap, [x1b.reg()], [R("x1_dram", j, j + 1)], sem="st")
                if debug:
                    dma_sp(dbg["x1"][tsl, :], x1b.ap, [x1b.reg()], [R("dbg_x1", j, j + 1)], sem="st")
            A.top = PERSIST_TOP

            checkpoint(4)
            modsC = A.get("modsC", 3072 * 4, F32)
            widx = A.get("widx", NOV * 4, I32)
            CKEEP = A.top
            wr = A.get("wr", 8 * 256 * 4, F32, "p (k n) -> p k n", k=8)
            wgus = A.get("wgus", 8 * 512 * 4, F32, "p (k n) -> p k n", k=8)
            wds = A.get("wds", 2 * 1024 * 4, F32, "p (c n) -> p c n", c=2)
            rbias = A.get("rbias", 1024, F32)
            mcum = A.get("mcum", 1024, F32)
            x1t = [A.get(f"x1t{i}", 4096, F32) for i in range(2)]
            h2 = A.get("h2", 4096, F32)
            h2T = A.get("h2T", 4096, F32, "p (k t) -> p k t", k=8)
            sc = A.get("sc", 1024, F32)
            bi_ = A.get("bi", 1024, F32)
            msk = A.get("msk", 1024, F32)
            eqt = A.get("eqt", 1024, F32)
            offm = A.get("offm", 1024, F32)
            rs = A.get("rs", 128 * 4, F32)
            off8f = A.get("off8f", 32, F32)
            sc8 = A.get("sc8", 32, F32)
            mx8 = A.get("mx8", 32, F32)
            idx8 = A.get("idx8", 32, U32)
            idx8f = A.get("idx8f", 32, F32)
            shs = A.get("shs", 1024, F32)
            shh = A.get("shh", 1024, F32)
            shT = A.get("shT", 1024, F32, "p (c t) -> p c t", c=2)
            baseb = A.get("baseb", 4096, F32)
            zrow = A.get("zrow", 4096, F32)
            listinit = A.get("listinit", (NE + NOV) * 2 * 4, I32)
            posall = A.get("posall", 16 * 256 * 4, F32, "p (j e) -> p j e", j=16)
            idxall = A.get("idxall", 16 * 8 * 4, F32, "p (j k) -> p j k", j=16)
            ovT = A.get("ovT", 32, F32)
            rankT = A.get("rankT", 32, F32)
            diagR = A.get("diagR", 512, F32)
            ovb = A.get("ovb", 1024, F32)
            diffr = A.get("diffr", 1024, F32)
            selT = A.get("selT", 2 * NOV * 4, F32, "p (c o) -> p c o", c=2)
            widxf = A.get("widxf", NOV * 4, F32)
            CWIDE = A.top
            dma_sp(modsC.ap, mods_dram[:, 3072:6144], [R("mods_dram")], [modsC.reg()])
            dma_sp(wr.ap, wr_d.rearrange("(p k) n -> p k n", k=8), [], [wr.reg()])
            dma_pool(wgus.ap[:, :, 0:256].bitcast(F32R), wgs_d.rearrange("(p k) n -> p k n", k=8), [], [wgus.reg()], sem="win")
            dma_pool(wgus.ap[:, :, 256:512].bitcast(F32R), wus_d.rearrange("(p k) n -> p k n", k=8), [], [wgus.reg()], sem="win")
            dma_pool(wds.ap.bitcast(F32R), wds_d.rearrange("(p c) n -> p c n", c=2), [], [wds.reg()], sem="win")
            dma_pool(rbias.ap, rb_d.partition_broadcast(128), [], [rbias.reg()])
            S.op("pool", lambda e: e.memset(mcum.ap, 0.0), writes=[mcum.reg()])
            S.op("pool", lambda e: e.memset(zrow.ap, 0.0), writes=[zrow.reg()])
            dma_sp(h2_dram[2048:2049, :], zrow.ap[0:1, :], [zrow.reg()], [R("h2_dram", 16, 17)], sem="st")
            S.op("pool", lambda e: e.memset(listinit.ap, 2048), writes=[listinit.reg()])
            dma_sp(list_dram.rearrange("(p n) o -> p (n o)", p=128), listinit.ap[:, 0:(NE + NOV) * 2], [listinit.reg()], [R("list_dram", 0, 1000)], sem="st_listinit")

            NGRP = 8
            for j in range(NT):
                tsl = slice(j * 128, (j + 1) * 128)
                xb = x1t[j % 2]
                dma_sp(xb.ap, x1_dram[tsl, :], [R("x1_dram", j, j + 1)], [xb.reg()], sem="x1%d" % (j % 2))
                rmsnorm_mod(xb.ap, xb.reg(), h2.ap, h2.reg(), modsC.ap[:, 1024:2048], modsC.ap[:, 0:1024], modsC.reg(), rs, "c1")
                dma_sp(h2_dram[tsl, :], h2.ap, [h2.reg()], [R("h2_dram", j, j + 1)], sem="st")
                if debug:
                    dma_sp(dbg["h2"][tsl, :], h2.ap, [h2.reg()], [R("dbg_h2", j, j + 1)], sem="st")
                transpose8(h2.ap, h2.reg(), h2T, 0, 1)
                for k in range(8):
                    S.op("pe", lambda e, k=k: e.matmul(banks[2][:, 0:256], lhsT=h2T.ap[:, k, :], rhs=wr.ap[:, k, :], start=(k == 0), stop=(k == 7)),
                         reads=[h2T.reg(), wr.reg()], writes=[bank_reg(2)])
                for k in range(8):
                    S.op("pe", lambda e, k=k: e.matmul(banks[4][:], lhsT=h2T.ap[:, k, :].bitcast(F32R), rhs=wgus.ap[:, k, :].bitcast(F32R), start=(k == 0), stop=(k == 7)),
                         reads=[h2T.reg(), wgus.reg()], writes=[bank_reg(4)])
                S.op("act", lambda e: e.activation(out=sc.ap, in_=banks[2][:, 0:256], func=ACTF.Sigmoid), reads=[bank_reg(2)], writes=[sc.reg()])
                S.op("act", lambda e: e.activation(out=shs.ap, in_=banks[4][:, 0:256], func=ACTF.Silu), reads=[bank_reg(4)], writes=[shs.reg()])
                S.op("dve", lambda e: e.tensor_tensor(out=shh.ap, in0=shs.ap, in1=banks[4][:, 256:512], op=ALU.mult), reads=[shs.reg(), bank_reg(4)], writes=[shh.reg()])
                shv = shh.ap.rearrange("t (p c) -> t c p", c=2)
                for c in range(2):
                    S.op("pe", lambda e, c=c: e.transpose(banks[5][:, c * 128:(c + 1) * 128], shv[:, c, :], ident),
                         reads=[shh.reg(), R("consts")], writes=[bank_reg(5, c * 512, c * 512 + 512)])
                S.op("act", lambda e: e.activation(out=shT.ap.bitcast(F32R), in_=banks[5][:, 0:256].rearrange("p (c t) -> p c t", c=2), func=ACTF.Copy),
                     reads=[bank_reg(5)], writes=[shT.reg()])
                for nh_ in range(2):
                    for c in range(2):
                        S.op("pe", lambda e, c=c, nh_=nh_: e.matmul(banks[6 + nh_][:], lhsT=shT.ap[:, c, :].bitcast(F32R),
                                                                  rhs=wds.ap[:, c, nh_ * 512:(nh_ + 1) * 512].bitcast(F32R), start=(c == 0), stop=(c == 1)),
                             reads=[shT.reg(), wds.reg()], writes=[bank_reg(6 + nh_)])
                for nh_ in range(2):
                    csl = slice(nh_ * 512, (nh_ + 1) * 512)
                    S.op("dve", lambda e, nh_=nh_, csl=csl: e.tensor_tensor(out=baseb.ap[:, csl], in0=banks[6 + nh_][:], in1=modsC.ap[:, 2048 + nh_ * 512:2048 + (nh_ + 1) * 512], op=ALU.mult),
                         reads=[bank_reg(6 + nh_), modsC.reg()], writes=[baseb.reg(nh_ * 2048, (nh_ + 1) * 2048)])
                S.op("dve", lambda e, xb=xb: e.tensor_tensor(out=baseb.ap, in0=baseb.ap, in1=xb.ap, op=ALU.add), reads=[baseb.reg(), xb.reg()], writes=[baseb.reg()])
                dma_sp(base_dram[tsl, :], baseb.ap, [baseb.reg()], [R("base_dram", j, j + 1)], sem="st")
                if debug:
                    dma_sp(dbg["base"][tsl, :], baseb.ap, [baseb.reg()], [R("dbg_base", j, j + 1)], sem="st")
                S.op("dve", lambda e: e.tensor_tensor(out=bi_.ap, in0=sc.ap, in1=rbias.ap, op=ALU.add), reads=[sc.reg(), rbias.reg()], writes=[bi_.reg()])
                bv = bi_.ap.rearrange("p (g e) -> p g e", g=NGRP)
                m1 = rs.ap[:, 0:8]
                m2 = rs.ap[:, 8:16]
                S.op("dve", lambda e: e.tensor_reduce(out=m1, in_=bv, axis=AX.X, op=ALU.max), reads=[bi_.reg()], writes=[rs.reg()])
                ev = eqt.ap.rearrange("p (g e) -> p g e", g=NGRP)
                S.op("dve", lambda e: e.tensor_tensor(out=ev, in0=bv, in1=m1.unsqueeze(2).to_broadcast([128, 8, 32]), op=ALU.is_equal),
                     reads=[bi_.reg(), rs.reg()], writes=[eqt.reg()])
                S.op("dve", lambda e: e.scalar_tensor_tensor(out=eqt.ap, in0=eqt.ap, scalar=-1e9, in1=bi_.ap, op0=ALU.mult, op1=ALU.add),
                     reads=[eqt.reg(), bi_.reg()], writes=[eqt.reg()])
                S.op("dve", lambda e: e.tensor_reduce(out=m2, in_=ev, axis=AX.X, op=ALU.max), reads=[eqt.reg()], writes=[rs.reg()])
                gs = rs.ap[:, 16:24]
                S.op("dve", lambda e: e.tensor_tensor(out=gs, in0=m1, in1=m2, op=ALU.add), reads=[rs.reg()], writes=[rs.reg()])
                cmp = rs.ap[:, 32:96].rearrange("p (a b) -> p a b", a=8)
                S.op("dve", lambda e: e.tensor_tensor(out=cmp, in0=gs.unsqueeze(1).to_broadcast([128, 8, 8]), in1=gs.unsqueeze(2).to_broadcast([128, 8, 8]), op=ALU.is_gt),
                     reads=[rs.reg()], writes=[rs.reg()])
                cntg = rs.ap[:, 24:32]
                S.op("dve", lambda e: e.tensor_reduce(out=cntg, in_=cmp, axis=AX.X, op=ALU.add), reads=[rs.reg()], writes=[rs.reg()])
                S.op("dve", lambda e: e.tensor_scalar(out=cntg, in0=cntg, scalar1=3.5, scalar2=-1e9, op0=ALU.is_gt, op1=ALU.mult),
                     reads=[rs.reg()], writes=[rs.reg()])
                mv = msk.ap.rearrange("p (g e) -> p g e", g=NGRP)
                S.op("dve", lambda e: e.tensor_tensor(out=mv, in0=bv, in1=cntg.unsqueeze(2).to_broadcast([128, 8, 32]), op=ALU.add),
                     reads=[bi_.reg(), rs.reg()], writes=[msk.reg()])
                S.op("dve", lambda e: e.max(out=mx8.ap, in_=msk.ap), reads=[msk.reg()], writes=[mx8.reg()])
                S.op("dve", lambda e: e.max_index(out=idx8.ap, in_max=mx8.ap, in_values=msk.ap), reads=[msk.reg(), mx8.reg()], writes=[idx8.reg()])
                S.op("dve", lambda e: e.tensor_copy(out=idx8f.ap, in_=idx8.ap), reads=[idx8.reg()], writes=[idx8f.reg()])
                S.op("dve", lambda e: e.tensor_scalar(out=eqt.ap, in0=msk.ap, scalar1=mx8.ap[:, 7:8], scalar2=None, op0=ALU.is_ge),
                     reads=[msk.reg(), mx8.reg()], writes=[eqt.reg()])
                S.op("pe", lambda e: e.matmul(banks[3][:, 0:256], lhsT=tri, rhs=eqt.ap, start=True, stop=False),
                     reads=[eqt.reg(), R("consts")], writes=[bank_reg(3)])
                S.op("pe", lambda e: e.matmul(banks[3][:, 0:256], lhsT=ones, rhs=mcum.ap, start=False, stop=True),
                     reads=[mcum.reg(), R("consts")], writes=[bank_reg(3)])
                S.op("dve", lambda e: e.tensor_tensor(out=mcum.ap, in0=mcum.ap, in1=eqt.ap, op=ALU.add), reads=[mcum.reg(), eqt.reg()], writes=[mcum.reg()])
                S.op("dve", lambda e, j=j: e.tensor_copy(out=posall.ap[:, j, :], in_=banks[3][:, 0:256]), reads=[bank_reg(3)], writes=[posall.reg(j * 1024, (j + 1) * 1024)])
                S.op("dve", lambda e, j=j: e.tensor_copy(out=idxall.ap[:, j, :], in_=idx8f.ap), reads=[idx8f.reg()], writes=[idxall.reg(j * 32, j * 32 + 32)])
                for k in range(8):
                    S.op("dve", lambda e, k=k: e.scalar_tensor_tensor(out=junk.ap[:, 256:512], in0=iota_e, scalar=idx8f.ap[:, k:k + 1], in1=sc.ap,
                                                                     op0=ALU.is_equal, op1=ALU.mult, accum_out=sc8.ap[:, k:k + 1]),
                         reads=[idx8f.reg(), sc.reg(), R("consts")], writes=[junk.reg(), sc8.reg(k * 4, k * 4 + 4)])
                ssum = rs.ap[:, 96:97]
                S.op("dve", lambda e: e.tensor_reduce(out=ssum, in_=sc8.ap, axis=AX.X, op=ALU.add), reads=[sc8.reg()], writes=[rs.reg()])
                S.op("dve", lambda e: e.reciprocal(out=rs.ap[:, 97:98], in_=ssum), reads=[rs.reg()], writes=[rs.reg()])
                S.op("dve", lambda e, j=j: e.tensor_scalar(out=gate8.ap[:, j, :], in0=sc8.ap, scalar1=rs.ap[:, 97:98], scalar2=2.5, op0=ALU.mult, op1=ALU.mult),
                     reads=[sc8.reg(), rs.reg()], writes=[gate8.reg(j * 32, j * 32 + 32)])

            for c in range(2):
                S.op("pe", lambda e, c=c: e.matmul(banks[3][:, c:c + 1], lhsT=mcum.ap[:, c * 128:(c + 1) * 128], rhs=ones[:, 0:1], start=True, stop=True),
                     reads=[mcum.reg(), R("consts")], writes=[bank_reg(3)])
            S.op("dve", lambda e: e.tensor_scalar(out=ovT.ap[:, 0:2], in0=banks[3][:, 0:2], scalar1=float(CAP) + 0.5, scalar2=None, op0=ALU.is_gt),
                 reads=[bank_reg(3)], writes=[ovT.reg()])
            S.op("pe", lambda e: e.matmul(banks[2][:, 0:2], lhsT=tri, rhs=ovT.ap[:, 0:2], start=True, stop=False),
                 reads=[ovT.reg(), R("consts")], writes=[bank_reg(2)])
            S.op("pe", lambda e: e.matmul(banks[2][:, 1:2], lhsT=ones, rhs=ovT.ap[:, 0:1], start=False, stop=True),
                 reads=[ovT.reg(), R("consts")], writes=[bank_reg(2)])
            S.op("dve", lambda e: e.tensor_copy(out=rankT.ap[:, 0:2], in_=banks[2][:, 0:2]), reads=[bank_reg(2)], writes=[rankT.reg()])
            for c in range(2):
                S.op("dve", lambda e, c=c: e.tensor_scalar(out=diagR.ap, in0=ident, scalar1=rankT.ap[:, c:c + 1], scalar2=None, op0=ALU.mult),
                     reads=[rankT.reg(), R("consts")], writes=[diagR.reg()])
                S.op("pe", lambda e, c=c: e.matmul(banks[3][:, c * 128:(c + 1) * 128], lhsT=ones, rhs=diagR.ap, start=True, stop=True),
                     reads=[diagR.reg(), R("consts")], writes=[bank_reg(3)])
            S.op("dve", lambda e: e.tensor_scalar(out=ovb.ap, in0=banks[3][:, 0:256], scalar1=float(NOV) - 0.5, scalar2=1e7, op0=ALU.is_gt, op1=ALU.mult),
                 reads=[bank_reg(3)], writes=[ovb.reg()])
            S.op("dve", lambda e: e.scalar_tensor_tensor(out=ovb.ap, in0=banks[3][:, 0:256], scalar=float(CAP), in1=ovb.ap, op0=ALU.mult, op1=ALU.add),
                 reads=[bank_reg(3), ovb.reg()], writes=[ovb.reg()])
            S.op("dve", lambda e: e.tensor_scalar(out=ovb.ap, in0=ovb.ap, scalar1=float((NE - 1) * CAP), scalar2=None, op0=ALU.add),
                 reads=[ovb.reg()], writes=[ovb.reg()])
            S.op("dve", lambda e: e.scalar_tensor_tensor(out=diffr.ap, in0=iota_e, scalar=float(CAP), in1=ovb.ap, op0=ALU.mult, op1=ALU.subtract),
                 reads=[ovb.reg(), R("consts")], writes=[diffr.reg()])
            for c in range(2):
                S.op("dve", lambda e, c=c: e.tensor_scalar(out=selT.ap[:, c, :], in0=iota_e[:, 0:NOV], scalar1=rankT.ap[:, c:c + 1], scalar2=ovT.ap[:, c:c + 1],
                                                           op0=ALU.is_equal, op1=ALU.mult),
                     reads=[rankT.reg(), ovT.reg(), R("consts")], writes=[selT.reg()])
            for c in range(2):
                S.op("pe", lambda e, c=c: e.matmul(banks[2][:, 0:NOV], lhsT=(eid0 if c == 0 else eid1), rhs=selT.ap[:, c, :], start=(c == 0), stop=(c == 1)),
                     reads=[selT.reg(), R("consts")], writes=[bank_reg(2)])
            S.op("dve", lambda e: e.tensor_scalar(out=widxf.ap, in0=banks[2][:, 0:NOV], scalar1=128.0, scalar2=tokf[:, 0:1], op0=ALU.mult, op1=ALU.add),
                 reads=[bank_reg(2), R("consts")], writes=[widxf.reg()])
            S.op("dve", lambda e: e.tensor_copy(out=widx.ap, in_=widxf.ap), reads=[widxf.reg()], writes=[widx.reg()])

            for j in range(NT):
                pj = posall.ap[:, j, :]
                S.op("dve", lambda e, pj=pj: e.tensor_scalar(out=eqt.ap, in0=pj, scalar1=float(CAP) - 0.5, scalar2=None, op0=ALU.is_lt),
                     reads=[posall.reg(j * 1024, (j + 1) * 1024)], writes=[eqt.reg()])
                S.op("dve", lambda e: e.tensor_tensor(out=offm.ap, in0=eqt.ap, in1=diffr.ap, op=ALU.mult), reads=[eqt.reg(), diffr.reg()], writes=[offm.reg()])
                S.op("dve", lambda e: e.tensor_tensor(out=offm.ap, in0=offm.ap, in1=ovb.ap, op=ALU.add), reads=[offm.reg(), ovb.reg()], writes=[offm.reg()])
                S.op("dve", lambda e, pj=pj: e.tensor_tensor(out=offm.ap, in0=offm.ap, in1=pj, op=ALU.add), reads=[offm.reg(), posall.reg(j * 1024, (j + 1) * 1024)], writes=[offm.reg()])
                S.op("dve", lambda e, pj=pj: e.tensor_scalar(out=eqt.ap, in0=pj, scalar1=2.0 * CAP - 0.5, scalar2=1e7, op0=ALU.is_gt, op1=ALU.mult),
                     reads=[posall.reg(j * 1024, (j + 1) * 1024)], writes=[eqt.reg()])
                S.op("dve", lambda e: e.tensor_tensor(out=offm.ap, in0=offm.ap, in1=eqt.ap, op=ALU.add), reads=[offm.reg(), eqt.reg()], writes=[offm.reg()])
                for k in range(8):
                    S.op("dve", lambda e, k=k, j=j: e.scalar_tensor_tensor(out=junk.ap[:, 0:256], in0=iota_e, scalar=idxall.ap[:, j, k:k + 1], in1=offm.ap,
                                                                          op0=ALU.is_equal, op1=ALU.mult, accum_out=off8f.ap[:, k:k + 1]),
                         reads=[idxall.reg(j * 32, j * 32 + 32), offm.reg(), R("consts")], writes=[junk.reg(), off8f.reg(k * 4, k * 4 + 4)])
                S.op("dve", lambda e, j=j: e.tensor_copy(out=off8.ap[:, j, :], in_=off8f.ap), reads=[off8f.reg()], writes=[off8.reg(j * 32, j * 32 + 32)])
                for k in range(8):
                    S.op("pool", lambda e, j=j, k=k: e.indirect_dma_start(out=list_dram[:, :], out_offset=bass.IndirectOffsetOnAxis(ap=off8.ap[:, j, k:k + 1], axis=0),
                                                                         in_=tok_i.ap[:, j, :], in_offset=None, bounds_check=breg(e, (NE + NOV) * CAP - 1), oob_is_err=False),
                         reads=[off8.reg(j * 32, j * 32 + 32), tok_i.reg()], writes=[R("list_dram", j * 8 + k + 1, j * 8 + k + 2)], dsem="lsc")
            if debug:
                dma_sp(dbg["off8"], off8.ap.rearrange("p j k -> p (j k)"), [off8.reg()], [R("dbg_off8")], sem="st")
                dma_sp(dbg["gate8"], gate8.ap.rearrange("p j k -> p (j k)"), [gate8.reg()], [R("dbg_gate8")], sem="st")

            checkpoint(5)
            A.top = CKEEP
            NW = 3
            wgu = [A.get(f"wgu{i}", 8 * 512 * 4, F32, "p (k n) -> p k n", k=8) for i in range(NW)]
            wdn = [A.get(f"wdn{i}", 2 * 1024 * 4, F32, "p (c n) -> p c n", c=2) for i in range(NW)]
            xe = [A.get(f"xe{i}", 4096, F32) for i in range(NW)]
            lidx = [A.get(f"lidx{i}", 8, I32) for i in range(NW)]
            xeT = [A.get(f"xeT{i}", 4096, F32, "p (k t) -> p k t", k=8) for i in range(2)]
            es = [A.get(f"es{i}", 1024, F32) for i in range(2)]
            eh = [A.get(f"eh{i}", 1024, F32) for i in range(2)]
            ehT = [A.get(f"ehT{i}", 1024, F32, "p (c t) -> p c t", c=2) for i in range(2)]
            ysb = [A.get(f"ysb{i}", 4096, F32) for i in range(2)]

            def load_expert(e_):
                s_ = e_ % NW
                dma_pool(wgu[s_].ap[:, :, 0:256].bitcast(F32R), wge_d[e_].rearrange("(p k) n -> p k n", k=8), [], [wgu[s_].reg()], sem="wg%d" % s_)
                dma_pool(wgu[s_].ap[:, :, 256:512].bitcast(F32R), wue_d[e_].rearrange("(p k) n -> p k n", k=8), [], [wgu[s_].reg()], sem="wg%d" % s_)
                dma_pool(wdn[s_].ap.bitcast(F32R), wde_d[e_].rearrange("(p c) n -> p c n", c=2), [], [wdn[s_].reg()], sem="wd%d" % s_)
                dma_sp(lidx[s_].ap[:, 0:2], list_dram[e_ * CAP:(e_ + 1) * CAP, :], [R("list_dram", 0, 1000)], [lidx[s_].reg()], sem="li%d" % s_)
                S.op("pool", lambda e, s_=s_: e.indirect_dma_start(out=xe[s_].ap, out_offset=None, in_=h2_dram[:, :],
                                                                   in_offset=bass.IndirectOffsetOnAxis(ap=lidx[s_].ap[:, 0:1], axis=0),
                                                                   bounds_check=breg(e, 2048), oob_is_err=False),
                     reads=[lidx[s_].reg(), R("h2_dram", 0, 17)], writes=[xe[s_].reg()], dsem="xg%d" % s_)

            for e_ in range(min(NW - 1, NE)):
                load_expert(e_)
            for e_ in range(NE):
                if e_ + NW - 1 < NE:
                    load_expert(e_ + NW - 1)
                s_ = e_ % NW
                d_ = e_ % 2
                b0, b1 = (0, 1) if d_ == 0 else (2, 3)
                transpose8(xe[s_].ap, xe[s_].reg(), xeT[d_], b0, b1)
                for k in range(8):
                    S.op("pe", lambda e, k=k, s_=s_, d_=d_: e.matmul(banks[4][:], lhsT=xeT[d_].ap[:, k, :].bitcast(F32R), rhs=wgu[s_].ap[:, k, :].bitcast(F32R),
                                                                    start=(k == 0), stop=(k == 7)),
                         reads=[xeT[d_].reg(), wgu[s_].reg()], writes=[bank_reg(4)])
                S.op("act", lambda e, d_=d_: e.activation(out=es[d_].ap, in_=banks[4][:, 0:256], func=ACTF.Silu), reads=[bank_reg(4)], writes=[es[d_].reg()])
                S.op("dve", lambda e, d_=d_: e.tensor_tensor(out=eh[d_].ap, in0=es[d_].ap, in1=banks[4][:, 256:512], op=ALU.mult),
                     reads=[es[d_].reg(), bank_reg(4)], writes=[eh[d_].reg()])
                ehv = eh[d_].ap.rearrange("t (p c) -> t c p", c=2)
                for c in range(2):
                    S.op("pe", lambda e, c=c, ehv=ehv, d_=d_: e.transpose(banks[5][:, c * 128:(c + 1) * 128], ehv[:, c, :], ident),
                         reads=[eh[d_].reg(), R("consts")], writes=[bank_reg(5, c * 512, c * 512 + 512)])
                S.op("act", lambda e, d_=d_: e.activation(out=ehT[d_].ap.bitcast(F32R), in_=banks[5][:, 0:256].rearrange("p (c t) -> p c t", c=2), func=ACTF.Copy),
                     reads=[bank_reg(5)], writes=[ehT[d_].reg()])
                for nh_ in range(2):
                    for c in range(2):
                        S.op("pe", lambda e, c=c, nh_=nh_, s_=s_, d_=d_: e.matmul(banks[6 + nh_][:], lhsT=ehT[d_].ap[:, c, :].bitcast(F32R),
                                                                               rhs=wdn[s_].ap[:, c, nh_ * 512:(nh_ + 1) * 512].bitcast(F32R), start=(c == 0), stop=(c == 1)),
                             reads=[ehT[d_].reg(), wdn[s_].reg()], writes=[bank_reg(6 + nh_)])
                S.op("dve", lambda e, d_=d_: e.tensor_copy(out=ysb[d_].ap[:, 0:512], in_=banks[6][:]), reads=[bank_reg(6)], writes=[ysb[d_].reg(0, 2048)])
                S.op("act", lambda e, d_=d_: e.activation(out=ysb[d_].ap[:, 512:1024], in_=banks[7][:], func=ACTF.Copy), reads=[bank_reg(7)], writes=[ysb[d_].reg(2048, 4096)])
                dma_sp(y_dram[e_ * CAP:(e_ + 1) * CAP, :], ysb[d_].ap, [ysb[d_].reg()], [R("y_dram", e_, e_ + 1)], sem="yst%d" % d_)

            A.top = CKEEP
            wgo = [A.get(f"wgo{i}", 8 * 256 * 4, F32, "p (k n) -> p k n", k=8) for i in range(2)]
            wuo = [A.get(f"wuo{i}", 8 * 256 * 4, F32, "p (k n) -> p k n", k=8) for i in range(2)]
            wdo = [A.get(f"wdo{i}", 2 * 1024 * 4, F32, "p (c n) -> p c n", c=2) for i in range(2)]
            wge_rows = wge_d.rearrange("e (p k) n -> (e p) (k n)", k=8)
            wue_rows = wue_d.rearrange("e (p k) n -> (e p) (k n)", k=8)
            wde_rows = wde_d.rearrange("e (p c) n -> (e p) (c n)", c=2)
            for ob in range(NOV):
                d_ = ob % 2
                s_ = ob % NW
                for (dst, src, nm) in ((wgo[d_], wge_rows, "og"), (wuo[d_], wue_rows, "ou"), (wdo[d_], wde_rows, "od")):
                    S.op("pool", lambda e, dst=dst, src=src, ob=ob: e.indirect_dma_start(
                        out=dst.ap.rearrange("p a b -> p (a b)"), out_offset=None, in_=src[:, :],
                        in_offset=bass.IndirectOffsetOnAxis(ap=widx.ap[:, ob:ob + 1], axis=0), bounds_check=breg(e, NE * 128 - 1), oob_is_err=False),
                        reads=[widx.reg()], writes=[dst.reg()], dsem="%s%d" % (nm, d_))
                dma_sp(lidx[s_].ap[:, 0:2], list_dram[(NE + ob) * CAP:(NE + ob + 1) * CAP, :], [R("list_dram", 0, 1000)], [lidx[s_].reg()], sem="li%d" % s_)
                S.op("pool", lambda e, s_=s_: e.indirect_dma_start(out=xe[s_].ap, out_offset=None, in_=h2_dram[:, :],
                                                                   in_offset=bass.IndirectOffsetOnAxis(ap=lidx[s_].ap[:, 0:1], axis=0),
                                                                   bounds_check=breg(e, 2048), oob_is_err=False),
                     reads=[lidx[s_].reg(), R("h2_dram", 0, 17)], writes=[xe[s_].reg()], dsem="xg%d" % s_)
                b0, b1 = (0, 1) if d_ == 0 else (2, 3)
                transpose8(xe[s_].ap, xe[s_].reg(), xeT[d_], b0, b1)
                for (wbuf, c0) in ((wgo[d_], 0), (wuo[d_], 256)):
                    for k in range(8):
                        S.op("pe", lambda e, k=k, wbuf=wbuf, c0=c0, d_=d_: e.matmul(banks[4][:, c0:c0 + 256], lhsT=xeT[d_].ap[:, k, :], rhs=wbuf.ap[:, k, :],
                                                                                 start=(k == 0), stop=(k == 7)),
                             reads=[xeT[d_].reg(), wbuf.reg()], writes=[bank_reg(4)])
                S.op("act", lambda e, d_=d_: e.activation(out=es[d_].ap, in_=banks[4][:, 0:256], func=ACTF.Silu), reads=[bank_reg(4)], writes=[es[d_].reg()])
                S.op("dve", lambda e, d_=d_: e.tensor_tensor(out=eh[d_].ap, in0=es[d_].ap, in1=banks[4][:, 256:512], op=ALU.mult),
                     reads=[es[d_].reg(), bank_reg(4)], writes=[eh[d_].reg()])
                ehv = eh[d_].ap.rearrange("t (p c) -> t c p", c=2)
                for c in range(2):
                    S.op("pe", lambda e, c=c, ehv=ehv, d_=d_: e.transpose(banks[5][:, c * 128:(c + 1) * 128], ehv[:, c, :], ident),
                         reads=[eh[d_].reg(), R("consts")], writes=[bank_reg(5, c * 512, c * 512 + 512)])
                S.op("act", lambda e, d_=d_: e.activation(out=ehT[d_].ap.bitcast(F32R), in_=banks[5][:, 0:256].rearrange("p (c t) -> p c t", c=2), func=ACTF.Copy),
                     reads=[bank_reg(5)], writes=[ehT[d_].reg()])
                for nh_ in range(2):
                    for c in range(2):
                        S.op("pe", lambda e, c=c, nh_=nh_, d_=d_: e.matmul(banks[6 + nh_][:], lhsT=ehT[d_].ap[:, c, :], rhs=wdo[d_].ap[:, c, nh_ * 512:(nh_ + 1) * 512],
                                                                        start=(c == 0), stop=(c == 1)),
                             reads=[ehT[d_].reg(), wdo[d_].reg()], writes=[bank_reg(6 + nh_)])
                S.op("dve", lambda e, d_=d_: e.tensor_copy(out=ysb[d_].ap[:, 0:512], in_=banks[6][:]), reads=[bank_reg(6)], writes=[ysb[d_].reg(0, 2048)])
                S.op("act", lambda e, d_=d_: e.activation(out=ysb[d_].ap[:, 512:1024], in_=banks[7][:], func=ACTF.Copy), reads=[bank_reg(7)], writes=[ysb[d_].reg(2048, 4096)])
                dma_sp(y_dram[(NE + ob) * CAP:(NE + ob + 1) * CAP, :], ysb[d_].ap, [ysb[d_].reg()], [R("y_dram", NE + ob, NE + ob + 1)], sem="yst%d" % d_)

            checkpoint(6)
            A.top = CKEEP
            yg = [A.get(f"yg{i}", 4096, F32) for i in range(4)]
            acc = [A.get(f"acc{i}", 4096, F32) for i in range(2)]
            bs = [A.get(f"bs{i}", 4096, F32) for i in range(2)]
            gi_ = 0
            last_tok = None
            for j in range(NT):
                tsl = slice(j * 128, (j + 1) * 128)
                ac = acc[j % 2]
                bsb = bs[j % 2]
                dma_sp(bsb.ap, base_dram[tsl, :], [R("base_dram", j, j + 1)], [bsb.reg()], sem="bs%d" % (j % 2))
                for k in range(8):
                    g = yg[gi_ % 4]
                    gs_ = gi_ % 4
                    gi_ += 1
                    S.op("pool", lambda e, g=g: e.memset(g.ap, 0.0), writes=[g.reg()])
                    S.op("pool", lambda e, g=g, j=j, k=k: e.indirect_dma_start(out=g.ap, out_offset=None, in_=y_dram[:, :],
                                                                               in_offset=bass.IndirectOffsetOnAxis(ap=off8.ap[:, j, k:k + 1], axis=0),
                                                                               bounds_check=breg(e, (NE + NOV) * CAP - 1), oob_is_err=False),
                         reads=[off8.reg(j * 32, j * 32 + 32), R("y_dram", 0, 1000)], writes=[g.reg()], dsem="yg%d" % gs_)
                    if k == 0:
                        S.op("dve", lambda e, g=g, ac=ac, j=j, k=k: e.tensor_scalar(out=ac.ap, in0=g.ap, scalar1=gate8.ap[:, j, k:k + 1], scalar2=None, op0=ALU.mult),
                             reads=[g.reg(), gate8.reg()], writes=[ac.reg()])
                    else:
                        S.op("dve", lambda e, g=g, ac=ac, j=j, k=k: e.scalar_tensor_tensor(out=ac.ap, in0=g.ap, scalar=gate8.ap[:, j, k:k + 1], in1=ac.ap, op0=ALU.mult, op1=ALU.add),
                             reads=[g.reg(), gate8.reg(), ac.reg()], writes=[ac.reg()])
                S.op("dve", lambda e, ac=ac: e.tensor_tensor(out=ac.ap, in0=ac.ap, in1=modsC.ap[:, 2048:3072], op=ALU.mult), reads=[ac.reg(), modsC.reg()], writes=[ac.reg()])
                S.op("dve", lambda e, ac=ac, bsb=bsb: e.tensor_tensor(out=ac.ap, in0=ac.ap, in1=bsb.ap, op=ALU.add), reads=[ac.reg(), bsb.reg()], writes=[ac.reg()])
                last_tok = dma_sp(out_d[tsl, :], ac.ap, [ac.reg()], [R("out_d", j, j + 1)], sem="ost")
        except _Stop:
            pass
        S.wait_all("sp", [(k, v) for k, v in S.cnt.items() if k not in COMPUTE])
        S.emit()
    return nc


_NC_CACHE = {}


def make_in_maps(inputs, ncores=8):
    f = lambda a: np.ascontiguousarray(np.asarray(a))
    x = f(inputs["x"]); c = f(inputs["c"]); pos = f(inputs["positions"])
    shared = dict(
        w_ada=f(inputs["w_ada"][0]), b_ada=f(inputs["b_ada"][0]).reshape(1, 6144),
        g_mix=f(inputs["g_norm_mix"][0]).reshape(1, 1024), g_ffn=f(inputs["g_norm_ffn"][0]).reshape(1, 1024),
        w_in=f(inputs["w_in"][0]),
        gqk=np.concatenate([f(inputs["g_q_a"][0]), f(inputs["g_k_a"][0]), f(inputs["g_q_b"][0]), f(inputs["g_k_b"][0])]).reshape(1, 256),
        sinks=f(inputs["sinks_a"][0]).reshape(1, 8),
        g_out=np.concatenate([f(inputs["g_out_a"][0]), f(inputs["g_out_b"][0])]).reshape(1, 1024),
        w_out=f(inputs["w_out"][0]), w_router=f(inputs["w_router"][0]), rbias=f(inputs["router_bias"][0]).reshape(1, 256),
        w_gate_e=f(inputs["w_gate_e"][0]), w_up_e=f(inputs["w_up_e"][0]), w_down_e=f(inputs["w_down_e"][0]),
        w_gate_s=f(inputs["w_gate_s"][0]), w_up_s=f(inputs["w_up_s"][0]), w_down_s=f(inputs["w_down_s"][0]),
        consts=make_consts(),
    )
    maps = []
    for b in range(ncores):
        m = dict(shared)
        m["x"] = x[b]
        m["cT"] = np.ascontiguousarray(c[b].reshape(128, 8))
        m["posT"] = np.ascontiguousarray(pos[b].reshape(16, 128).T.astype(np.int32))
        maps.append(m)
    return maps


def kernel(**inputs):
    if "nc" not in _NC_CACHE:
        _NC_CACHE["nc"] = build_nc()
    nc = _NC_CACHE["nc"]
    maps = make_in_maps(inputs, 8)
    res = run_bass_kernel_spmd(nc, maps, core_ids=list(range(8)))
    out = np.stack([np.asarray(r["out"]) for r in res.results], axis=0)
    return out.astype(np.float32)
```

```python
import contextlib
import numpy as np
import concourse.bass as bass
import concourse.mybir as mybir
from concourse.bass_utils import run_bass_kernel_spmd

F32 = mybir.dt.float32
F32R = mybir.dt.float32r
BF16 = mybir.dt.bfloat16
I32 = mybir.dt.int32
U32 = mybir.dt.uint32
ALU = mybir.AluOpType
ACTF = mybir.ActivationFunctionType
AX = mybir.AxisListType

COMPUTE = ("pe", "act", "dve", "pool")
ENGS = ("pe", "act", "dve", "pool", "sp")
NT = 16
CAP = 128
NE = 256
EPS = 1e-6
NCONST = 1048 + 256
NOV = 24


class Sched:
    def __init__(self, nc, stack, same_engine_sync=True):
        self.nc = nc
        self.stack = stack
        self.same = same_engine_sync
        self.ops = {e: [] for e in ENGS}
        self.cnt = {}
        self.sems = {}
        self.recs = {}
        self.bank_last = {}
        self.waited = {e: {} for e in ENGS}
        for e in COMPUTE:
            self._sem(e)

    def _sem(self, key):
        if key not in self.sems:
            self.sems[key] = self.stack.enter_context(self.nc.semaphore("s_" + key))
            self.cnt[key] = 0
        return self.sems[key]

    def _deps(self, reads, writes):
        deps = []
        for (sp, lo, hi) in reads:
            if sp.startswith("bank"):
                continue
            for r in self.recs.get(sp, ()):
                if r[2] == "w" and r[0] < hi and lo < r[1]:
                    deps.append(r[3])
        for (sp, lo, hi) in writes:
            if sp.startswith("bank"):
                continue
            for r in self.recs.get(sp, ()):
                if r[0] < hi and lo < r[1]:
                    deps.append(r[3])
        for (sp, lo, hi) in list(reads) + list(writes):
            if sp.startswith("bank") and sp in self.bank_last:
                deps.append(self.bank_last[sp])
        return deps

    def _record(self, reads, writes, tok):
        for (sp, lo, hi) in list(reads) + list(writes):
            if sp.startswith("bank"):
                self.bank_last[sp] = tok
        for (sp, lo, hi) in writes:
            if sp.startswith("bank"):
                continue
            lst = self.recs.setdefault(sp, [])
            lst[:] = [r for r in lst if not (lo <= r[0] and r[1] <= hi)]
            lst.append([lo, hi, "w", tok])
        for (sp, lo, hi) in reads:
            if sp.startswith("bank"):
                continue
            lst = self.recs.setdefault(sp, [])
            lst[:] = [r for r in lst if not (r[2] == "r" and r[3][0] == tok[0]
                                             and lo <= r[0] and r[1] <= hi)]
            lst.append([lo, hi, "r", tok])

    def op(self, eng, emit, reads=(), writes=(), dsem=None):
        deps = self._deps(reads, writes)
        if dsem is None:
            key, amt = eng, 1
        else:
            key, amt = dsem, 16
            self._sem(key)
        waits = {}
        for (k, v) in deps:
            if k == eng and dsem is None and (eng == "pe" or not self.same):
                continue
            if self.waited[eng].get(k, 0) >= v:
                continue
            waits[k] = max(waits.get(k, 0), v)
        for k, v in waits.items():
            self.waited[eng][k] = v
        self.cnt[key] += amt
        tok = (key, self.cnt[key])
        self.ops[eng].append((sorted(waits.items()), emit, key, amt))
        self._record(reads, writes, tok)
        return tok

    def wait_all(self, eng, toks):
        waits = {}
        for (k, v) in toks:
            waits[k] = max(waits.get(k, 0), v)
        self.ops[eng].append((sorted(waits.items()), None, None, 0))

    def emit(self):
        nc = self.nc
        with nc.Block() as block:
            def run(engname):
                def body(eng):
                    for waits, emit, key, amt in self.ops[engname]:
                        for (k, v) in waits:
                            eng.wait_ge(self.sems[k], v)
                        if emit is not None:
                            ins = emit(eng)
                            ins.then_inc(self.sems[key], amt)
                return body
            block.tensor(run("pe"))
            block.scalar(run("act"))
            block.vector(run("dve"))
            block.gpsimd(run("pool"))
            block.sync(run("sp"))


def R(name, lo=0, hi=1):
    return (name, lo, hi)


def make_consts():
    c = np.zeros((128, NCONST), np.float32)
    p = np.arange(128)
    c[:, 0:128] = np.eye(128, dtype=np.float32)
    c[:, 128:256] = (p[:, None] < p[None, :]).astype(np.float32)
    c[:, 256:384] = 1.0
    c[:, 384:640] = np.arange(256, dtype=np.float32)[None, :]
    c[:, 640:768] = (p[:, None] <= p[None, :]).astype(np.float32)
    c[:, 768:896] = (p[:, None] > p[None, :]).astype(np.float32)
    c[:, 896:1024] = (p[:, None] >= p[None, :]).astype(np.float32)
    inv = (np.float32(500000.0) ** (-np.arange(0, 16, 2, dtype=np.float32) / np.float32(16))).astype(np.float32)
    c[:, 1024:1032] = inv[None, :]
    c[:, 1032:1048] = (128 * np.arange(16)[None, :] + p[:, None]).astype(np.float32)
    c[:, 1048:1176] = p[:, None].astype(np.float32)
    c[:, 1176:1304] = (128 + p[:, None]).astype(np.float32)
    return c


class _Stop(Exception):
    pass


def build_nc(debug=False, stop_at=None):
    nc = bass.Bass("TRN2", target_bir_lowering=False)

    def din(name, shape, dt=F32):
        return nc.dram_tensor(name, shape, dt, kind="ExternalInput").ap()

    x_d = din("x", [2048, 1024])
    cT_d = din("cT", [128, 8])
    pos_d = din("posT", [128, 16], I32)
    w_ada_d = din("w_ada", [1024, 6144])
    b_ada_d = din("b_ada", [1, 6144])
    gmix_d = din("g_mix", [1, 1024])
    gffn_d = din("g_ffn", [1, 1024])
    w_in_d = din("w_in", [1024, 2304])
    gqk_d = din("gqk", [1, 256])
    sinks_d = din("sinks", [1, 8])
    gout_d = din("g_out", [1, 1024])
    w_out_d = din("w_out", [1024, 1024])
    wr_d = din("w_router", [1024, 256])
    rb_d = din("rbias", [1, 256])
    wge_d = din("w_gate_e", [256, 1024, 256])
    wue_d = din("w_up_e", [256, 1024, 256])
    wde_d = din("w_down_e", [256, 256, 1024])
    wgs_d = din("w_gate_s", [1024, 256])
    wus_d = din("w_up_s", [1024, 256])
    wds_d = din("w_down_s", [256, 1024])
    consts_d = din("consts", [128, NCONST])
    out_d = nc.dram_tensor("out", [2048, 1024], F32, kind="ExternalOutput").ap()

    mods_dram = nc.dram_tensor("mods_scr", [128, 6144], F32).ap()
    v_dram = nc.dram_tensor("v_scr", [2048, 520], BF16).ap()
    o4_dram = nc.dram_tensor("o4_scr", [2048, 520], F32).ap()
    o16_dram = nc.dram_tensor("o16_scr", [2048, 520], F32).ap()
    x1_dram = nc.dram_tensor("x1_scr", [2048, 1024], F32).ap()
    base_dram = nc.dram_tensor("base_scr", [2048, 1024], F32).ap()
    h2_dram = nc.dram_tensor("h2_scr", [2049, 1024], F32).ap()
    list_dram = nc.dram_tensor("list_scr", [(NE + NOV) * CAP, 2], I32).ap()
    y_dram = nc.dram_tensor("y_scr", [(NE + NOV) * CAP, 1024], F32).ap()

    dbg = {}
    if debug:
        dbg["x1"] = nc.dram_tensor("dbg_x1", [2048, 1024], F32, kind="ExternalOutput").ap()
        dbg["h2"] = nc.dram_tensor("dbg_h2", [2048, 1024], F32, kind="ExternalOutput").ap()
        dbg["off8"] = nc.dram_tensor("dbg_off8", [128, 128], I32, kind="ExternalOutput").ap()
        dbg["gate8"] = nc.dram_tensor("dbg_gate8", [128, 128], F32, kind="ExternalOutput").ap()
        dbg["base"] = nc.dram_tensor("dbg_base", [2048, 1024], F32, kind="ExternalOutput").ap()
        dbg["mods"] = nc.dram_tensor("dbg_mods", [128, 6144], F32, kind="ExternalOutput").ap()
        dbg["ocat"] = nc.dram_tensor("dbg_ocat", [2048, 1024], F32, kind="ExternalOutput").ap()
        dbg["qk"] = nc.dram_tensor("dbg_qk", [2048, 1664], BF16, kind="ExternalOutput").ap()
        dbg["kTa"] = nc.dram_tensor("dbg_kTa", [128, 2048], BF16, kind="ExternalOutput").ap()
        dbg["kTb"] = nc.dram_tensor("dbg_kTb", [128, 4 * 2048], BF16, kind="ExternalOutput").ap()
        dbg["qTa"] = nc.dram_tensor("dbg_qTa", [128, 4 * 2048], BF16, kind="ExternalOutput").ap()
        dbg["qTb"] = nc.dram_tensor("dbg_qTb", [128, 4 * 2048], BF16, kind="ExternalOutput").ap()
        dbg["v"] = nc.dram_tensor("dbg_v", [2048, 650], BF16, kind="ExternalOutput").ap()

    with contextlib.ExitStack() as st:
        S = Sched(nc, st)
        ARENA_F = 52100
        consts = nc.alloc_sbuf_tensor_at("consts_sb", [128, NCONST], F32, offset=16512)
        banks = [st.enter_context(nc.psum_tensor(f"bank{i}", [128, 512], F32)) for i in range(8)]

        uid = [0]
        ABASE = 21760

        class Buf:
            def __init__(self, name, off_b, nbytes, dt, shape_str=None, **kw):
                self.name = name
                self.off = off_b
                self.nbytes = nbytes
                isz = 2 if dt == BF16 else 4
                uid[0] += 1
                t = nc.alloc_sbuf_tensor_at("%s_%d" % (name, uid[0]), [128, nbytes // isz], dt, offset=ABASE + off_b)
                ap = t[:]
                if shape_str:
                    ap = ap.rearrange(shape_str, **kw)
                self.ap = ap

            def reg(self, lo=None, hi=None):
                if lo is None:
                    return ("arena", self.off, self.off + self.nbytes)
                return ("arena", self.off + lo, self.off + hi)

        class Alloc:
            def __init__(self):
                self.top = 0

            def get(self, name, nbytes, dt=F32, shape_str=None, **kw):
                nbytes = (nbytes + 31) // 32 * 32
                b = Buf(name, self.top, nbytes, dt, shape_str, **kw)
                self.top += nbytes
                assert self.top <= ARENA_F * 4, (name, self.top)
                return b

        def checkpoint(n):
            if stop_at == n:
                raise _Stop()

        last_tok = None
        try:
            A = Alloc()
            ident = consts[:, 0:128]
            tri = consts[:, 128:256]
            ones = consts[:, 256:384]
            iota_e = consts[:, 384:640]
            invf = consts[:, 1024:1032]
            tokf = consts[:, 1032:1048]
            eid0 = consts[:, 1048:1176]
            eid1 = consts[:, 1176:1304]
            ident_b = A.get("ident_b", 256, BF16)
            masks_b = A.get("masks_b", 3 * 256, BF16, "p (m k) -> p m k", m=3)
            maskA2 = A.get("maskA2", 2 * 4 * 256, BF16, "p (j h k) -> p j h k", j=2, h=4)
            maskB4 = A.get("maskB4", 4 * 256, BF16, "p (b k) -> p b k", b=4)
            cosb = A.get("cos", 16 * 8 * 4, F32, "p (j f) -> p j f", j=16)
            sinb = A.get("sin", 16 * 8 * 4, F32, "p (j f) -> p j f", j=16)
            off8 = A.get("off8", 128 * 4, I32, "p (j k) -> p j k", j=16)
            gate8 = A.get("gate8", 128 * 4, F32, "p (j k) -> p j k", j=16)
            tok_i = A.get("tok_i", 32 * 4, I32, "p (j t) -> p j t", t=2)
            small = A.get("small", 64 * 4, F32)
            neghalf = A.get("neghalf", 32 * 4, F32)
            esink = A.get("esink", 8 * 4, F32)
            junk = A.get("junk", 4096, F32)
            PERSIST_TOP = A.top

            ld = [0]

            def _autosem(reads, writes, sem):
                if sem not in (None, "st"):
                    return sem
                regs = reads if sem == "st" else writes
                r0 = regs[0]
                return ("s" if sem == "st" else "l") + "_%s_%s" % (r0[0], r0[1])

            def dma_sp(out, in_, reads, writes, sem=None):
                return S.op("sp", lambda e: e.dma_start(out=out, in_=in_), reads=reads, writes=writes, dsem=_autosem(reads, writes, sem))

            def dma_pool(out, in_, reads, writes, sem=None):
                return S.op("pool", lambda e: e.dma_start(out=out, in_=in_), reads=reads, writes=writes, dsem=_autosem(reads, writes, sem))

            _regs = {}

            def breg(e, val):
                if val not in _regs:
                    _regs[val] = e.to_reg(val)
                return _regs[val]

            def bank_reg(i, lo=0, hi=2048):
                return ("bank%d" % i, lo, hi)

            dma_sp(consts[:], consts_d, [], [R("consts")])
            S.op("dve", lambda e: e.tensor_copy(out=ident_b.ap, in_=ident), reads=[R("consts")], writes=[ident_b.reg()])
            for m in range(3):
                S.op("dve", lambda e, m=m: e.tensor_copy(out=masks_b.ap[:, m, :], in_=consts[:, 640 + 128 * m:768 + 128 * m]),
                     reads=[R("consts")], writes=[masks_b.reg()])
            for h in range(4):
                S.op("dve", lambda e, h=h: e.tensor_copy(out=maskA2.ap[:, 0, h, :], in_=consts[:, 768:896]),
                     reads=[R("consts")], writes=[maskA2.reg()])
                S.op("dve", lambda e, h=h: e.tensor_copy(out=maskA2.ap[:, 1, h, :], in_=consts[:, 640:768]),
                     reads=[R("consts")], writes=[maskA2.reg()])
                S.op("dve", lambda e, h=h: e.tensor_copy(out=maskB4.ap[:, h, :], in_=(consts[:, 896:1024] if h % 2 == 0 else consts[:, 640:768])),
                     reads=[R("consts")], writes=[maskB4.reg()])
            S.op("pool", lambda e: e.memset(neghalf.ap, -0.5), writes=[neghalf.reg()])
            S.op("dve", lambda e: e.tensor_copy(out=tok_i.ap, in_=tokf.unsqueeze(2).to_broadcast([128, 16, 2])), reads=[R("consts")], writes=[tok_i.reg()])

            TWO_PI = float(2 * np.pi)
            pos_i = A.get("pos_i", 64, I32)
            pos_f = A.get("pos_f", 64, F32)
            ang = A.get("ang", 512, F32, "p (j f) -> p j f", j=16)
            nrot = A.get("nrot", 512, F32, "p (j f) -> p j f", j=16)
            nrot_i = A.get("nrot_i", 512, I32, "p (j f) -> p j f", j=16)
            tmp_r = A.get("tmp_r", 512, F32, "p (j f) -> p j f", j=16)
            dma_sp(pos_i.ap, pos_d, [], [pos_i.reg()])
            S.op("dve", lambda e: e.tensor_copy(out=pos_f.ap, in_=pos_i.ap), reads=[pos_i.reg()], writes=[pos_f.reg()])
            S.op("dve", lambda e: e.tensor_tensor(out=ang.ap, in0=pos_f.ap.unsqueeze(2).to_broadcast([128, 16, 8]),
                                                  in1=invf.unsqueeze(1).to_broadcast([128, 16, 8]), op=ALU.mult),
                 reads=[pos_f.reg(), R("consts")], writes=[ang.reg()])
            S.op("dve", lambda e: e.tensor_scalar(out=nrot.ap, in0=ang.ap, scalar1=1.0 / TWO_PI, scalar2=None, op0=ALU.mult),
                 reads=[ang.reg()], writes=[nrot.reg()])
            S.op("dve", lambda e: e.tensor_copy(out=nrot_i.ap, in_=nrot.ap), reads=[nrot.reg()], writes=[nrot_i.reg()])
            S.op("dve", lambda e: e.tensor_copy(out=nrot.ap, in_=nrot_i.ap), reads=[nrot_i.reg()], writes=[nrot.reg()])
            S.op("dve", lambda e: e.scalar_tensor_tensor(out=ang.ap, in0=nrot.ap, scalar=-6.28125, in1=ang.ap, op0=ALU.mult, op1=ALU.add),
                 reads=[nrot.reg(), ang.reg()], writes=[ang.reg()])
            S.op("dve", lambda e: e.scalar_tensor_tensor(out=ang.ap, in0=nrot.ap, scalar=-(TWO_PI - 6.28125), in1=ang.ap, op0=ALU.mult, op1=ALU.add),
                 reads=[nrot.reg(), ang.reg()], writes=[ang.reg()])
            PI = float(np.pi)
            S.op("dve", lambda e: e.tensor_scalar(out=tmp_r.ap, in0=ang.ap, scalar1=PI, scalar2=-TWO_PI, op0=ALU.is_gt, op1=ALU.mult),
                 reads=[ang.reg()], writes=[tmp_r.reg()])
            S.op("dve", lambda e: e.tensor_tensor(out=ang.ap, in0=ang.ap, in1=tmp_r.ap, op=ALU.add),
                 reads=[ang.reg(), tmp_r.reg()], writes=[ang.reg()])
            S.op("dve", lambda e: e.tensor_scalar(out=tmp_r.ap, in0=ang.ap, scalar1=-PI, scalar2=TWO_PI, op0=ALU.is_lt, op1=ALU.mult),
                 reads=[ang.reg()], writes=[tmp_r.reg()])
            S.op("dve", lambda e: e.tensor_tensor(out=ang.ap, in0=ang.ap, in1=tmp_r.ap, op=ALU.add),
                 reads=[ang.reg(), tmp_r.reg()], writes=[ang.reg()])
            S.op("act", lambda e: e.activation(out=sinb.ap, in_=ang.ap, func=ACTF.Sin), reads=[ang.reg()], writes=[sinb.reg()])
            S.op("dve", lambda e: e.tensor_scalar(out=tmp_r.ap, in0=ang.ap, scalar1=-1.0, scalar2=None, op0=ALU.mult),
                 reads=[ang.reg()], writes=[tmp_r.reg()])
            S.op("dve", lambda e: e.tensor_tensor(out=tmp_r.ap, in0=tmp_r.ap, in1=ang.ap, op=ALU.max),
                 reads=[ang.reg(), tmp_r.reg()], writes=[tmp_r.reg()])
            S.op("dve", lambda e: e.tensor_scalar(out=tmp_r.ap, in0=tmp_r.ap, scalar1=-1.0, scalar2=PI / 2, op0=ALU.mult, op1=ALU.add),
                 reads=[tmp_r.reg()], writes=[tmp_r.reg()])
            S.op("act", lambda e: e.activation(out=cosb.ap, in_=tmp_r.ap, func=ACTF.Sin), reads=[tmp_r.reg()], writes=[cosb.reg()])
            A.top = PERSIST_TOP

            wada = [A.get(f"wada{i}", 8 * 512 * 4, F32, "p (k n) -> p k n", k=8) for i in range(2)]
            csb = A.get("csb", 8 * 128 * 4, F32, "p (k m) -> p k m", k=8)
            cs = A.get("cs", 32, F32)
            bbc = A.get("bbc", 6144 * 4, F32)
            gmix = A.get("gmix", 4096, F32)
            gffn = A.get("gffn", 4096, F32)
            mods = A.get("mods", 6144 * 4, F32)
            dma_sp(cs.ap, cT_d, [], [cs.reg()])
            dma_pool(bbc.ap, b_ada_d.partition_broadcast(128), [], [bbc.reg()])
            dma_pool(gmix.ap, gmix_d.partition_broadcast(128), [], [gmix.reg()])
            dma_pool(gffn.ap, gffn_d.partition_broadcast(128), [], [gffn.reg()])
            S.op("act", lambda e: e.activation(out=cs.ap, in_=cs.ap, func=ACTF.Silu), reads=[cs.reg()], writes=[cs.reg()])
            S.op("dve", lambda e: e.tensor_copy(out=csb.ap.bitcast(F32R), in_=cs.ap.unsqueeze(2).to_broadcast([128, 8, 128])),
                 reads=[cs.reg()], writes=[csb.reg()])
            wada_v = w_ada_d.rearrange("(p k) n -> p k n", k=8)
            for c in range(12):
                wb = wada[c % 2]
                dma_pool(wb.ap.bitcast(F32R), wada_v[:, :, c * 512:(c + 1) * 512], [], [wb.reg()], sem="wada%d" % (c % 2))
                bk = c % 2
                for k in range(8):
                    S.op("pe", lambda e, k=k, wb=wb, bk=bk: e.matmul(banks[bk][:], lhsT=csb.ap[:, k, :].bitcast(F32R),
                                                                   rhs=wb.ap[:, k, :].bitcast(F32R), start=(k == 0), stop=(k == 7)),
                         reads=[csb.reg(), wb.reg()], writes=[bank_reg(bk)])
                mi = c // 2
                cols = slice(c * 512, (c + 1) * 512)
                S.op("dve", lambda e, bk=bk, cols=cols: e.tensor_tensor(out=mods.ap[:, cols], in0=banks[bk][:], in1=bbc.ap[:, cols], op=ALU.add),
                     reads=[bank_reg(bk), bbc.reg()], writes=[mods.reg(c * 2048, (c + 1) * 2048)])
                if mi in (1, 4):
                    g = gmix if mi == 1 else gffn
                    gc = slice((c % 2) * 512, (c % 2) * 512 + 512)
                    S.op("dve", lambda e, cols=cols, g=g, gc=gc: e.scalar_tensor_tensor(out=mods.ap[:, cols], in0=mods.ap[:, cols], scalar=1.0,
                                                                                      in1=g.ap[:, gc], op0=ALU.add, op1=ALU.mult),
                         reads=[mods.reg(c * 2048, (c + 1) * 2048), g.reg()], writes=[mods.reg(c * 2048, (c + 1) * 2048)])
            dma_sp(mods_dram, mods.ap, [mods.reg()], [R("mods_dram")], sem="st")
            if debug:
                dma_sp(dbg["mods"], mods.ap, [mods.reg()], [R("dbg_mods")], sem="st")
            A.top = PERSIST_TOP

            checkpoint(1)
            modsB = A.get("modsB", 2048 * 4, F32)
            gqk = A.get("gqk", 26 * 64 * 4, F32, "p (h d) -> p h d", h=26)
            qTa = A.get("qTa", 4 * 2048 * 2, BF16, "p (i t) -> p i t", i=4)
            kTa = A.get("kTa", 2048 * 2, BF16)
            qTb = A.get("qTb", 4 * 2048 * 2, BF16, "p (i t) -> p i t", i=4)
            kTb = A.get("kTb", 4 * 2048 * 2, BF16, "p (i t) -> p i t", i=4)
            vnat = A.get("vnat", 16 * 650 * 2, BF16, "p (j h d) -> p j h d", j=16, h=10)
            BWIDE_TOP = A.top
            dma_sp(modsB.ap, mods_dram[:, 0:2048], [R("mods_dram")], [modsB.reg()])
            gq_tmp = A.get("gq_tmp", 1024, F32)
            dma_pool(gq_tmp.ap, gqk_d.partition_broadcast(128), [], [gq_tmp.reg()])
            for h in range(26):
                if h < 8:
                    src, sc = 0, 0.125
                elif h < 10:
                    src, sc = 1, 1.0
                elif h < 18:
                    src, sc = 2, 0.125
                else:
                    src, sc = 3, 1.0
                S.op("dve", lambda e, h=h, src=src, sc=sc: e.tensor_scalar(out=gqk.ap[:, h, :], in0=gq_tmp.ap[:, src * 64:src * 64 + 64],
                                                                            scalar1=sc, scalar2=None, op0=ALU.mult),
                     reads=[gq_tmp.reg()], writes=[gqk.reg()])
            S.op("pool", lambda e: e.memset(vnat.ap, 1.0), writes=[vnat.reg()])

            w_in = A.get("w_in", 8 * 2304 * 4, F32, "p (k n) -> p k n", k=8)
            xt = [A.get(f"xt{i}", 4096, F32) for i in range(1)]
            hbuf = A.get("h", 4096, F32)
            hT = [A.get(f"hT{i}", 4096, F32, "p (k t) -> p k t", k=8) for i in range(1)]
            sq = [A.get(f"sq{i}", 2048, F32) for i in range(2)]
            qkf = [A.get(f"qkf{i}", 2048, F32) for i in range(2)]
            qkb = A.get("qkb", 1664 * 2, BF16)
            rope_t = A.get("rope_t", 4 * 8 * 8 * 4, F32, "p (a h f) -> p a h f", a=4, h=8)
            st_small = A.get("st_small", 64 * 4, F32)
            dma_pool(w_in.ap.bitcast(F32R), w_in_d.rearrange("(p k) n -> p k n", k=8), [], [w_in.reg()], sem="win")

            def rmsnorm_mod(xin, xin_reg, out_ap, out_reg, a_ap, s_ap, mod_reg, scr, tagsem):
                ss = scr.ap[:, 0:1]
                S.op("act", lambda e: e.activation(out=junk.ap,
                                                   in_=xin, func=ACTF.Square, accum_out=ss),
                     reads=[xin_reg], writes=[junk.reg(), scr.reg()])
                S.op("dve", lambda e: e.tensor_scalar(out=scr.ap[:, 1:2], in0=ss, scalar1=1.0 / 1024, scalar2=EPS, op0=ALU.mult, op1=ALU.add),
                     reads=[scr.reg()], writes=[scr.reg()])
                S.op("pool", lambda e: e.tensor_tensor(out=scr.ap[:, 2:3], in0=scr.ap[:, 1:2], in1=neghalf.ap[:, 0:1], op=ALU.pow),
                     reads=[scr.reg(), neghalf.reg()], writes=[scr.reg()])
                S.op("dve", lambda e: e.scalar_tensor_tensor(out=out_ap, in0=xin, scalar=scr.ap[:, 2:3], in1=a_ap, op0=ALU.mult, op1=ALU.mult),
                     reads=[xin_reg, scr.reg(), mod_reg], writes=[out_reg])
                S.op("dve", lambda e: e.tensor_tensor(out=out_ap, in0=out_ap, in1=s_ap, op=ALU.add),
                     reads=[out_reg, mod_reg], writes=[out_reg])

            def transpose8(src_ap, src_reg, dst, bk0=0, bk1=1):
                v = src_ap.rearrange("t (p k) -> t k p", k=8)
                for k in range(8):
                    bk = bk0 if k < 4 else bk1
                    S.op("pe", lambda e, k=k, bk=bk: e.transpose(banks[bk][:, (k % 4) * 128:(k % 4) * 128 + 128], v[:, k, :], ident),
                         reads=[src_reg, R("consts")], writes=[bank_reg(bk, (k % 4) * 512, (k % 4) * 512 + 512)])
                S.op("act", lambda e: e.activation(out=dst.ap[:, 0:4, :].bitcast(F32R), in_=banks[bk0][:].rearrange("p (k t) -> p k t", k=4), func=ACTF.Copy),
                     reads=[bank_reg(bk0)], writes=[dst.reg(0, 2048)])
                S.op("act", lambda e: e.activation(out=dst.ap[:, 4:8, :].bitcast(F32R), in_=banks[bk1][:].rearrange("p (k t) -> p k t", k=4), func=ACTF.Copy),
                     reads=[bank_reg(bk1)], writes=[dst.reg(2048, 4096)])

            chunks = [(0, 512, 8, 0), (512, 256, 2, 8), (768, 512, 8, 10), (1280, 512, 8, 18), (1792, 512, 0, 0)]
            for j in range(NT):
                xb = xt[0]
                hTb = hT[0]
                dma_sp(xb.ap, x_d[j * 128:(j + 1) * 128, :], [], [xb.reg()], sem="x0")
                rmsnorm_mod(xb.ap, xb.reg(), hbuf.ap, hbuf.reg(), modsB.ap[:, 1024:2048], modsB.ap[:, 0:1024], modsB.reg(), st_small, "b1")
                transpose8(hbuf.ap, hbuf.reg(), hTb)
                for ci, (c0, ncol, nh, h0) in enumerate(chunks):
                    bk = 2 + ci
                    for k in range(8):
                        S.op("pe", lambda e, k=k, bk=bk, c0=c0, ncol=ncol, hTb=hTb: e.matmul(
                            banks[bk][:, 0:ncol], lhsT=hTb.ap[:, k, :].bitcast(F32R), rhs=w_in.ap[:, k, c0:c0 + ncol].bitcast(F32R),
                            start=(k == 0), stop=(k == 7)),
                            reads=[hTb.reg(), w_in.reg()], writes=[bank_reg(bk)])
                for ci, (c0, ncol, nh, h0) in enumerate(chunks):
                    bk = 2 + ci
                    if nh > 0:
                        sqb = sq[ci % 2]
                        qf = qkf[ci % 2]
                        nq = nh * 64
                        S.op("act", lambda e, bk=bk, sqb=sqb, nq=nq: e.activation(out=sqb.ap[:, 0:nq], in_=banks[bk][:, 0:nq], func=ACTF.Square),
                             reads=[bank_reg(bk)], writes=[sqb.reg()])
                        ssum = st_small.ap[:, 8:8 + nh]
                        S.op("dve", lambda e, sqb=sqb, nh=nh, nq=nq, ssum=ssum: e.tensor_reduce(out=ssum, in_=sqb.ap[:, 0:nq].rearrange("p (h d) -> p h d", h=nh),
                                                                                             axis=AX.X, op=ALU.add),
                             reads=[sqb.reg()], writes=[st_small.reg()])
                        S.op("dve", lambda e, ssum=ssum: e.tensor_scalar(out=ssum, in0=ssum, scalar1=1.0 / 64, scalar2=EPS, op0=ALU.mult, op1=ALU.add),
                             reads=[st_small.reg()], writes=[st_small.reg()])
                        rstd = st_small.ap[:, 16:16 + nh]
                        S.op("pool", lambda e, ssum=ssum, rstd=rstd, nh=nh: e.tensor_tensor(out=rstd, in0=ssum, in1=neghalf.ap[:, 0:nh], op=ALU.pow),
                             reads=[st_small.reg(), neghalf.reg()], writes=[st_small.reg()])
                        qv = qf.ap[:, 0:nq].rearrange("p (h d) -> p h d", h=nh)
                        S.op("dve", lambda e, bk=bk, qv=qv, rstd=rstd, nh=nh, nq=nq: e.tensor_tensor(
                            out=qv, in0=banks[bk][:, 0:nq].rearrange("p (h d) -> p h d", h=nh),
                            in1=rstd.unsqueeze(2).to_broadcast([128, nh, 64]), op=ALU.mult),
                            reads=[bank_reg(bk), st_small.reg()], writes=[qf.reg()])
                        S.op("dve", lambda e, qv=qv, h0=h0, nh=nh: e.tensor_tensor(out=qv, in0=qv, in1=gqk.ap[:, h0:h0 + nh, :], op=ALU.mult),
                             reads=[qf.reg(), gqk.reg()], writes=[qf.reg()])
                        cb = cosb.ap[:, j, :].unsqueeze(1).to_broadcast([128, nh, 8])
                        sb_ = sinb.ap[:, j, :].unsqueeze(1).to_broadcast([128, nh, 8])
                        t1 = qv[:, :, 0:8]
                        t2 = qv[:, :, 8:16]
                        ra, rb_, rc, rd = (rope_t.ap[:, a, 0:nh, :] for a in range(4))
                        for (o_, i0, i1) in ((ra, t1, cb), (rb_, t2, sb_), (rc, t2, cb), (rd, t1, sb_)):
                            S.op("dve", lambda e, o_=o_, i0=i0, i1=i1: e.tensor_tensor(out=o_, in0=i0, in1=i1, op=ALU.mult),
                                 reads=[qf.reg(), cosb.reg(), sinb.reg()], writes=[rope_t.reg()])
                        S.op("dve", lambda e, t1=t1, ra=ra, rb_=rb_: e.tensor_tensor(out=t1, in0=ra, in1=rb_, op=ALU.subtract),
                             reads=[rope_t.reg()], writes=[qf.reg()])
                        S.op("dve", lambda e, t2=t2, rc=rc, rd=rd: e.tensor_tensor(out=t2, in0=rc, in1=rd, op=ALU.add),
                             reads=[rope_t.reg()], writes=[qf.reg()])
                        qoff = {0: 0, 1: 512, 2: 640, 3: 1152}[ci]
                        if ci == 0:
                            S.op("act", lambda e, qf=qf: e.activation(out=qkb.ap[:, 0:512].rearrange("p (i g d) -> p g i d", i=4, g=2),
                                                                      in_=qf.ap[:, 0:512].rearrange("p (g i d) -> p g i d", g=2, i=4), func=ACTF.Copy),
                                 reads=[qf.reg()], writes=[qkb.reg(0, 1024)])
                        else:
                            S.op("act", lambda e, qf=qf, nq=nq, qoff=qoff: e.activation(out=qkb.ap[:, qoff:qoff + nq], in_=qf.ap[:, 0:nq], func=ACTF.Copy),
                                 reads=[qf.reg()], writes=[qkb.reg(qoff * 2, (qoff + nq) * 2)])
                    if ci == 1:
                        S.op("act", lambda e, bk=bk, j=j: e.activation(out=vnat.ap[:, j, 0:2, 0:64], in_=banks[bk][:, 128:256].rearrange("p (h d) -> p h d", h=2), func=ACTF.Copy),
                             reads=[bank_reg(bk)], writes=[vnat.reg(j * 1300, j * 1300 + 260)])
                    if ci == 4:
                        S.op("act", lambda e, bk=bk, j=j: e.activation(out=vnat.ap[:, j, 2:10, 0:64], in_=banks[bk][:, 0:512].rearrange("p (h d) -> p h d", h=8), func=ACTF.Copy),
                             reads=[bank_reg(bk)], writes=[vnat.reg(j * 1300 + 260, (j + 1) * 1300)])
                        dma_sp(v_dram[j * 128:(j + 1) * 128, :], vnat.ap[:, j, 2:10, :].rearrange("p h d -> p (h d)"),
                               [vnat.reg(j * 1300 + 260, (j + 1) * 1300)], [R("v_dram", j, j + 1)], sem="st_vnat")
                if debug:
                    dma_sp(dbg["qk"][j * 128:(j + 1) * 128, :], qkb.ap, [qkb.reg()], [R("dbg_qk", j, j + 1)], sem="st")
                    dma_sp(dbg["v"][j * 128:(j + 1) * 128, :], vnat.ap[:, j, :, :].rearrange("p h d -> p (h d)"), [vnat.reg(j * 1300, (j + 1) * 1300)], [R("dbg_v", j, j + 1)], sem="st_dbgv")
                pb7 = banks[7][:].bitcast(BF16).rearrange("p (s t) -> p s t", s=8)
                pb0 = banks[0][:].bitcast(BF16).rearrange("p (s t) -> p s t", s=8)
                tsl = slice(j * 128, (j + 1) * 128)
                for i in range(4):
                    S.op("pe", lambda e, i=i: e.transpose(pb7[:, i, :], qkb.ap[:, i * 128:(i + 1) * 128], ident_b.ap),
                         reads=[qkb.reg(0, 1024), ident_b.reg()], writes=[bank_reg(7, i * 256, i * 256 + 256)])
                for i in range(4):
                    S.op("pe", lambda e, i=i: e.transpose(pb7[:, 4 + i, :], qkb.ap[:, 640 + i * 128:640 + i * 128 + 128], ident_b.ap),
                         reads=[qkb.reg(1280, 2304), ident_b.reg()], writes=[bank_reg(7, (4 + i) * 256, (4 + i) * 256 + 256)])
                S.op("act", lambda e, tsl=tsl: e.activation(out=qTa.ap[:, :, tsl], in_=pb7[:, 0:4, :], func=ACTF.Copy),
                     reads=[bank_reg(7)], writes=[qTa.reg()])
                S.op("dve", lambda e, tsl=tsl: e.tensor_copy(out=qTb.ap[:, :, tsl], in_=pb7[:, 4:8, :]),
                     reads=[bank_reg(7)], writes=[qTb.reg()])
                for i in range(4):
                    S.op("pe", lambda e, i=i: e.transpose(pb7[:, i, :], qkb.ap[:, 1152 + i * 128:1152 + i * 128 + 128], ident_b.ap),
                         reads=[qkb.reg(2304, 3328), ident_b.reg()], writes=[bank_reg(7, i * 256, i * 256 + 256)])
                S.op("pe", lambda e: e.transpose(pb7[:, 4, :], qkb.ap[:, 512:640], ident_b.ap),
                     reads=[qkb.reg(1024, 1280), ident_b.reg()], writes=[bank_reg(7, 1024, 1280)])
                S.op("act", lambda e, tsl=tsl: e.activation(out=kTb.ap[:, :, tsl], in_=pb7[:, 0:4, :], func=ACTF.Copy),
                     reads=[bank_reg(7)], writes=[kTb.reg()])
                S.op("dve", lambda e, tsl=tsl: e.tensor_copy(out=kTa.ap[:, tsl], in_=pb7[:, 4, :]),
                     reads=[bank_reg(7)], writes=[kTa.reg()])
            A.top = BWIDE_TOP
            if debug:
                dma_sp(dbg["kTa"], kTa.ap, [kTa.reg()], [R("dbg_kTa")], sem="st_d1")
                dma_sp(dbg["kTb"], kTb.ap.rearrange("p i t -> p (i t)"), [kTb.reg()], [R("dbg_kTb")], sem="st_d2")
                dma_sp(dbg["qTa"], qTa.ap.rearrange("p i t -> p (i t)"), [qTa.reg()], [R("dbg_qTa")], sem="st_d3")
                dma_sp(dbg["qTb"], qTb.ap.rearrange("p i t -> p (i t)"), [qTb.reg()], [R("dbg_qTb")], sem="st_d4")

            checkpoint(2)
            w_out = A.get("w_out", 8 * 1024 * 4, F32, "p (k n) -> p k n", k=8)
            v4 = A.get("v4", 16 * 520 * 2, BF16, "p (j h d) -> p j h d", j=16, h=8)
            v16 = A.get("v16", 16 * 520 * 2, BF16, "p (j h d) -> p j h d", j=16, h=8)
            modg = A.get("modg", 4096, F32)
            goutb = A.get("goutb", 4096, F32)
            pT = [A.get(f"pT{i}", 512 * 2, BF16) for i in range(4)]
            osb = [A.get(f"osb{i}", 520 * 4, F32, "p (h d) -> p h d", h=8) for i in range(2)]
            o4n = A.get("o4n", 520 * 4, F32, "p (h d) -> p h d", h=8)
            o16n = A.get("o16n", 520 * 4, F32, "p (h d) -> p h d", h=8)
            ocat = A.get("ocat", 4096, F32)
            ocT = A.get("ocT", 4096, F32, "p (k t) -> p k t", k=8)
            xb2 = A.get("xb2", 4096, F32)
            x1b = ocat
            att_s = A.get("att_s", 64 * 4, F32)
            dma_pool(w_out.ap.bitcast(F32R), w_out_d.rearrange("(p k) n -> p k n", k=8), [], [w_out.reg()], sem="win")
            dma_sp(modg.ap, mods_dram[:, 2048:3072], [R("mods_dram")], [modg.reg()])
            dma_pool(goutb.ap, gout_d.partition_broadcast(128), [], [goutb.reg()])
            dma_pool(esink.ap, sinks_d.partition_broadcast(128), [], [esink.reg()])
            S.op("act", lambda e: e.activation(out=esink.ap, in_=esink.ap, func=ACTF.Exp), reads=[esink.reg()], writes=[esink.reg()])
            v4d = v_dram.rearrange("(i d) c -> d i c", d=4)
            v16d = v_dram.rearrange("(i d) c -> d i c", d=16)
            for r in range(4):
                for m in range(4):
                    dma_sp(v4.ap[:, r * 4 + m, :, :].rearrange("p h d -> p (h d)"), v4d[r, m * 128:(m + 1) * 128, :],
                           [R("v_dram", 0, 16)], [v4.reg((r * 4 + m) * 1040, (r * 4 + m + 1) * 1040)], sem="l_v4")
            for r in range(16):
                dma_sp(v16.ap[:, r, :, :].rearrange("p h d -> p (h d)"), v16d[r, 0:128, :],
                       [R("v_dram", 0, 16)], [v16.reg(r * 1040, (r + 1) * 1040)], sem="l_v16")

            pcount = [0]

            def attn_b_tile(q_sel, k_sels, vbuf, vidx, out_banks):
                nk = len(k_sels)
                for hp2 in range(2):
                    pts = [pT[(pcount[0]) % 4], pT[(pcount[0] + 1) % 4]]
                    pcount[0] += 2
                    blocks = []
                    for hp in (2 * hp2, 2 * hp2 + 1):
                        for (ks, vt, isprev) in k_sels:
                            blocks.append((hp, ks, vt))
                    nb = len(blocks)
                    for s in range(2):
                        sbk = s
                        pt = pts[s]
                        for bi, (hp, ks, vt) in enumerate(blocks):
                            S.op("pe", lambda e, s=s, ks=ks, hp=hp, bi=bi, sbk=sbk: e.matmul(
                                banks[sbk][:, bi * 128:(bi + 1) * 128], lhsT=kTb.ap[64 * s:64 * s + 64, hp, ks],
                                rhs=qTb.ap[64 * s:64 * s + 64, hp, q_sel], start=True, stop=True),
                                reads=[kTb.reg(), qTb.reg()], writes=[bank_reg(sbk, bi * 512, (bi + 1) * 512)])
                        S.op("act", lambda e, pt=pt, sbk=sbk, nb=nb: e.activation(out=pt.ap[:, 0:nb * 128], in_=banks[sbk][:, 0:nb * 128], func=ACTF.Exp),
                             reads=[bank_reg(sbk, 0, nb * 512)], writes=[pt.reg()])
                        if nk == 2:
                            S.op("dve", lambda e, pt=pt: e.tensor_tensor(out=pt.ap[:, 0:512], in0=pt.ap[:, 0:512],
                                                                        in1=maskB4.ap[:, :, :].rearrange("p b k -> p (b k)"), op=ALU.mult),
                                 reads=[pt.reg(), maskB4.reg()], writes=[pt.reg()])
                        else:
                            S.op("dve", lambda e, pt=pt, nb=nb: e.tensor_tensor(out=pt.ap[:, 0:nb * 128].rearrange("p (b k) -> p b k", b=nb),
                                                                               in0=pt.ap[:, 0:nb * 128].rearrange("p (b k) -> p b k", b=nb),
                                                                               in1=masks_b.ap[:, 0:1, :].to_broadcast([128, nb, 128]), op=ALU.mult),
                                 reads=[pt.reg(), masks_b.reg()], writes=[pt.reg()])
                    for s in range(2):
                        pt = pts[s]
                        for hp in (2 * hp2, 2 * hp2 + 1):
                            hh = 2 * hp + s
                            ob = out_banks[hh // 4]
                            col = (hh % 4) * 65
                            mine = [(bi, b) for bi, b in enumerate(blocks) if b[0] == hp]
                            for n_, (bi, (hp_, ks, vt)) in enumerate(mine):
                                S.op("pe", lambda e, pt=pt, bi=bi, vt=vt, hh=hh, ob=ob, col=col, n_=n_, last=(n_ == len(mine) - 1): e.matmul(
                                    banks[ob][:, col:col + 65], lhsT=pt.ap[:, bi * 128:(bi + 1) * 128], rhs=vbuf.ap[:, vt, vidx + hh, :],
                                    start=(n_ == 0), stop=last),
                                    reads=[pt.reg(), vbuf.reg()], writes=[bank_reg(ob, col * 4, col * 4 + 260)])

            o4d = o4_dram.rearrange("(i d) c -> d i c", d=4)
            o16d = o16_dram.rearrange("(i d) c -> d i c", d=16)
            oi = [0]

            def evac_store(dst_ap, dst_reg):
                ob = osb[oi[0] % 2]
                oi[0] += 1
                S.op("act", lambda e, ob=ob: e.activation(out=ob.ap[:, 0:4, :], in_=banks[2][:, 0:260].rearrange("p (h d) -> p h d", h=4), func=ACTF.Copy),
                     reads=[bank_reg(2)], writes=[ob.reg(0, 1040)])
                S.op("dve", lambda e, ob=ob: e.tensor_copy(out=ob.ap[:, 4:8, :], in_=banks[3][:, 0:260].rearrange("p (h d) -> p h d", h=4)),
                     reads=[bank_reg(3)], writes=[ob.reg(1040, 2080)])
                dma_sp(dst_ap, ob.ap.rearrange("p h d -> p (h d)"), [ob.reg()], [dst_reg], sem="st")

            for r in range(4):
                for m in range(4):
                    def sel(mm, r=r):
                        st_ = r + 512 * mm
                        return slice(st_, st_ + 4 * 127 + 1, 4)
                    ks = []
                    if m > 0:
                        ks.append((sel(m - 1), r * 4 + m - 1, True))
                    ks.append((sel(m), r * 4 + m, False))
                    attn_b_tile(sel(m), ks, v4, 0, (2, 3))
                    evac_store(o4d[r, m * 128:(m + 1) * 128, :], R("o4_dram", r * 4 + m, r * 4 + m + 1))
            for r in range(16):
                sl = slice(r, r + 16 * 127 + 1, 16)
                attn_b_tile(sl, [(sl, r, False)], v16, 0, (2, 3))
                evac_store(o16d[r, 0:128, :], R("o16_dram", r, r + 1))

            checkpoint(3)
            for j in range(NT):
                tsl = slice(j * 128, (j + 1) * 128)
                dma_sp(xb2.ap, x_d[tsl, :], [], [xb2.reg()], sem="x2")
                dma_sp(o4n.ap.rearrange("p h d -> p (h d)"), o4_dram[tsl, :], [R("o4_dram", 0, 16)], [o4n.reg()], sem="on")
                dma_sp(o16n.ap.rearrange("p h d -> p (h d)"), o16_dram[tsl, :], [R("o16_dram", 0, 16)], [o16n.reg()], sem="on")
                for kvh in range(2):
                    jks = ([j - 1] if j > 0 else []) + [j]
                    pa = pT[2 * kvh].ap if False else None
                    pt0 = pT[(pcount[0]) % 4]
                    pt1 = pT[(pcount[0] + 1) % 4]
                    pcount[0] += 2
                    pts = [pt0, pt1]
                    for n_, jk in enumerate(jks):
                        sbk = 4 + n_
                        S.op("pe", lambda e, kvh=kvh, jk=jk, sbk=sbk, tsl=tsl: e.matmul(
                            banks[sbk][:].rearrange("p (i t) -> p i t", i=4), lhsT=kTa.ap[64 * kvh:64 * kvh + 64, jk * 128:(jk + 1) * 128],
                            rhs=qTa.ap[64 * kvh:64 * kvh + 64, :, tsl], start=True, stop=True),
                            reads=[kTa.reg(), qTa.reg()], writes=[bank_reg(sbk)])
                        pt = pts[n_]
                        S.op("act", lambda e, pt=pt, sbk=sbk: e.activation(out=pt.ap, in_=banks[sbk][:], func=ACTF.Exp),
                             reads=[bank_reg(sbk)], writes=[pt.reg()])
                        midx = 1 if jk == j else 0
                        S.op("dve", lambda e, pt=pt, midx=midx: e.tensor_tensor(out=pt.ap, in0=pt.ap, in1=maskA2.ap[:, midx, :, :].rearrange("p h k -> p (h k)"), op=ALU.mult),
                             reads=[pt.reg(), maskA2.reg()], writes=[pt.reg()])
                    ob = 6 + kvh
                    for i in range(4):
                        for n_, jk in enumerate(jks):
                            pt = pts[n_]
                            S.op("pe", lambda e, pt=pt, i=i, jk=jk, kvh=kvh, ob=ob, n_=n_, last=(n_ == len(jks) - 1): e.matmul(
                                banks[ob][:, i * 65:(i + 1) * 65], lhsT=pt.ap[:, i * 128:(i + 1) * 128], rhs=vnat.ap[:, jk, kvh, :],
                                start=(n_ == 0), stop=last),
                                reads=[pt.reg(), vnat.reg()], writes=[bank_reg(ob, i * 260, (i + 1) * 260)])
                ks = ([(slice((j - 1) * 128, j * 128), j - 1, True)] if j > 0 else []) + [(tsl, j, False)]
                if j == 0:
                    attn_b_tile(tsl, ks, vnat, 2, (2, 3))
                else:
                    attn_b_tile(tsl, ks, vnat, 2, (2, 3))
                den = att_s.ap[:, 0:8]
                for kvh in range(2):
                    S.op("dve", lambda e, kvh=kvh: e.tensor_tensor(out=att_s.ap[:, kvh * 4:kvh * 4 + 4], in0=banks[6 + kvh][:, 0:260].rearrange("p (h d) -> p h d", h=4)[:, :, 64],
                                                                  in1=esink.ap[:, kvh * 4:kvh * 4 + 4], op=ALU.add),
                         reads=[bank_reg(6 + kvh), esink.reg()], writes=[att_s.reg()])
                S.op("dve", lambda e: e.reciprocal(out=att_s.ap[:, 8:16], in_=att_s.ap[:, 0:8]), reads=[att_s.reg()], writes=[att_s.reg()])
                for kvh in range(2):
                    S.op("dve", lambda e, kvh=kvh: e.tensor_tensor(out=ocat.ap[:, kvh * 256:(kvh + 1) * 256].rearrange("p (h d) -> p h d", h=4),
                                                                  in0=banks[6 + kvh][:, 0:260].rearrange("p (h d) -> p h d", h=4)[:, :, 0:64],
                                                                  in1=att_s.ap[:, 8 + kvh * 4:12 + kvh * 4].unsqueeze(2).to_broadcast([128, 4, 64]), op=ALU.mult),
                         reads=[bank_reg(6 + kvh), att_s.reg()], writes=[ocat.reg(kvh * 1024, (kvh + 1) * 1024)])
                S.op("dve", lambda e: e.tensor_tensor(out=o4n.ap, in0=o4n.ap, in1=o16n.ap, op=ALU.add), reads=[o4n.reg(), o16n.reg()], writes=[o4n.reg()])
                for half in range(2):
                    S.op("dve", lambda e, half=half: e.tensor_tensor(out=o4n.ap[:, half * 4:half * 4 + 4, :], in0=o4n.ap[:, half * 4:half * 4 + 4, :],
                                                                    in1=banks[2 + half][:, 0:260].rearrange("p (h d) -> p h d", h=4), op=ALU.add),
                         reads=[o4n.reg(), bank_reg(2 + half)], writes=[o4n.reg()])
                S.op("dve", lambda e: e.reciprocal(out=att_s.ap[:, 16:24], in_=o4n.ap[:, :, 64]), reads=[o4n.reg()], writes=[att_s.reg()])
                S.op("dve", lambda e: e.tensor_tensor(out=ocat.ap[:, 512:1024].rearrange("p (h d) -> p h d", h=8), in0=o4n.ap[:, :, 0:64],
                                                      in1=att_s.ap[:, 16:24].unsqueeze(2).to_broadcast([128, 8, 64]), op=ALU.mult),
                     reads=[o4n.reg(), att_s.reg()], writes=[ocat.reg(2048, 4096)])
                for gi in range(2):
                    gsl = slice(gi * 512, (gi + 1) * 512)
                    S.op("act", lambda e, gsl=gsl, gi=gi: e.activation(out=junk.ap[:, 0:512], in_=ocat.ap[:, gsl], func=ACTF.Square, accum_out=att_s.ap[:, 24 + gi:25 + gi]),
                         reads=[ocat.reg(gi * 2048, (gi + 1) * 2048)], writes=[junk.reg(), att_s.reg()])
                S.op("dve", lambda e: e.tensor_scalar(out=att_s.ap[:, 26:28], in0=att_s.ap[:, 24:26], scalar1=1.0 / 512, scalar2=EPS, op0=ALU.mult, op1=ALU.add),
                     reads=[att_s.reg()], writes=[att_s.reg()])
                S.op("pool", lambda e: e.tensor_tensor(out=att_s.ap[:, 28:30], in0=att_s.ap[:, 26:28], in1=neghalf.ap[:, 0:2], op=ALU.pow),
                     reads=[att_s.reg(), neghalf.reg()], writes=[att_s.reg()])
                for gi in range(2):
                    gsl = slice(gi * 512, (gi + 1) * 512)
                    S.op("dve", lambda e, gsl=gsl, gi=gi: e.scalar_tensor_tensor(out=ocat.ap[:, gsl], in0=ocat.ap[:, gsl], scalar=att_s.ap[:, 28 + gi:29 + gi],
                                                                               in1=goutb.ap[:, gsl], op0=ALU.mult, op1=ALU.mult),
                         reads=[ocat.reg(gi * 2048, (gi + 1) * 2048), att_s.reg(), goutb.reg()], writes=[ocat.reg(gi * 2048, (gi + 1) * 2048)])
                if debug:
                    dma_sp(dbg["ocat"][tsl, :], ocat.ap, [ocat.reg()], [R("dbg_ocat", j, j + 1)], sem="st")
                transpose8(ocat.ap, ocat.reg(), ocT, 0, 1)
                for nh_ in range(2):
                    bk = 4 + nh_
                    for k in range(8):
                        S.op("pe", lambda e, k=k, bk=bk, nh_=nh_: e.matmul(banks[bk][:], lhsT=ocT.ap[:, k, :].bitcast(F32R),
                                                                         rhs=w_out.ap[:, k, nh_ * 512:(nh_ + 1) * 512].bitcast(F32R), start=(k == 0), stop=(k == 7)),
                             reads=[ocT.reg(), w_out.reg()], writes=[bank_reg(bk)])
                for nh_ in range(2):
                    csl = slice(nh_ * 512, (nh_ + 1) * 512)
                    S.op("dve", lambda e, nh_=nh_, csl=csl: e.tensor_tensor(out=x1b.ap[:, csl], in0=banks[4 + nh_][:], in1=modg.ap[:, csl], op=ALU.mult),
                         reads=[bank_reg(4 + nh_), modg.reg()], writes=[x1b.reg(nh_ * 2048, (nh_ + 1) * 2048)])
                S.op("dve", lambda e: e.tensor_tensor(out=x1b.ap, in0=x1b.ap, in1=xb2.ap, op=ALU.add), reads=[x1b.reg(), xb2.reg()], writes=[x1b.reg()])
                dma_sp(x1_dram[tsl, :], x1b.ap, [x1b.reg()], [R("x1_dram", j, j + 1)], sem="st")
                if debug:
                    dma_sp(dbg["x1"][tsl, :], x1b.ap, [x1b.reg()], [R("dbg_x1", j, j + 1)], sem="st")
            A.top = PERSIST_TOP

            checkpoint(4)
            modsC = A.get("modsC", 3072 * 4, F32)
            widx = A.get("widx", NOV * 4, I32)
            CKEEP = A.top
            wr = A.get("wr", 8 * 256 * 4, F32, "p (k n) -> p k n", k=8)
            wgus = A.get("wgus", 8 * 512 * 4, F32, "p (k n) -> p k n", k=8)
            wds = A.get("wds", 2 * 1024 * 4, F32, "p (c n) -> p c n", c=2)
            rbias = A.get("rbias", 1024, F32)
            mcum = A.get("mcum", 1024, F32)
            x1t = [A.get(f"x1t{i}", 4096, F32) for i in range(2)]
            h2 = A.get("h2", 4096, F32)
            h2T = A.get("h2T", 4096, F32, "p (k t) -> p k t", k=8)
            sc = A.get("sc", 1024, F32)
            bi_ = A.get("bi", 1024, F32)
            msk = A.get("msk", 1024, F32)
            eqt = A.get("eqt", 1024, F32)
            offm = A.get("offm", 1024, F32)
            rs = A.get("rs", 128 * 4, F32)
            off8f = A.get("off8f", 32, F32)
            sc8 = A.get("sc8", 32, F32)
            mx8 = A.get("mx8", 32, F32)
            idx8 = A.get("idx8", 32, U32)
            idx8f = A.get("idx8f", 32, F32)
            shs = A.get("shs", 1024, F32)
            shh = A.get("shh", 1024, F32)
            shT = A.get("shT", 1024, F32, "p (c t) -> p c t", c=2)
            baseb = A.get("baseb", 4096, F32)
            zrow = A.get("zrow", 4096, F32)
            listinit = A.get("listinit", (NE + NOV) * 2 * 4, I32)
            posall = A.get("posall", 16 * 256 * 4, F32, "p (j e) -> p j e", j=16)
            idxall = A.get("idxall", 16 * 8 * 4, F32, "p (j k) -> p j k", j=16)
            ovT = A.get("ovT", 32, F32)
            rankT = A.get("rankT", 32, F32)
            diagR = A.get("diagR", 512, F32)
            ovb = A.get("ovb", 1024, F32)
            diffr = A.get("diffr", 1024, F32)
            selT = A.get("selT", 2 * NOV * 4, F32, "p (c o) -> p c o", c=2)
            widxf = A.get("widxf", NOV * 4, F32)
            CWIDE = A.top
            dma_sp(modsC.ap, mods_dram[:, 3072:6144], [R("mods_dram")], [modsC.reg()])
            dma_sp(wr.ap, wr_d.rearrange("(p k) n -> p k n", k=8), [], [wr.reg()])
            dma_pool(wgus.ap[:, :, 0:256].bitcast(F32R), wgs_d.rearrange("(p k) n -> p k n", k=8), [], [wgus.reg()], sem="win")
            dma_pool(wgus.ap[:, :, 256:512].bitcast(F32R), wus_d.rearrange("(p k) n -> p k n", k=8), [], [wgus.reg()], sem="win")
            dma_pool(wds.ap.bitcast(F32R), wds_d.rearrange("(p c) n -> p c n", c=2), [], [wds.reg()], sem="win")
            dma_pool(rbias.ap, rb_d.partition_broadcast(128), [], [rbias.reg()])
            S.op("pool", lambda e: e.memset(mcum.ap, 0.0), writes=[mcum.reg()])
            S.op("pool", lambda e: e.memset(zrow.ap, 0.0), writes=[zrow.reg()])
            dma_sp(h2_dram[2048:2049, :], zrow.ap[0:1, :], [zrow.reg()], [R("h2_dram", 16, 17)], sem="st")
            S.op("pool", lambda e: e.memset(listinit.ap, 2048), writes=[listinit.reg()])
            dma_sp(list_dram.rearrange("(p n) o -> p (n o)", p=128), listinit.ap[:, 0:(NE + NOV) * 2], [listinit.reg()], [R("list_dram", 0, 1000)], sem="st_listinit")

            NGRP = 8
            for j in range(NT):
                tsl = slice(j * 128, (j + 1) * 128)
                xb = x1t[j % 2]
                dma_sp(xb.ap, x1_dram[tsl, :], [R("x1_dram", j, j + 1)], [xb.reg()], sem="x1%d" % (j % 2))
                rmsnorm_mod(xb.ap, xb.reg(), h2.ap, h2.reg(), modsC.ap[:, 1024:2048], modsC.ap[:, 0:1024], modsC.reg(), rs, "c1")
                dma_sp(h2_dram[tsl, :], h2.ap, [h2.reg()], [R("h2_dram", j, j + 1)], sem="st")
                if debug:
                    dma_sp(dbg["h2"][tsl, :], h2.ap, [h2.reg()], [R("dbg_h2", j, j + 1)], sem="st")
                transpose8(h2.ap, h2.reg(), h2T, 0, 1)
                for k in range(8):
                    S.op("pe", lambda e, k=k: e.matmul(banks[2][:, 0:256], lhsT=h2T.ap[:, k, :], rhs=wr.ap[:, k, :], start=(k == 0), stop=(k == 7)),
                         reads=[h2T.reg(), wr.reg()], writes=[bank_reg(2)])
                for k in range(8):
                    S.op("pe", lambda e, k=k: e.matmul(banks[4][:], lhsT=h2T.ap[:, k, :].bitcast(F32R), rhs=wgus.ap[:, k, :].bitcast(F32R), start=(k == 0), stop=(k == 7)),
                         reads=[h2T.reg(), wgus.reg()], writes=[bank_reg(4)])
                S.op("act", lambda e: e.activation(out=sc.ap, in_=banks[2][:, 0:256], func=ACTF.Sigmoid), reads=[bank_reg(2)], writes=[sc.reg()])
                S.op("act", lambda e: e.activation(out=shs.ap, in_=banks[4][:, 0:256], func=ACTF.Silu), reads=[bank_reg(4)], writes=[shs.reg()])
                S.op("dve", lambda e: e.tensor_tensor(out=shh.ap, in0=shs.ap, in1=banks[4][:, 256:512], op=ALU.mult), reads=[shs.reg(), bank_reg(4)], writes=[shh.reg()])
                shv = shh.ap.rearrange("t (p c) -> t c p", c=2)
                for c in range(2):
                    S.op("pe", lambda e, c=c: e.transpose(banks[5][:, c * 128:(c + 1) * 128], shv[:, c, :], ident),
                         reads=[shh.reg(), R("consts")], writes=[bank_reg(5, c * 512, c * 512 + 512)])
                S.op("act", lambda e: e.activation(out=shT.ap.bitcast(F32R), in_=banks[5][:, 0:256].rearrange("p (c t) -> p c t", c=2), func=ACTF.Copy),
                     reads=[bank_reg(5)], writes=[shT.reg()])
                for nh_ in range(2):
                    for c in range(2):
                        S.op("pe", lambda e, c=c, nh_=nh_: e.matmul(banks[6 + nh_][:], lhsT=shT.ap[:, c, :].bitcast(F32R),
                                                                  rhs=wds.ap[:, c, nh_ * 512:(nh_ + 1) * 512].bitcast(F32R), start=(c == 0), stop=(c == 1)),
                             reads=[shT.reg(), wds.reg()], writes=[bank_reg(6 + nh_)])
                for nh_ in range(2):
                    csl = slice(nh_ * 512, (nh_ + 1) * 512)
                    S.op("dve", lambda e, nh_=nh_, csl=csl: e.tensor_tensor(out=baseb.ap[:, csl], in0=banks[6 + nh_][:], in1=modsC.ap[:, 2048 + nh_ * 512:2048 + (nh_ + 1) * 512], op=ALU.mult),
                         reads=[bank_reg(6 + nh_), modsC.reg()], writes=[baseb.reg(nh_ * 2048, (nh_ + 1) * 2048)])
                S.op("dve", lambda e, xb=xb: e.tensor_tensor(out=baseb.ap, in0=baseb.ap, in1=xb.ap, op=ALU.add), reads=[baseb.reg(), xb.reg()], writes=[baseb.reg()])
                dma_sp(base_dram[tsl, :], baseb.ap, [baseb.reg()], [R("base_dram", j, j + 1)], sem="st")
                if debug:
                    dma_sp(dbg["base"][tsl, :], baseb.ap, [baseb.reg()], [R("dbg_base", j, j + 1)], sem="st")
                S.op("dve", lambda e: e.tensor_tensor(out=bi_.ap, in0=sc.ap, in1=rbias.ap, op=ALU.add), reads=[sc.reg(), rbias.reg()], writes=[bi_.reg()])
                bv = bi_.ap.rearrange("p (g e) -> p g e", g=NGRP)
                m1 = rs.ap[:, 0:8]
                m2 = rs.ap[:, 8:16]
                S.op("dve", lambda e: e.tensor_reduce(out=m1, in_=bv, axis=AX.X, op=ALU.max), reads=[bi_.reg()], writes=[rs.reg()])
                ev = eqt.ap.rearrange("p (g e) -> p g e", g=NGRP)
                S.op("dve", lambda e: e.tensor_tensor(out=ev, in0=bv, in1=m1.unsqueeze(2).to_broadcast([128, 8, 32]), op=ALU.is_equal),
                     reads=[bi_.reg(), rs.reg()], writes=[eqt.reg()])
                S.op("dve", lambda e: e.scalar_tensor_tensor(out=eqt.ap, in0=eqt.ap, scalar=-1e9, in1=bi_.ap, op0=ALU.mult, op1=ALU.add),
                     reads=[eqt.reg(), bi_.reg()], writes=[eqt.reg()])
                S.op("dve", lambda e: e.tensor_reduce(out=m2, in_=ev, axis=AX.X, op=ALU.max), reads=[eqt.reg()], writes=[rs.reg()])
                gs = rs.ap[:, 16:24]
                S.op("dve", lambda e: e.tensor_tensor(out=gs, in0=m1, in1=m2, op=ALU.add), reads=[rs.reg()], writes=[rs.reg()])
                cmp = rs.ap[:, 32:96].rearrange("p (a b) -> p a b", a=8)
                S.op("dve", lambda e: e.tensor_tensor(out=cmp, in0=gs.unsqueeze(1).to_broadcast([128, 8, 8]), in1=gs.unsqueeze(2).to_broadcast([128, 8, 8]), op=ALU.is_gt),
                     reads=[rs.reg()], writes=[rs.reg()])
                cntg = rs.ap[:, 24:32]
                S.op("dve", lambda e: e.tensor_reduce(out=cntg, in_=cmp, axis=AX.X, op=ALU.add), reads=[rs.reg()], writes=[rs.reg()])
                S.op("dve", lambda e: e.tensor_scalar(out=cntg, in0=cntg, scalar1=3.5, scalar2=-1e9, op0=ALU.is_gt, op1=ALU.mult),
                     reads=[rs.reg()], writes=[rs.reg()])
                mv = msk.ap.rearrange("p (g e) -> p g e", g=NGRP)
                S.op("dve", lambda e: e.tensor_tensor(out=mv, in0=bv, in1=cntg.unsqueeze(2).to_broadcast([128, 8, 32]), op=ALU.add),
                     reads=[bi_.reg(), rs.reg()], writes=[msk.reg()])
                S.op("dve", lambda e: e.max(out=mx8.ap, in_=msk.ap), reads=[msk.reg()], writes=[mx8.reg()])
                S.op("dve", lambda e: e.max_index(out=idx8.ap, in_max=mx8.ap, in_values=msk.ap), reads=[msk.reg(), mx8.reg()], writes=[idx8.reg()])
                S.op("dve", lambda e: e.tensor_copy(out=idx8f.ap, in_=idx8.ap), reads=[idx8.reg()], writes=[idx8f.reg()])
                S.op("dve", lambda e: e.tensor_scalar(out=eqt.ap, in0=msk.ap, scalar1=mx8.ap[:, 7:8], scalar2=None, op0=ALU.is_ge),
                     reads=[msk.reg(), mx8.reg()], writes=[eqt.reg()])
                S.op("pe", lambda e: e.matmul(banks[3][:, 0:256], lhsT=tri, rhs=eqt.ap, start=True, stop=False),
                     reads=[eqt.reg(), R("consts")], writes=[bank_reg(3)])
                S.op("pe", lambda e: e.matmul(banks[3][:, 0:256], lhsT=ones, rhs=mcum.ap, start=False, stop=True),
                     reads=[mcum.reg(), R("consts")], writes=[bank_reg(3)])
                S.op("dve", lambda e: e.tensor_tensor(out=mcum.ap, in0=mcum.ap, in1=eqt.ap, op=ALU.add), reads=[mcum.reg(), eqt.reg()], writes=[mcum.reg()])
                S.op("dve", lambda e, j=j: e.tensor_copy(out=posall.ap[:, j, :], in_=banks[3][:, 0:256]), reads=[bank_reg(3)], writes=[posall.reg(j * 1024, (j + 1) * 1024)])
                S.op("dve", lambda e, j=j: e.tensor_copy(out=idxall.ap[:, j, :], in_=idx8f.ap), reads=[idx8f.reg()], writes=[idxall.reg(j * 32, j * 32 + 32)])
                for k in range(8):
                    S.op("dve", lambda e, k=k: e.scalar_tensor_tensor(out=junk.ap[:, 256:512], in0=iota_e, scalar=idx8f.ap[:, k:k + 1], in1=sc.ap,
                                                                     op0=ALU.is_equal, op1=ALU.mult, accum_out=sc8.ap[:, k:k + 1]),
                         reads=[idx8f.reg(), sc.reg(), R("consts")], writes=[junk.reg(), sc8.reg(k * 4, k * 4 + 4)])
                ssum = rs.ap[:, 96:97]
                S.op("dve", lambda e: e.tensor_reduce(out=ssum, in_=sc8.ap, axis=AX.X, op=ALU.add), reads=[sc8.reg()], writes=[rs.reg()])
                S.op("dve", lambda e: e.reciprocal(out=rs.ap[:, 97:98], in_=ssum), reads=[rs.reg()], writes=[rs.reg()])
                S.op("dve", lambda e, j=j: e.tensor_scalar(out=gate8.ap[:, j, :], in0=sc8.ap, scalar1=rs.ap[:, 97:98], scalar2=2.5, op0=ALU.mult, op1=ALU.mult),
                     reads=[sc8.reg(), rs.reg()], writes=[gate8.reg(j * 32, j * 32 + 32)])

            for c in range(2):
                S.op("pe", lambda e, c=c: e.matmul(banks[3][:, c:c + 1], lhsT=mcum.ap[:, c * 128:(c + 1) * 128], rhs=ones[:, 0:1], start=True, stop=True),
                     reads=[mcum.reg(), R("consts")], writes=[bank_reg(3)])
            S.op("dve", lambda e: e.tensor_scalar(out=ovT.ap[:, 0:2], in0=banks[3][:, 0:2], scalar1=float(CAP) + 0.5, scalar2=None, op0=ALU.is_gt),
                 reads=[bank_reg(3)], writes=[ovT.reg()])
            S.op("pe", lambda e: e.matmul(banks[2][:, 0:2], lhsT=tri, rhs=ovT.ap[:, 0:2], start=True, stop=False),
                 reads=[ovT.reg(), R("consts")], writes=[bank_reg(2)])
            S.op("pe", lambda e: e.matmul(banks[2][:, 1:2], lhsT=ones, rhs=ovT.ap[:, 0:1], start=False, stop=True),
                 reads=[ovT.reg(), R("consts")], writes=[bank_reg(2)])
            S.op("dve", lambda e: e.tensor_copy(out=rankT.ap[:, 0:2], in_=banks[2][:, 0:2]), reads=[bank_reg(2)], writes=[rankT.reg()])
            for c in range(2):
                S.op("dve", lambda e, c=c: e.tensor_scalar(out=diagR.ap, in0=ident, scalar1=rankT.ap[:, c:c + 1], scalar2=None, op0=ALU.mult),
                     reads=[rankT.reg(), R("consts")], writes=[diagR.reg()])
                S.op("pe", lambda e, c=c: e.matmul(banks[3][:, c * 128:(c + 1) * 128], lhsT=ones, rhs=diagR.ap, start=True, stop=True),
                     reads=[diagR.reg(), R("consts")], writes=[bank_reg(3)])
            S.op("dve", lambda e: e.tensor_scalar(out=ovb.ap, in0=banks[3][:, 0:256], scalar1=float(NOV) - 0.5, scalar2=1e7, op0=ALU.is_gt, op1=ALU.mult),
                 reads=[bank_reg(3)], writes=[ovb.reg()])
            S.op("dve", lambda e: e.scalar_tensor_tensor(out=ovb.ap, in0=banks[3][:, 0:256], scalar=float(CAP), in1=ovb.ap, op0=ALU.mult, op1=ALU.add),
                 reads=[bank_reg(3), ovb.reg()], writes=[ovb.reg()])
            S.op("dve", lambda e: e.tensor_scalar(out=ovb.ap, in0=ovb.ap, scalar1=float((NE - 1) * CAP), scalar2=None, op0=ALU.add),
                 reads=[ovb.reg()], writes=[ovb.reg()])
            S.op("dve", lambda e: e.scalar_tensor_tensor(out=diffr.ap, in0=iota_e, scalar=float(CAP), in1=ovb.ap, op0=ALU.mult, op1=ALU.subtract),
                 reads=[ovb.reg(), R("consts")], writes=[diffr.reg()])
            for c in range(2):
                S.op("dve", lambda e, c=c: e.tensor_scalar(out=selT.ap[:, c, :], in0=iota_e[:, 0:NOV], scalar1=rankT.ap[:, c:c + 1], scalar2=ovT.ap[:, c:c + 1],
                                                           op0=ALU.is_equal, op1=ALU.mult),
                     reads=[rankT.reg(), ovT.reg(), R("consts")], writes=[selT.reg()])
            for c in range(2):
                S.op("pe", lambda e, c=c: e.matmul(banks[2][:, 0:NOV], lhsT=(eid0 if c == 0 else eid1), rhs=selT.ap[:, c, :], start=(c == 0), stop=(c == 1)),
                     reads=[selT.reg(), R("consts")], writes=[bank_reg(2)])
            S.op("dve", lambda e: e.tensor_scalar(out=widxf.ap, in0=banks[2][:, 0:NOV], scalar1=128.0, scalar2=tokf[:, 0:1], op0=ALU.mult, op1=ALU.add),
                 reads=[bank_reg(2), R("consts")], writes=[widxf.reg()])
            S.op("dve", lambda e: e.tensor_copy(out=widx.ap, in_=widxf.ap), reads=[widxf.reg()], writes=[widx.reg()])

            for j in range(NT):
                pj = posall.ap[:, j, :]
                S.op("dve", lambda e, pj=pj: e.tensor_scalar(out=eqt.ap, in0=pj, scalar1=float(CAP) - 0.5, scalar2=None, op0=ALU.is_lt),
                     reads=[posall.reg(j * 1024, (j + 1) * 1024)], writes=[eqt.reg()])
                S.op("dve", lambda e: e.tensor_tensor(out=offm.ap, in0=eqt.ap, in1=diffr.ap, op=ALU.mult), reads=[eqt.reg(), diffr.reg()], writes=[offm.reg()])
                S.op("dve", lambda e: e.tensor_tensor(out=offm.ap, in0=offm.ap, in1=ovb.ap, op=ALU.add), reads=[offm.reg(), ovb.reg()], writes=[offm.reg()])
                S.op("dve", lambda e, pj=pj: e.tensor_tensor(out=offm.ap, in0=offm.ap, in1=pj, op=ALU.add), reads=[offm.reg(), posall.reg(j * 1024, (j + 1) * 1024)], writes=[offm.reg()])
                S.op("dve", lambda e, pj=pj: e.tensor_scalar(out=eqt.ap, in0=pj, scalar1=2.0 * CAP - 0.5, scalar2=1e7, op0=ALU.is_gt, op1=ALU.mult),
                     reads=[posall.reg(j * 1024, (j + 1) * 1024)], writes=[eqt.reg()])
                S.op("dve", lambda e: e.tensor_tensor(out=offm.ap, in0=offm.ap, in1=eqt.ap, op=ALU.add), reads=[offm.reg(), eqt.reg()], writes=[offm.reg()])
                for k in range(8):
                    S.op("dve", lambda e, k=k, j=j: e.scalar_tensor_tensor(out=junk.ap[:, 0:256], in0=iota_e, scalar=idxall.ap[:, j, k:k + 1], in1=offm.ap,
                                                                          op0=ALU.is_equal, op1=ALU.mult, accum_out=off8f.ap[:, k:k + 1]),
                         reads=[idxall.reg(j * 32, j * 32 + 32), offm.reg(), R("consts")], writes=[junk.reg(), off8f.reg(k * 4, k * 4 + 4)])
                S.op("dve", lambda e, j=j: e.tensor_copy(out=off8.ap[:, j, :], in_=off8f.ap), reads=[off8f.reg()], writes=[off8.reg(j * 32, j * 32 + 32)])
                for k in range(8):
                    S.op("pool", lambda e, j=j, k=k: e.indirect_dma_start(out=list_dram[:, :], out_offset=bass.IndirectOffsetOnAxis(ap=off8.ap[:, j, k:k + 1], axis=0),
                                                                         in_=tok_i.ap[:, j, :], in_offset=None, bounds_check=breg(e, (NE + NOV) * CAP - 1), oob_is_err=False),
                         reads=[off8.reg(j * 32, j * 32 + 32), tok_i.reg()], writes=[R("list_dram", j * 8 + k + 1, j * 8 + k + 2)], dsem="lsc")
            if debug:
                dma_sp(dbg["off8"], off8.ap.rearrange("p j k -> p (j k)"), [off8.reg()], [R("dbg_off8")], sem="st")
                dma_sp(dbg["gate8"], gate8.ap.rearrange("p j k -> p (j k)"), [gate8.reg()], [R("dbg_gate8")], sem="st")

            checkpoint(5)
            A.top = CKEEP
            NW = 3
            wg_ = [A.get(f"wg{i}", 8 * 256 * 4, F32, "p (k n) -> p k n", k=8) for i in range(NW)]
            wu_ = [A.get(f"wu{i}", 8 * 256 * 4, F32, "p (k n) -> p k n", k=8) for i in range(NW)]
            wdn = [A.get(f"wdn{i}", 2 * 1024 * 4, F32, "p (c n) -> p c n", c=2) for i in range(NW)]
            xe = [A.get(f"xe{i}", 4096, F32) for i in range(NW)]
            lidx = [A.get(f"lidx{i}", 8, I32) for i in range(NW)]
            xeT = [A.get(f"xeT{i}", 4096, F32, "p (k t) -> p k t", k=8) for i in range(2)]
            es = [A.get(f"es{i}", 1024, F32) for i in range(2)]
            eh = [A.get(f"eh{i}", 1024, F32) for i in range(2)]
            ehT = [A.get(f"ehT{i}", 1024, F32, "p (c t) -> p c t", c=2) for i in range(2)]
            ysb = [A.get(f"ysb{i}", 4096, F32) for i in range(2)]

            def load_expert(e_):
                s_ = e_ % NW
                dma_pool(wg_[s_].ap.bitcast(F32R), wge_d[e_].rearrange("(p k) n -> p k n", k=8), [], [wg_[s_].reg()], sem="wg%d" % s_)
                dma_pool(wu_[s_].ap.bitcast(F32R), wue_d[e_].rearrange("(p k) n -> p k n", k=8), [], [wu_[s_].reg()], sem="wu%d" % s_)
                dma_pool(wdn[s_].ap.bitcast(F32R), wde_d[e_].rearrange("(p c) n -> p c n", c=2), [], [wdn[s_].reg()], sem="wd%d" % s_)
                dma_sp(lidx[s_].ap[:, 0:2], list_dram[e_ * CAP:(e_ + 1) * CAP, :], [R("list_dram", 0, 1000)], [lidx[s_].reg()], sem="li%d" % s_)
                S.op("pool", lambda e, s_=s_: e.indirect_dma_start(out=xe[s_].ap, out_offset=None, in_=h2_dram[:, :],
                                                                   in_offset=bass.IndirectOffsetOnAxis(ap=lidx[s_].ap[:, 0:1], axis=0),
                                                                   bounds_check=breg(e, 2048), oob_is_err=False),
                     reads=[lidx[s_].reg(), R("h2_dram", 0, 17)], writes=[xe[s_].reg()], dsem="xg%d" % s_)

            for e_ in range(min(NW - 1, NE)):
                load_expert(e_)
            for e_ in range(NE):
                if e_ + NW - 1 < NE:
                    load_expert(e_ + NW - 1)
                s_ = e_ % NW
                d_ = e_ % 2
                b0, b1 = (0, 1) if d_ == 0 else (2, 3)
                transpose8(xe[s_].ap, xe[s_].reg(), xeT[d_], b0, b1)
                for (wbuf, c0) in ((wg_[s_], 0), (wu_[s_], 256)):
                    for k in range(8):
                        S.op("pe", lambda e, k=k, wbuf=wbuf, c0=c0, d_=d_: e.matmul(banks[4][:, c0:c0 + 256], lhsT=xeT[d_].ap[:, k, :].bitcast(F32R),
                                                                                 rhs=wbuf.ap[:, k, :].bitcast(F32R), start=(k == 0), stop=(k == 7)),
                             reads=[xeT[d_].reg(), wbuf.reg()], writes=[bank_reg(4)])
                S.op("act", lambda e, d_=d_: e.activation(out=es[d_].ap, in_=banks[4][:, 0:256], func=ACTF.Silu), reads=[bank_reg(4)], writes=[es[d_].reg()])
                S.op("dve", lambda e, d_=d_: e.tensor_tensor(out=eh[d_].ap, in0=es[d_].ap, in1=banks[4][:, 256:512], op=ALU.mult),
                     reads=[es[d_].reg(), bank_reg(4)], writes=[eh[d_].reg()])
                ehv = eh[d_].ap.rearrange("t (p c) -> t c p", c=2)
                for c in range(2):
                    S.op("pe", lambda e, c=c, ehv=ehv, d_=d_: e.transpose(banks[5][:, c * 128:(c + 1) * 128], ehv[:, c, :], ident),
                         reads=[eh[d_].reg(), R("consts")], writes=[bank_reg(5, c * 512, c * 512 + 512)])
                S.op("act", lambda e, d_=d_: e.activation(out=ehT[d_].ap.bitcast(F32R), in_=banks[5][:, 0:256].rearrange("p (c t) -> p c t", c=2), func=ACTF.Copy),
                     reads=[bank_reg(5)], writes=[ehT[d_].reg()])
                for nh_ in range(2):
                    for c in range(2):
                        S.op("pe", lambda e, c=c, nh_=nh_, s_=s_, d_=d_: e.matmul(banks[6 + nh_][:], lhsT=ehT[d_].ap[:, c, :].bitcast(F32R),
                                                                               rhs=wdn[s_].ap[:, c, nh_ * 512:(nh_ + 1) * 512].bitcast(F32R), start=(c == 0), stop=(c == 1)),
                             reads=[ehT[d_].reg(), wdn[s_].reg()], writes=[bank_reg(6 + nh_)])
                S.op("dve", lambda e, d_=d_: e.tensor_copy(out=ysb[d_].ap[:, 0:512], in_=banks[6][:]), reads=[bank_reg(6)], writes=[ysb[d_].reg(0, 2048)])
                S.op("act", lambda e, d_=d_: e.activation(out=ysb[d_].ap[:, 512:1024], in_=banks[7][:], func=ACTF.Copy), reads=[bank_reg(7)], writes=[ysb[d_].reg(2048, 4096)])
                dma_sp(y_dram[e_ * CAP:(e_ + 1) * CAP, :], ysb[d_].ap, [ysb[d_].reg()], [R("y_dram", e_, e_ + 1)], sem="yst%d" % d_)

            A.top = CKEEP
            wgo = [A.get(f"wgo{i}", 8 * 256 * 4, F32, "p (k n) -> p k n", k=8) for i in range(2)]
            wuo = [A.get(f"wuo{i}", 8 * 256 * 4, F32, "p (k n) -> p k n", k=8) for i in range(2)]
            wdo = [A.get(f"wdo{i}", 2 * 1024 * 4, F32, "p (c n) -> p c n", c=2) for i in range(2)]
            wge_rows = wge_d.rearrange("e (p k) n -> (e p) (k n)", k=8)
            wue_rows = wue_d.rearrange("e (p k) n -> (e p) (k n)", k=8)
            wde_rows = wde_d.rearrange("e (p c) n -> (e p) (c n)", c=2)
            for ob in range(NOV):
                d_ = ob % 2
                s_ = ob % NW
                for (dst, src, nm) in ((wgo[d_], wge_rows, "og"), (wuo[d_], wue_rows, "ou"), (wdo[d_], wde_rows, "od")):
                    S.op("pool", lambda e, dst=dst, src=src, ob=ob: e.indirect_dma_start(
                        out=dst.ap.rearrange("p a b -> p (a b)"), out_offset=None, in_=src[:, :],
                        in_offset=bass.IndirectOffsetOnAxis(ap=widx.ap[:, ob:ob + 1], axis=0), bounds_check=breg(e, NE * 128 - 1), oob_is_err=False),
                        reads=[widx.reg()], writes=[dst.reg()], dsem="%s%d" % (nm, d_))
                dma_sp(lidx[s_].ap[:, 0:2], list_dram[(NE + ob) * CAP:(NE + ob + 1) * CAP, :], [R("list_dram", 0, 1000)], [lidx[s_].reg()], sem="li%d" % s_)
                S.op("pool", lambda e, s_=s_: e.indirect_dma_start(out=xe[s_].ap, out_offset=None, in_=h2_dram[:, :],
                                                                   in_offset=bass.IndirectOffsetOnAxis(ap=lidx[s_].ap[:, 0:1], axis=0),
                                                                   bounds_check=breg(e, 2048), oob_is_err=False),
                     reads=[lidx[s_].reg(), R("h2_dram", 0, 17)], writes=[xe[s_].reg()], dsem="xg%d" % s_)
                b0, b1 = (0, 1) if d_ == 0 else (2, 3)
                transpose8(xe[s_].ap, xe[s_].reg(), xeT[d_], b0, b1)
                for (wbuf, c0) in ((wgo[d_], 0), (wuo[d_], 256)):
                    for k in range(8):
                        S.op("pe", lambda e, k=k, wbuf=wbuf, c0=c0, d_=d_: e.matmul(banks[4][:, c0:c0 + 256], lhsT=xeT[d_].ap[:, k, :], rhs=wbuf.ap[:, k, :],
                                                                                 start=(k == 0), stop=(k == 7)),
                             reads=[xeT[d_].reg(), wbuf.reg()], writes=[bank_reg(4)])
                S.op("act", lambda e, d_=d_: e.activation(out=es[d_].ap, in_=banks[4][:, 0:256], func=ACTF.Silu), reads=[bank_reg(4)], writes=[es[d_].reg()])
                S.op("dve", lambda e, d_=d_: e.tensor_tensor(out=eh[d_].ap, in0=es[d_].ap, in1=banks[4][:, 256:512], op=ALU.mult),
                     reads=[es[d_].reg(), bank_reg(4)], writes=[eh[d_].reg()])
                ehv = eh[d_].ap.rearrange("t (p c) -> t c p", c=2)
                for c in range(2):
                    S.op("pe", lambda e, c=c, ehv=ehv, d_=d_: e.transpose(banks[5][:, c * 128:(c + 1) * 128], ehv[:, c, :], ident),
                         reads=[eh[d_].reg(), R("consts")], writes=[bank_reg(5, c * 512, c * 512 + 512)])
                S.op("act", lambda e, d_=d_: e.activation(out=ehT[d_].ap.bitcast(F32R), in_=banks[5][:, 0:256].rearrange("p (c t) -> p c t", c=2), func=ACTF.Copy),
                     reads=[bank_reg(5)], writes=[ehT[d_].reg()])
                for nh_ in range(2):
                    for c in range(2):
                        S.op("pe", lambda e, c=c, nh_=nh_, d_=d_: e.matmul(banks[6 + nh_][:], lhsT=ehT[d_].ap[:, c, :], rhs=wdo[d_].ap[:, c, nh_ * 512:(nh_ + 1) * 512],
                                                                        start=(c == 0), stop=(c == 1)),
                             reads=[ehT[d_].reg(), wdo[d_].reg()], writes=[bank_reg(6 + nh_)])
                S.op("dve", lambda e, d_=d_: e.tensor_copy(out=ysb[d_].ap[:, 0:512], in_=banks[6][:]), reads=[bank_reg(6)], writes=[ysb[d_].reg(0, 2048)])
                S.op("act", lambda e, d_=d_: e.activation(out=ysb[d_].ap[:, 512:1024], in_=banks[7][:], func=ACTF.Copy), reads=[bank_reg(7)], writes=[ysb[d_].reg(2048, 4096)])
                dma_sp(y_dram[(NE + ob) * CAP:(NE + ob + 1) * CAP, :], ysb[d_].ap, [ysb[d_].reg()], [R("y_dram", NE + ob, NE + ob + 1)], sem="yst%d" % d_)

            checkpoint(6)
            A.top = CKEEP
            yg = [A.get(f"yg{i}", 4096, F32) for i in range(4)]
            acc = [A.get(f"acc{i}", 4096, F32) for i in range(2)]
            bs = [A.get(f"bs{i}", 4096, F32) for i in range(2)]
            gi_ = 0
            last_tok = None
            for j in range(NT):
                tsl = slice(j * 128, (j + 1) * 128)
                ac = acc[j % 2]
                bsb = bs[j % 2]
                dma_sp(bsb.ap, base_dram[tsl, :], [R("base_dram", j, j + 1)], [bsb.reg()], sem="bs%d" % (j % 2))
                for k in range(8):
                    g = yg[gi_ % 4]
                    gs_ = gi_ % 4
                    gi_ += 1
                    S.op("pool", lambda e, g=g: e.memset(g.ap, 0.0), writes=[g.reg()])
                    S.op("pool", lambda e, g=g, j=j, k=k: e.indirect_dma_start(out=g.ap, out_offset=None, in_=y_dram[:, :],
                                                                               in_offset=bass.IndirectOffsetOnAxis(ap=off8.ap[:, j, k:k + 1], axis=0),
                                                                               bounds_check=breg(e, (NE + NOV) * CAP - 1), oob_is_err=False),
                         reads=[off8.reg(j * 32, j * 32 + 32), R("y_dram", 0, 1000)], writes=[g.reg()], dsem="yg%d" % gs_)
                    if k == 0:
                        S.op("dve", lambda e, g=g, ac=ac, j=j, k=k: e.tensor_scalar(out=ac.ap, in0=g.ap, scalar1=gate8.ap[:, j, k:k + 1], scalar2=None, op0=ALU.mult),
                             reads=[g.reg(), gate8.reg()], writes=[ac.reg()])
                    else:
                        S.op("dve", lambda e, g=g, ac=ac, j=j, k=k: e.scalar_tensor_tensor(out=ac.ap, in0=g.ap, scalar=gate8.ap[:, j, k:k + 1], in1=ac.ap, op0=ALU.mult, op1=ALU.add),
                             reads=[g.reg(), gate8.reg(), ac.reg()], writes=[ac.reg()])
                S.op("dve", lambda e, ac=ac: e.tensor_tensor(out=ac.ap, in0=ac.ap, in1=modsC.ap[:, 2048:3072], op=ALU.mult), reads=[ac.reg(), modsC.reg()], writes=[ac.reg()])
                S.op("dve", lambda e, ac=ac, bsb=bsb: e.tensor_tensor(out=ac.ap, in0=ac.ap, in1=bsb.ap, op=ALU.add), reads=[ac.reg(), bsb.reg()], writes=[ac.reg()])
                last_tok = dma_sp(out_d[tsl, :], ac.ap, [ac.reg()], [R("out_d", j, j + 1)], sem="ost")
        except _Stop:
            pass
        S.wait_all("sp", [(k, v) for k, v in S.cnt.items() if k not in COMPUTE])
        S.emit()
    return nc


_NC_CACHE = {}


def make_in_maps(inputs, ncores=8):
    f = lambda a: np.ascontiguousarray(np.asarray(a))
    x = f(inputs["x"]); c = f(inputs["c"]); pos = f(inputs["positions"])
    shared = dict(
        w_ada=f(inputs["w_ada"][0]), b_ada=f(inputs["b_ada"][0]).reshape(1, 6144),
        g_mix=f(inputs["g_norm_mix"][0]).reshape(1, 1024), g_ffn=f(inputs["g_norm_ffn"][0]).reshape(1, 1024),
        w_in=f(inputs["w_in"][0]),
        gqk=np.concatenate([f(inputs["g_q_a"][0]), f(inputs["g_k_a"][0]), f(inputs["g_q_b"][0]), f(inputs["g_k_b"][0])]).reshape(1, 256),
        sinks=f(inputs["sinks_a"][0]).reshape(1, 8),
        g_out=np.concatenate([f(inputs["g_out_a"][0]), f(inputs["g_out_b"][0])]).reshape(1, 1024),
        w_out=f(inputs["w_out"][0]), w_router=f(inputs["w_router"][0]), rbias=f(inputs["router_bias"][0]).reshape(1, 256),
        w_gate_e=f(inputs["w_gate_e"][0]), w_up_e=f(inputs["w_up_e"][0]), w_down_e=f(inputs["w_down_e"][0]),
        w_gate_s=f(inputs["w_gate_s"][0]), w_up_s=f(inputs["w_up_s"][0]), w_down_s=f(inputs["w_down_s"][0]),
        consts=make_consts(),
    )
    maps = []
    for b in range(ncores):
        m = dict(shared)
        m["x"] = x[b]
        m["cT"] = np.ascontiguousarray(c[b].reshape(128, 8))
        m["posT"] = np.ascontiguousarray(pos[b].reshape(16, 128).T.astype(np.int32))
        maps.append(m)
    return maps


def kernel(**inputs):
    if "nc" not in _NC_CACHE:
        _NC_CACHE["nc"] = build_nc()
    nc = _NC_CACHE["nc"]
    maps = make_in_maps(inputs, 8)
    res = run_bass_kernel_spmd(nc, maps, core_ids=list(range(8)))
    out = np.stack([np.asarray(r["out"]) for r in res.results], axis=0)
    return out.astype(np.float32)
```

```python
import contextlib
import numpy as np
import concourse.bass as bass
import concourse.mybir as mybir
from concourse.bass_utils import run_bass_kernel_spmd

F32 = mybir.dt.float32
F32R = mybir.dt.float32r
BF16 = mybir.dt.bfloat16
I32 = mybir.dt.int32
U32 = mybir.dt.uint32
ALU = mybir.AluOpType
ACTF = mybir.ActivationFunctionType
AX = mybir.AxisListType

COMPUTE = ("pe", "act", "dve", "pool")
ENGS = ("pe", "act", "dve", "pool", "sp")
NT = 16
CAP = 128
NE = 256
EPS = 1e-6
NCONST = 1048 + 256
NOV = 24


class Sched:
    def __init__(self, nc, stack, same_engine_sync=True):
        self.nc = nc
        self.stack = stack
        self.same = same_engine_sync
        self.ops = {e: [] for e in ENGS}
        self.cnt = {}
        self.sems = {}
        self.recs = {}
        self.bank_last = {}
        self.waited = {e: {} for e in ENGS}
        for e in COMPUTE:
            self._sem(e)

    def _sem(self, key):
        if key not in self.sems:
            self.sems[key] = self.stack.enter_context(self.nc.semaphore("s_" + key))
            self.cnt[key] = 0
        return self.sems[key]

    def _deps(self, reads, writes):
        deps = []
        for (sp, lo, hi) in reads:
            if sp.startswith("bank"):
                continue
            for r in self.recs.get(sp, ()):
                if r[2] == "w" and r[0] < hi and lo < r[1]:
                    deps.append(r[3])
        for (sp, lo, hi) in writes:
            if sp.startswith("bank"):
                continue
            for r in self.recs.get(sp, ()):
                if r[0] < hi and lo < r[1]:
                    deps.append(r[3])
        for (sp, lo, hi) in list(reads) + list(writes):
            if sp.startswith("bank") and sp in self.bank_last:
                deps.append(self.bank_last[sp])
        return deps

    def _record(self, reads, writes, tok):
        for (sp, lo, hi) in list(reads) + list(writes):
            if sp.startswith("bank"):
                self.bank_last[sp] = tok
        for (sp, lo, hi) in writes:
            if sp.startswith("bank"):
                continue
            lst = self.recs.setdefault(sp, [])
            lst[:] = [r for r in lst if not (lo <= r[0] and r[1] <= hi)]
            lst.append([lo, hi, "w", tok])
        for (sp, lo, hi) in reads:
            if sp.startswith("bank"):
                continue
            lst = self.recs.setdefault(sp, [])
            lst[:] = [r for r in lst if not (r[2] == "r" and r[3][0] == tok[0]
                                             and lo <= r[0] and r[1] <= hi)]
            lst.append([lo, hi, "r", tok])

    def op(self, eng, emit, reads=(), writes=(), dsem=None):
        deps = self._deps(reads, writes)
        if dsem is None:
            key, amt = eng, 1
        else:
            key, amt = dsem, 16
            self._sem(key)
        waits = {}
        for (k, v) in deps:
            if k == eng and dsem is None and (eng == "pe" or not self.same):
                continue
            if self.waited[eng].get(k, 0) >= v:
                continue
            waits[k] = max(waits.get(k, 0), v)
        for k, v in waits.items():
            self.waited[eng][k] = v
        self.cnt[key] += amt
        tok = (key, self.cnt[key])
        self.ops[eng].append((sorted(waits.items()), emit, key, amt))
        self._record(reads, writes, tok)
        return tok

    def wait_all(self, eng, toks):
        waits = {}
        for (k, v) in toks:
            waits[k] = max(waits.get(k, 0), v)
        self.ops[eng].append((sorted(waits.items()), None, None, 0))

    def emit(self):
        nc = self.nc
        with nc.Block() as block:
            def run(engname):
                def body(eng):
                    for waits, emit, key, amt in self.ops[engname]:
                        for (k, v) in waits:
                            eng.wait_ge(self.sems[k], v)
                        if emit is not None:
                            ins = emit(eng)
                            ins.then_inc(self.sems[key], amt)
                return body
            block.tensor(run("pe"))
            block.scalar(run("act"))
            block.vector(run("dve"))
            block.gpsimd(run("pool"))
            block.sync(run("sp"))


def R(name, lo=0, hi=1):
    return (name, lo, hi)


def make_consts():
    c = np.zeros((128, NCONST), np.float32)
    p = np.arange(128)
    c[:, 0:128] = np.eye(128, dtype=np.float32)
    c[:, 128:256] = (p[:, None] < p[None, :]).astype(np.float32)
    c[:, 256:384] = 1.0
    c[:, 384:640] = np.arange(256, dtype=np.float32)[None, :]
    c[:, 640:768] = (p[:, None] <= p[None, :]).astype(np.float32)
    c[:, 768:896] = (p[:, None] > p[None, :]).astype(np.float32)
    c[:, 896:1024] = (p[:, None] >= p[None, :]).astype(np.float32)
    inv = (np.float32(500000.0) ** (-np.arange(0, 16, 2, dtype=np.float32) / np.float32(16))).astype(np.float32)
    c[:, 1024:1032] = inv[None, :]
    c[:, 1032:1048] = (128 * np.arange(16)[None, :] + p[:, None]).astype(np.float32)
    c[:, 1048:1176] = p[:, None].astype(np.float32)
    c[:, 1176:1304] = (128 + p[:, None]).astype(np.float32)
    return c


class _Stop(Exception):
    pass


def build_nc(debug=False, stop_at=None):
    nc = bass.Bass("TRN2", target_bir_lowering=False)

    def din(name, shape, dt=F32):
        return nc.dram_tensor(name, shape, dt, kind="ExternalInput").ap()

    x_d = din("x", [2048, 1024])
    cT_d = din("cT", [128, 8])
    pos_d = din("posT", [128, 16], I32)
    w_ada_d = din("w_ada", [1024, 6144])
    b_ada_d = din("b_ada", [1, 6144])
    gmix_d = din("g_mix", [1, 1024])
    gffn_d = din("g_ffn", [1, 1024])
    w_in_d = din("w_in", [1024, 2304])
    gqk_d = din("gqk", [1, 256])
    sinks_d = din("sinks", [1, 8])
    gout_d = din("g_out", [1, 1024])
    w_out_d = din("w_out", [1024, 1024])
    wr_d = din("w_router", [1024, 256])
    rb_d = din("rbias", [1, 256])
    wge_d = din("w_gate_e", [256, 1024, 256])
    wue_d = din("w_up_e", [256, 1024, 256])
    wde_d = din("w_down_e", [256, 256, 1024])
    wgs_d = din("w_gate_s", [1024, 256])
    wus_d = din("w_up_s", [1024, 256])
    wds_d = din("w_down_s", [256, 1024])
    consts_d = din("consts", [128, NCONST])
    out_d = nc.dram_tensor("out", [2048, 1024], F32, kind="ExternalOutput").ap()

    mods_dram = nc.dram_tensor("mods_scr", [128, 6144], F32).ap()
    v_dram = nc.dram_tensor("v_scr", [2048, 520], BF16).ap()
    o4_dram = nc.dram_tensor("o4_scr", [2048, 520], F32).ap()
    o16_dram = nc.dram_tensor("o16_scr", [2048, 520], F32).ap()
    x1_dram = nc.dram_tensor("x1_scr", [2048, 1024], F32).ap()
    base_dram = nc.dram_tensor("base_scr", [2048, 1024], F32).ap()
    h2_dram = nc.dram_tensor("h2_scr", [2049, 1024], F32).ap()
    list_dram = nc.dram_tensor("list_scr", [(NE + NOV) * CAP, 2], I32).ap()
    y_dram = nc.dram_tensor("y_scr", [(NE + NOV) * CAP, 1024], F32).ap()

    dbg = {}
    if debug:
        dbg["x1"] = nc.dram_tensor("dbg_x1", [2048, 1024], F32, kind="ExternalOutput").ap()
        dbg["h2"] = nc.dram_tensor("dbg_h2", [2048, 1024], F32, kind="ExternalOutput").ap()
        dbg["off8"] = nc.dram_tensor("dbg_off8", [128, 128], I32, kind="ExternalOutput").ap()
        dbg["gate8"] = nc.dram_tensor("dbg_gate8", [128, 128], F32, kind="ExternalOutput").ap()
        dbg["base"] = nc.dram_tensor("dbg_base", [2048, 1024], F32, kind="ExternalOutput").ap()
        dbg["mods"] = nc.dram_tensor("dbg_mods", [128, 6144], F32, kind="ExternalOutput").ap()
        dbg["ocat"] = nc.dram_tensor("dbg_ocat", [2048, 1024], F32, kind="ExternalOutput").ap()
        dbg["qk"] = nc.dram_tensor("dbg_qk", [2048, 1664], BF16, kind="ExternalOutput").ap()
        dbg["kTa"] = nc.dram_tensor("dbg_kTa", [128, 2048], BF16, kind="ExternalOutput").ap()
        dbg["kTb"] = nc.dram_tensor("dbg_kTb", [128, 4 * 2048], BF16, kind="ExternalOutput").ap()
        dbg["qTa"] = nc.dram_tensor("dbg_qTa", [128, 4 * 2048], BF16, kind="ExternalOutput").ap()
        dbg["qTb"] = nc.dram_tensor("dbg_qTb", [128, 4 * 2048], BF16, kind="ExternalOutput").ap()
        dbg["v"] = nc.dram_tensor("dbg_v", [2048, 650], BF16, kind="ExternalOutput").ap()

    with contextlib.ExitStack() as st:
        S = Sched(nc, st)
        ARENA_F = 52100
        consts = nc.alloc_sbuf_tensor_at("consts_sb", [128, NCONST], F32, offset=16512)
        banks = [st.enter_context(nc.psum_tensor(f"bank{i}", [128, 512], F32)) for i in range(8)]

        uid = [0]
        ABASE = 21760

        class Buf:
            def __init__(self, name, off_b, nbytes, dt, shape_str=None, **kw):
                self.name = name
                self.off = off_b
                self.nbytes = nbytes
                isz = 2 if dt == BF16 else 4
                uid[0] += 1
                t = nc.alloc_sbuf_tensor_at("%s_%d" % (name, uid[0]), [128, nbytes // isz], dt, offset=ABASE + off_b)
                ap = t[:]
                if shape_str:
                    ap = ap.rearrange(shape_str, **kw)
                self.ap = ap

            def reg(self, lo=None, hi=None):
                if lo is None:
                    return ("arena", self.off, self.off + self.nbytes)
                return ("arena", self.off + lo, self.off + hi)

        class Alloc:
            def __init__(self):
                self.top = 0

            def get(self, name, nbytes, dt=F32, shape_str=None, **kw):
                nbytes = (nbytes + 31) // 32 * 32
                b = Buf(name, self.top, nbytes, dt, shape_str, **kw)
                self.top += nbytes
                assert self.top <= ARENA_F * 4, (name, self.top)
                return b

        def checkpoint(n):
            if stop_at == n:
                raise _Stop()

        last_tok = None
        try:
            A = Alloc()
            ident = consts[:, 0:128]
            tri = consts[:, 128:256]
            ones = consts[:, 256:384]
            iota_e = consts[:, 384:640]
            invf = consts[:, 1024:1032]
            tokf = consts[:, 1032:1048]
            eid0 = consts[:, 1048:1176]
            eid1 = consts[:, 1176:1304]
            ident_b = A.get("ident_b", 256, BF16)
            masks_b = A.get("masks_b", 3 * 256, BF16, "p (m k) -> p m k", m=3)
            maskA2 = A.get("maskA2", 2 * 4 * 256, BF16, "p (j h k) -> p j h k", j=2, h=4)
            maskB4 = A.get("maskB4", 4 * 256, BF16, "p (b k) -> p b k", b=4)
            cosb = A.get("cos", 16 * 8 * 4, F32, "p (j f) -> p j f", j=16)
            sinb = A.get("sin", 16 * 8 * 4, F32, "p (j f) -> p j f", j=16)
            off8 = A.get("off8", 128 * 4, I32, "p (j k) -> p j k", j=16)
            gate8 = A.get("gate8", 128 * 4, F32, "p (j k) -> p j k", j=16)
            tok_i = A.get("tok_i", 32 * 4, I32, "p (j t) -> p j t", t=2)
            small = A.get("small", 64 * 4, F32)
            neghalf = A.get("neghalf", 32 * 4, F32)
            esink = A.get("esink", 8 * 4, F32)
            junk = A.get("junk", 4096, F32)
            PERSIST_TOP = A.top

            ld = [0]

            def _autosem(reads, writes, sem):
                if sem not in (None, "st"):
                    return sem
                regs = reads if sem == "st" else writes
                r0 = regs[0]
                return ("s" if sem == "st" else "l") + "_%s_%s" % (r0[0], r0[1])

            def dma_sp(out, in_, reads, writes, sem=None):
                return S.op("sp", lambda e: e.dma_start(out=out, in_=in_), reads=reads, writes=writes, dsem=_autosem(reads, writes, sem))

            def dma_pool(out, in_, reads, writes, sem=None):
                return S.op("pool", lambda e: e.dma_start(out=out, in_=in_), reads=reads, writes=writes, dsem=_autosem(reads, writes, sem))

            _regs = {}

            def breg(e, val):
                if val not in _regs:
                    _regs[val] = e.to_reg(val)
                return _regs[val]

            def bank_reg(i, lo=0, hi=2048):
                return ("bank%d" % i, lo, hi)

            dma_sp(consts[:], consts_d, [], [R("consts")])
            S.op("dve", lambda e: e.tensor_copy(out=ident_b.ap, in_=ident), reads=[R("consts")], writes=[ident_b.reg()])
            for m in range(3):
                S.op("dve", lambda e, m=m: e.tensor_copy(out=masks_b.ap[:, m, :], in_=consts[:, 640 + 128 * m:768 + 128 * m]),
                     reads=[R("consts")], writes=[masks_b.reg()])
            for h in range(4):
                S.op("dve", lambda e, h=h: e.tensor_copy(out=maskA2.ap[:, 0, h, :], in_=consts[:, 768:896]),
                     reads=[R("consts")], writes=[maskA2.reg()])
                S.op("dve", lambda e, h=h: e.tensor_copy(out=maskA2.ap[:, 1, h, :], in_=consts[:, 640:768]),
                     reads=[R("consts")], writes=[maskA2.reg()])
                S.op("dve", lambda e, h=h: e.tensor_copy(out=maskB4.ap[:, h, :], in_=(consts[:, 896:1024] if h % 2 == 0 else consts[:, 640:768])),
                     reads=[R("consts")], writes=[maskB4.reg()])
            S.op("pool", lambda e: e.memset(neghalf.ap, -0.5), writes=[neghalf.reg()])
            S.op("dve", lambda e: e.tensor_copy(out=tok_i.ap, in_=tokf.unsqueeze(2).to_broadcast([128, 16, 2])), reads=[R("consts")], writes=[tok_i.reg()])

            TWO_PI = float(2 * np.pi)
            pos_i = A.get("pos_i", 64, I32)
            pos_f = A.get("pos_f", 64, F32)
            ang = A.get("ang", 512, F32, "p (j f) -> p j f", j=16)
            nrot = A.get("nrot", 512, F32, "p (j f) -> p j f", j=16)
            nrot_i = A.get("nrot_i", 512, I32, "p (j f) -> p j f", j=16)
            tmp_r = A.get("tmp_r", 512, F32, "p (j f) -> p j f", j=16)
            dma_sp(pos_i.ap, pos_d, [], [pos_i.reg()])
            S.op("dve", lambda e: e.tensor_copy(out=pos_f.ap, in_=pos_i.ap), reads=[pos_i.reg()], writes=[pos_f.reg()])
            S.op("dve", lambda e: e.tensor_tensor(out=ang.ap, in0=pos_f.ap.unsqueeze(2).to_broadcast([128, 16, 8]),
                                                  in1=invf.unsqueeze(1).to_broadcast([128, 16, 8]), op=ALU.mult),
                 reads=[pos_f.reg(), R("consts")], writes=[ang.reg()])
            S.op("dve", lambda e: e.tensor_scalar(out=nrot.ap, in0=ang.ap, scalar1=1.0 / TWO_PI, scalar2=None, op0=ALU.mult),
                 reads=[ang.reg()], writes=[nrot.reg()])
            S.op("dve", lambda e: e.tensor_copy(out=nrot_i.ap, in_=nrot.ap), reads=[nrot.reg()], writes=[nrot_i.reg()])
            S.op("dve", lambda e: e.tensor_copy(out=nrot.ap, in_=nrot_i.ap), reads=[nrot_i.reg()], writes=[nrot.reg()])
            S.op("dve", lambda e: e.scalar_tensor_tensor(out=ang.ap, in0=nrot.ap, scalar=-6.28125, in1=ang.ap, op0=ALU.mult, op1=ALU.add),
                 reads=[nrot.reg(), ang.reg()], writes=[ang.reg()])
            S.op("dve", lambda e: e.scalar_tensor_tensor(out=ang.ap, in0=nrot.ap, scalar=-(TWO_PI - 6.28125), in1=ang.ap, op0=ALU.mult, op1=ALU.add),
                 reads=[nrot.reg(), ang.reg()], writes=[ang.reg()])
            PI = float(np.pi)
            S.op("dve", lambda e: e.tensor_scalar(out=tmp_r.ap, in0=ang.ap, scalar1=PI, scalar2=-TWO_PI, op0=ALU.is_gt, op1=ALU.mult),
                 reads=[ang.reg()], writes=[tmp_r.reg()])
            S.op("dve", lambda e: e.tensor_tensor(out=ang.ap, in0=ang.ap, in1=tmp_r.ap, op=ALU.add),
                 reads=[ang.reg(), tmp_r.reg()], writes=[ang.reg()])
            S.op("dve", lambda e: e.tensor_scalar(out=tmp_r.ap, in0=ang.ap, scalar1=-PI, scalar2=TWO_PI, op0=ALU.is_lt, op1=ALU.mult),
                 reads=[ang.reg()], writes=[tmp_r.reg()])
            S.op("dve", lambda e: e.tensor_tensor(out=ang.ap, in0=ang.ap, in1=tmp_r.ap, op=ALU.add),
                 reads=[ang.reg(), tmp_r.reg()], writes=[ang.reg()])
            S.op("act", lambda e: e.activation(out=sinb.ap, in_=ang.ap, func=ACTF.Sin), reads=[ang.reg()], writes=[sinb.reg()])
            S.op("dve", lambda e: e.tensor_scalar(out=tmp_r.ap, in0=ang.ap, scalar1=-1.0, scalar2=None, op0=ALU.mult),
                 reads=[ang.reg()], writes=[tmp_r.reg()])
            S.op("dve", lambda e: e.tensor_tensor(out=tmp_r.ap, in0=tmp_r.ap, in1=ang.ap, op=ALU.max),
                 reads=[ang.reg(), tmp_r.reg()], writes=[tmp_r.reg()])
            S.op("dve", lambda e: e.tensor_scalar(out=tmp_r.ap, in0=tmp_r.ap, scalar1=-1.0, scalar2=PI / 2, op0=ALU.mult, op1=ALU.add),
                 reads=[tmp_r.reg()], writes=[tmp_r.reg()])
            S.op("act", lambda e: e.activation(out=cosb.ap, in_=tmp_r.ap, func=ACTF.Sin), reads=[tmp_r.reg()], writes=[cosb.reg()])
            A.top = PERSIST_TOP

            wada = [A.get(f"wada{i}", 8 * 512 * 4, F32, "p (k n) -> p k n", k=8) for i in range(2)]
            csb = A.get("csb", 8 * 128 * 4, F32, "p (k m) -> p k m", k=8)
            cs = A.get("cs", 32, F32)
            bbc = A.get("bbc", 6144 * 4, F32)
            gmix = A.get("gmix", 4096, F32)
            gffn = A.get("gffn", 4096, F32)
            mods = A.get("mods", 6144 * 4, F32)
            dma_sp(cs.ap, cT_d, [], [cs.reg()])
            dma_pool(bbc.ap, b_ada_d.partition_broadcast(128), [], [bbc.reg()])
            dma_pool(gmix.ap, gmix_d.partition_broadcast(128), [], [gmix.reg()])
            dma_pool(gffn.ap, gffn_d.partition_broadcast(128), [], [gffn.reg()])
            S.op("act", lambda e: e.activation(out=cs.ap, in_=cs.ap, func=ACTF.Silu), reads=[cs.reg()], writes=[cs.reg()])
            S.op("dve", lambda e: e.tensor_copy(out=csb.ap.bitcast(F32R), in_=cs.ap.unsqueeze(2).to_broadcast([128, 8, 128])),
                 reads=[cs.reg()], writes=[csb.reg()])
            wada_v = w_ada_d.rearrange("(p k) n -> p k n", k=8)
            for c in range(12):
                wb = wada[c % 2]
                dma_pool(wb.ap.bitcast(F32R), wada_v[:, :, c * 512:(c + 1) * 512], [], [wb.reg()], sem="wada%d" % (c % 2))
                bk = c % 2
                for k in range(8):
                    S.op("pe", lambda e, k=k, wb=wb, bk=bk: e.matmul(banks[bk][:], lhsT=csb.ap[:, k, :].bitcast(F32R),
                                                                   rhs=wb.ap[:, k, :].bitcast(F32R), start=(k == 0), stop=(k == 7)),
                         reads=[csb.reg(), wb.reg()], writes=[bank_reg(bk)])
                mi = c // 2
                cols = slice(c * 512, (c + 1) * 512)
                S.op("dve", lambda e, bk=bk, cols=cols: e.tensor_tensor(out=mods.ap[:, cols], in0=banks[bk][:], in1=bbc.ap[:, cols], op=ALU.add),
                     reads=[bank_reg(bk), bbc.reg()], writes=[mods.reg(c * 2048, (c + 1) * 2048)])
                if mi in (1, 4):
                    g = gmix if mi == 1 else gffn
                    gc = slice((c % 2) * 512, (c % 2) * 512 + 512)
                    S.op("dve", lambda e, cols=cols, g=g, gc=gc: e.scalar_tensor_tensor(out=mods.ap[:, cols], in0=mods.ap[:, cols], scalar=1.0,
                                                                                      in1=g.ap[:, gc], op0=ALU.add, op1=ALU.mult),
                         reads=[mods.reg(c * 2048, (c + 1) * 2048), g.reg()], writes=[mods.reg(c * 2048, (c + 1) * 2048)])
            dma_sp(mods_dram, mods.ap, [mods.reg()], [R("mods_dram")], sem="st")
            if debug:
                dma_sp(dbg["mods"], mods.ap, [mods.reg()], [R("dbg_mods")], sem="st")
            A.top = PERSIST_TOP

            checkpoint(1)
            modsB = A.get("modsB", 2048 * 4, F32)
            gqk = A.get("gqk", 26 * 64 * 4, F32, "p (h d) -> p h d", h=26)
            qTa = A.get("qTa", 4 * 2048 * 2, BF16, "p (i t) -> p i t", i=4)
            kTa = A.get("kTa", 2048 * 2, BF16)
            qTb = A.get("qTb", 4 * 2048 * 2, BF16, "p (i t) -> p i t", i=4)
            kTb = A.get("kTb", 4 * 2048 * 2, BF16, "p (i t) -> p i t", i=4)
            vnat = A.get("vnat", 16 * 650 * 2, BF16, "p (j h d) -> p j h d", j=16, h=10)
            BWIDE_TOP = A.top
            dma_sp(modsB.ap, mods_dram[:, 0:2048], [R("mods_dram")], [modsB.reg()])
            gq_tmp = A.get("gq_tmp", 1024, F32)
            dma_pool(gq_tmp.ap, gqk_d.partition_broadcast(128), [], [gq_tmp.reg()])
            for h in range(26):
                if h < 8:
                    src, sc = 0, 0.125
                elif h < 10:
                    src, sc = 1, 1.0
                elif h < 18:
                    src, sc = 2, 0.125
                else:
                    src, sc = 3, 1.0
                S.op("dve", lambda e, h=h, src=src, sc=sc: e.tensor_scalar(out=gqk.ap[:, h, :], in0=gq_tmp.ap[:, src * 64:src * 64 + 64],
                                                                            scalar1=sc, scalar2=None, op0=ALU.mult),
                     reads=[gq_tmp.reg()], writes=[gqk.reg()])
            S.op("pool", lambda e: e.memset(vnat.ap, 1.0), writes=[vnat.reg()])

            w_in = A.get("w_in", 8 * 2304 * 4, F32, "p (k n) -> p k n", k=8)
            xt = [A.get(f"xt{i}", 4096, F32) for i in range(1)]
            hbuf = A.get("h", 4096, F32)
            hT = [A.get(f"hT{i}", 4096, F32, "p (k t) -> p k t", k=8) for i in range(1)]
            sq = [A.get(f"sq{i}", 2048, F32) for i in range(2)]
            qkf = [A.get(f"qkf{i}", 2048, F32) for i in range(2)]
            qkb = A.get("qkb", 1664 * 2, BF16)
            rope_t = A.get("rope_t", 4 * 8 * 8 * 4, F32, "p (a h f) -> p a h f", a=4, h=8)
            st_small = A.get("st_small", 64 * 4, F32)
            dma_pool(w_in.ap.bitcast(F32R), w_in_d.rearrange("(p k) n -> p k n", k=8), [], [w_in.reg()], sem="win")

            def rmsnorm_mod(xin, xin_reg, out_ap, out_reg, a_ap, s_ap, mod_reg, scr, tagsem):
                ss = scr.ap[:, 0:1]
                S.op("act", lambda e: e.activation(out=junk.ap,
                                                   in_=xin, func=ACTF.Square, accum_out=ss),
                     reads=[xin_reg], writes=[junk.reg(), scr.reg()])
                S.op("dve", lambda e: e.tensor_scalar(out=scr.ap[:, 1:2], in0=ss, scalar1=1.0 / 1024, scalar2=EPS, op0=ALU.mult, op1=ALU.add),
                     reads=[scr.reg()], writes=[scr.reg()])
                S.op("pool", lambda e: e.tensor_tensor(out=scr.ap[:, 2:3], in0=scr.ap[:, 1:2], in1=neghalf.ap[:, 0:1], op=ALU.pow),
                     reads=[scr.reg(), neghalf.reg()], writes=[scr.reg()])
                S.op("dve", lambda e: e.scalar_tensor_tensor(out=out_ap, in0=xin, scalar=scr.ap[:, 2:3], in1=a_ap, op0=ALU.mult, op1=ALU.mult),
                     reads=[xin_reg, scr.reg(), mod_reg], writes=[out_reg])
                S.op("dve", lambda e: e.tensor_tensor(out=out_ap, in0=out_ap, in1=s_ap, op=ALU.add),
                     reads=[out_reg, mod_reg], writes=[out_reg])

            def transpose8(src_ap, src_reg, dst, bk0=0, bk1=1):
                v = src_ap.rearrange("t (p k) -> t k p", k=8)
                for k in range(8):
                    bk = bk0 if k < 4 else bk1
                    S.op("pe", lambda e, k=k, bk=bk: e.transpose(banks[bk][:, (k % 4) * 128:(k % 4) * 128 + 128], v[:, k, :], ident),
                         reads=[src_reg, R("consts")], writes=[bank_reg(bk, (k % 4) * 512, (k % 4) * 512 + 512)])
                S.op("act", lambda e: e.activation(out=dst.ap[:, 0:4, :].bitcast(F32R), in_=banks[bk0][:].rearrange("p (k t) -> p k t", k=4), func=ACTF.Copy),
                     reads=[bank_reg(bk0)], writes=[dst.reg(0, 2048)])
                S.op("act", lambda e: e.activation(out=dst.ap[:, 4:8, :].bitcast(F32R), in_=banks[bk1][:].rearrange("p (k t) -> p k t", k=4), func=ACTF.Copy),
                     reads=[bank_reg(bk1)], writes=[dst.reg(2048, 4096)])

            chunks = [(0, 512, 8, 0), (512, 256, 2, 8), (768, 512, 8, 10), (1280, 512, 8, 18), (1792, 512, 0, 0)]
            for j in range(NT):
                xb = xt[0]
                hTb = hT[0]
                dma_sp(xb.ap, x_d[j * 128:(j + 1) * 128, :], [], [xb.reg()], sem="x0")
                rmsnorm_mod(xb.ap, xb.reg(), hbuf.ap, hbuf.reg(), modsB.ap[:, 1024:2048], modsB.ap[:, 0:1024], modsB.reg(), st_small, "b1")
                transpose8(hbuf.ap, hbuf.reg(), hTb)
                for ci, (c0, ncol, nh, h0) in enumerate(chunks):
                    bk = 2 + ci
                    for k in range(8):
                        S.op("pe", lambda e, k=k, bk=bk, c0=c0, ncol=ncol, hTb=hTb: e.matmul(
                            banks[bk][:, 0:ncol], lhsT=hTb.ap[:, k, :].bitcast(F32R), rhs=w_in.ap[:, k, c0:c0 + ncol].bitcast(F32R),
                            start=(k == 0), stop=(k == 7)),
                            reads=[hTb.reg(), w_in.reg()], writes=[bank_reg(bk)])
                for ci, (c0, ncol, nh, h0) in enumerate(chunks):
                    bk = 2 + ci
                    if nh > 0:
                        sqb = sq[ci % 2]
                        qf = qkf[ci % 2]
                        nq = nh * 64
                        S.op("act", lambda e, bk=bk, sqb=sqb, nq=nq: e.activation(out=sqb.ap[:, 0:nq], in_=banks[bk][:, 0:nq], func=ACTF.Square),
                             reads=[bank_reg(bk)], writes=[sqb.reg()])
                        ssum = st_small.ap[:, 8:8 + nh]
                        S.op("dve", lambda e, sqb=sqb, nh=nh, nq=nq, ssum=ssum: e.tensor_reduce(out=ssum, in_=sqb.ap[:, 0:nq].rearrange("p (h d) -> p h d", h=nh),
                                                                                             axis=AX.X, op=ALU.add),
                             reads=[sqb.reg()], writes=[st_small.reg()])
                        S.op("dve", lambda e, ssum=ssum: e.tensor_scalar(out=ssum, in0=ssum, scalar1=1.0 / 64, scalar2=EPS, op0=ALU.mult, op1=ALU.add),
                             reads=[st_small.reg()], writes=[st_small.reg()])
                        rstd = st_small.ap[:, 16:16 + nh]
                        S.op("pool", lambda e, ssum=ssum, rstd=rstd, nh=nh: e.tensor_tensor(out=rstd, in0=ssum, in1=neghalf.ap[:, 0:nh], op=ALU.pow),
                             reads=[st_small.reg(), neghalf.reg()], writes=[st_small.reg()])
                        qv = qf.ap[:, 0:nq].rearrange("p (h d) -> p h d", h=nh)
                        S.op("dve", lambda e, bk=bk, qv=qv, rstd=rstd, nh=nh, nq=nq: e.tensor_tensor(
                            out=qv, in0=banks[bk][:, 0:nq].rearrange("p (h d) -> p h d", h=nh),
                            in1=rstd.unsqueeze(2).to_broadcast([128, nh, 64]), op=ALU.mult),
                            reads=[bank_reg(bk), st_small.reg()], writes=[qf.reg()])
                        S.op("dve", lambda e, qv=qv, h0=h0, nh=nh: e.tensor_tensor(out=qv, in0=qv, in1=gqk.ap[:, h0:h0 + nh, :], op=ALU.mult),
                             reads=[qf.reg(), gqk.reg()], writes=[qf.reg()])
                        cb = cosb.ap[:, j, :].unsqueeze(1).to_broadcast([128, nh, 8])
                        sb_ = sinb.ap[:, j, :].unsqueeze(1).to_broadcast([128, nh, 8])
                        t1 = qv[:, :, 0:8]
                        t2 = qv[:, :, 8:16]
                        ra, rb_, rc, rd = (rope_t.ap[:, a, 0:nh, :] for a in range(4))
                        for (o_, i0, i1) in ((ra, t1, cb), (rb_, t2, sb_), (rc, t2, cb), (rd, t1, sb_)):
                            S.op("dve", lambda e, o_=o_, i0=i0, i1=i1: e.tensor_tensor(out=o_, in0=i0, in1=i1, op=ALU.mult),
                                 reads=[qf.reg(), cosb.reg(), sinb.reg()], writes=[rope_t.reg()])
                        S.op("dve", lambda e, t1=t1, ra=ra, rb_=rb_: e.tensor_tensor(out=t1, in0=ra, in1=rb_, op=ALU.subtract),
                             reads=[rope_t.reg()], writes=[qf.reg()])
                        S.op("dve", lambda e, t2=t2, rc=rc, rd=rd: e.tensor_tensor(out=t2, in0=rc, in1=rd, op=ALU.add),
                             reads=[rope_t.reg()], writes=[qf.reg()])
                        qoff = {0: 0, 1: 512, 2: 640, 3: 1152}[ci]
                        if ci == 0:
                            S.op("act", lambda e, qf=qf: e.activation(out=qkb.ap[:, 0:512].rearrange("p (i g d) -> p g i d", i=4, g=2),
                                                                      in_=qf.ap[:, 0:512].rearrange("p (g i d) -> p g i d", g=2, i=4), func=ACTF.Copy),
                                 reads=[qf.reg()], writes=[qkb.reg(0, 1024)])
                        else:
                            S.op("act", lambda e, qf=qf, nq=nq, qoff=qoff: e.activation(out=qkb.ap[:, qoff:qoff + nq], in_=qf.ap[:, 0:nq], func=ACTF.Copy),
                                 reads=[qf.reg()], writes=[qkb.reg(qoff * 2, (qoff + nq) * 2)])
                    if ci == 1:
                        S.op("act", lambda e, bk=bk, j=j: e.activation(out=vnat.ap[:, j, 0:2, 0:64], in_=banks[bk][:, 128:256].rearrange("p (h d) -> p h d", h=2), func=ACTF.Copy),
                             reads=[bank_reg(bk)], writes=[vnat.reg(j * 1300, j * 1300 + 260)])
                    if ci == 4:
                        S.op("act", lambda e, bk=bk, j=j: e.activation(out=vnat.ap[:, j, 2:10, 0:64], in_=banks[bk][:, 0:512].rearrange("p (h d) -> p h d", h=8), func=ACTF.Copy),
                             reads=[bank_reg(bk)], writes=[vnat.reg(j * 1300 + 260, (j + 1) * 1300)])
                        dma_sp(v_dram[j * 128:(j + 1) * 128, :], vnat.ap[:, j, 2:10, :].rearrange("p h d -> p (h d)"),
                               [vnat.reg(j * 1300 + 260, (j + 1) * 1300)], [R("v_dram", j, j + 1), R("vnat_chain")], sem="st_vnat")
                if debug:
                    dma_sp(dbg["qk"][j * 128:(j + 1) * 128, :], qkb.ap, [qkb.reg()], [R("dbg_qk", j, j + 1)], sem="st")
                    dma_sp(dbg["v"][j * 128:(j + 1) * 128, :], vnat.ap[:, j, :, :].rearrange("p h d -> p (h d)"), [vnat.reg(j * 1300, (j + 1) * 1300)], [R("dbg_v", j, j + 1)], sem="st_dbgv")
                pb7 = banks[7][:].bitcast(BF16).rearrange("p (s t) -> p s t", s=8)
                pb0 = banks[0][:].bitcast(BF16).rearrange("p (s t) -> p s t", s=8)
                tsl = slice(j * 128, (j + 1) * 128)
                for i in range(4):
                    S.op("pe", lambda e, i=i: e.transpose(pb7[:, i, :], qkb.ap[:, i * 128:(i + 1) * 128], ident_b.ap),
                         reads=[qkb.reg(0, 1024), ident_b.reg()], writes=[bank_reg(7, i * 256, i * 256 + 256)])
                for i in range(4):
                    S.op("pe", lambda e, i=i: e.transpose(pb7[:, 4 + i, :], qkb.ap[:, 640 + i * 128:640 + i * 128 + 128], ident_b.ap),
                         reads=[qkb.reg(1280, 2304), ident_b.reg()], writes=[bank_reg(7, (4 + i) * 256, (4 + i) * 256 + 256)])
                S.op("act", lambda e, tsl=tsl: e.activation(out=qTa.ap[:, :, tsl], in_=pb7[:, 0:4, :], func=ACTF.Copy),
                     reads=[bank_reg(7)], writes=[qTa.reg()])
                S.op("dve", lambda e, tsl=tsl: e.tensor_copy(out=qTb.ap[:, :, tsl], in_=pb7[:, 4:8, :]),
                     reads=[bank_reg(7)], writes=[qTb.reg()])
                for i in range(4):
                    S.op("pe", lambda e, i=i: e.transpose(pb7[:, i, :], qkb.ap[:, 1152 + i * 128:1152 + i * 128 + 128], ident_b.ap),
                         reads=[qkb.reg(2304, 3328), ident_b.reg()], writes=[bank_reg(7, i * 256, i * 256 + 256)])
                S.op("pe", lambda e: e.transpose(pb7[:, 4, :], qkb.ap[:, 512:640], ident_b.ap),
                     reads=[qkb.reg(1024, 1280), ident_b.reg()], writes=[bank_reg(7, 1024, 1280)])
                S.op("act", lambda e, tsl=tsl: e.activation(out=kTb.ap[:, :, tsl], in_=pb7[:, 0:4, :], func=ACTF.Copy),
                     reads=[bank_reg(7)], writes=[kTb.reg()])
                S.op("dve", lambda e, tsl=tsl: e.tensor_copy(out=kTa.ap[:, tsl], in_=pb7[:, 4, :]),
                     reads=[bank_reg(7)], writes=[kTa.reg()])
            A.top = BWIDE_TOP
            if debug:
                dma_sp(dbg["kTa"], kTa.ap, [kTa.reg()], [R("dbg_kTa")], sem="st_d1")
                dma_sp(dbg["kTb"], kTb.ap.rearrange("p i t -> p (i t)"), [kTb.reg()], [R("dbg_kTb")], sem="st_d2")
                dma_sp(dbg["qTa"], qTa.ap.rearrange("p i t -> p (i t)"), [qTa.reg()], [R("dbg_qTa")], sem="st_d3")
                dma_sp(dbg["qTb"], qTb.ap.rearrange("p i t -> p (i t)"), [qTb.reg()], [R("dbg_qTb")], sem="st_d4")

            checkpoint(2)
            w_out = A.get("w_out", 8 * 1024 * 4, F32, "p (k n) -> p k n", k=8)
            v4 = A.get("v4", 16 * 520 * 2, BF16, "p (j h d) -> p j h d", j=16, h=8)
            v16 = A.get("v16", 16 * 520 * 2, BF16, "p (j h d) -> p j h d", j=16, h=8)
            modg = A.get("modg", 4096, F32)
            goutb = A.get("goutb", 4096, F32)
            pT = [A.get(f"pT{i}", 512 * 2, BF16) for i in range(4)]
            osb = [A.get(f"osb{i}", 520 * 4, F32, "p (h d) -> p h d", h=8) for i in range(2)]
            o4n = A.get("o4n", 520 * 4, F32, "p (h d) -> p h d", h=8)
            o16n = A.get("o16n", 520 * 4, F32, "p (h d) -> p h d", h=8)
            ocat = A.get("ocat", 4096, F32)
            ocT = A.get("ocT", 4096, F32, "p (k t) -> p k t", k=8)
            xb2 = A.get("xb2", 4096, F32)
            x1b = ocat
            att_s = A.get("att_s", 64 * 4, F32)
            dma_pool(w_out.ap.bitcast(F32R), w_out_d.rearrange("(p k) n -> p k n", k=8), [], [w_out.reg()], sem="wout")
            dma_sp(modg.ap, mods_dram[:, 2048:3072], [R("mods_dram")], [modg.reg()])
            dma_pool(goutb.ap, gout_d.partition_broadcast(128), [], [goutb.reg()])
            dma_pool(esink.ap, sinks_d.partition_broadcast(128), [], [esink.reg()])
            S.op("act", lambda e: e.activation(out=esink.ap, in_=esink.ap, func=ACTF.Exp), reads=[esink.reg()], writes=[esink.reg()])
            v4d = v_dram.rearrange("(i d) c -> d i c", d=4)
            v16d = v_dram.rearrange("(i d) c -> d i c", d=16)
            for r in range(4):
                for m in range(4):
                    dma_sp(v4.ap[:, r * 4 + m, :, :].rearrange("p h d -> p (h d)"), v4d[r, m * 128:(m + 1) * 128, :],
                           [R("v_dram", 0, 16)], [v4.reg((r * 4 + m) * 1040, (r * 4 + m + 1) * 1040)], sem="l_v4")
            for r in range(16):
                dma_sp(v16.ap[:, r, :, :].rearrange("p h d -> p (h d)"), v16d[r, 0:128, :],
                       [R("v_dram", 0, 16)], [v16.reg(r * 1040, (r + 1) * 1040)], sem="l_v16")

            pcount = [0]

            def attn_b_tile(q_sel, k_sels, vbuf, vidx, out_banks):
                nk = len(k_sels)
                for hp2 in range(2):
                    pts = [pT[(pcount[0]) % 4], pT[(pcount[0] + 1) % 4]]
                    pcount[0] += 2
                    blocks = []
                    for hp in (2 * hp2, 2 * hp2 + 1):
                        for (ks, vt, isprev) in k_sels:
                            blocks.append((hp, ks, vt))
                    nb = len(blocks)
                    for s in range(2):
                        sbk = s
                        pt = pts[s]
                        for bi, (hp, ks, vt) in enumerate(blocks):
                            S.op("pe", lambda e, s=s, ks=ks, hp=hp, bi=bi, sbk=sbk: e.matmul(
                                banks[sbk][:, bi * 128:(bi + 1) * 128], lhsT=kTb.ap[64 * s:64 * s + 64, hp, ks],
                                rhs=qTb.ap[64 * s:64 * s + 64, hp, q_sel], start=True, stop=True),
                                reads=[kTb.reg(), qTb.reg()], writes=[bank_reg(sbk, bi * 512, (bi + 1) * 512)])
                        S.op("act", lambda e, pt=pt, sbk=sbk, nb=nb: e.activation(out=pt.ap[:, 0:nb * 128], in_=banks[sbk][:, 0:nb * 128], func=ACTF.Exp),
                             reads=[bank_reg(sbk, 0, nb * 512)], writes=[pt.reg()])
                        if nk == 2:
                            S.op("dve", lambda e, pt=pt: e.tensor_tensor(out=pt.ap[:, 0:512], in0=pt.ap[:, 0:512],
                                                                        in1=maskB4.ap[:, :, :].rearrange("p b k -> p (b k)"), op=ALU.mult),
                                 reads=[pt.reg(), maskB4.reg()], writes=[pt.reg()])
                        else:
                            S.op("dve", lambda e, pt=pt, nb=nb: e.tensor_tensor(out=pt.ap[:, 0:nb * 128].rearrange("p (b k) -> p b k", b=nb),
                                                                               in0=pt.ap[:, 0:nb * 128].rearrange("p (b k) -> p b k", b=nb),
                                                                               in1=masks_b.ap[:, 0:1, :].to_broadcast([128, nb, 128]), op=ALU.mult),
                                 reads=[pt.reg(), masks_b.reg()], writes=[pt.reg()])
                    for s in range(2):
                        pt = pts[s]
                        for hp in (2 * hp2, 2 * hp2 + 1):
                            hh = 2 * hp + s
                            ob = out_banks[hh // 4]
                            col = (hh % 4) * 65
                            mine = [(bi, b) for bi, b in enumerate(blocks) if b[0] == hp]
                            for n_, (bi, (hp_, ks, vt)) in enumerate(mine):
                                S.op("pe", lambda e, pt=pt, bi=bi, vt=vt, hh=hh, ob=ob, col=col, n_=n_, last=(n_ == len(mine) - 1): e.matmul(
                                    banks[ob][:, col:col + 65], lhsT=pt.ap[:, bi * 128:(bi + 1) * 128], rhs=vbuf.ap[:, vt, vidx + hh, :],
                                    start=(n_ == 0), stop=last),
                                    reads=[pt.reg(), vbuf.reg()], writes=[bank_reg(ob, col * 4, col * 4 + 260)])

            o4d = o4_dram.rearrange("(i d) c -> d i c", d=4)
            o16d = o16_dram.rearrange("(i d) c -> d i c", d=16)
            oi = [0]

            def evac_store(dst_ap, dst_reg):
                ob = osb[oi[0] % 2]
                oi[0] += 1
                S.op("act", lambda e, ob=ob: e.activation(out=ob.ap[:, 0:4, :], in_=banks[2][:, 0:260].rearrange("p (h d) -> p h d", h=4), func=ACTF.Copy),
                     reads=[bank_reg(2)], writes=[ob.reg(0, 1040)])
                S.op("dve", lambda e, ob=ob: e.tensor_copy(out=ob.ap[:, 4:8, :], in_=banks[3][:, 0:260].rearrange("p (h d) -> p h d", h=4)),
                     reads=[bank_reg(3)], writes=[ob.reg(1040, 2080)])
                dma_sp(dst_ap, ob.ap.rearrange("p h d -> p (h d)"), [ob.reg()], [dst_reg], sem="st")

            for r in range(4):
                for m in range(4):
                    def sel(mm, r=r):
                        st_ = r + 512 * mm
                        return slice(st_, st_ + 4 * 127 + 1, 4)
                    ks = []
                    if m > 0:
                        ks.append((sel(m - 1), r * 4 + m - 1, True))
                    ks.append((sel(m), r * 4 + m, False))
                    attn_b_tile(sel(m), ks, v4, 0, (2, 3))
                    evac_store(o4d[r, m * 128:(m + 1) * 128, :], R("o4_dram", r * 4 + m, r * 4 + m + 1))
            for r in range(16):
                sl = slice(r, r + 16 * 127 + 1, 16)
                attn_b_tile(sl, [(sl, r, False)], v16, 0, (2, 3))
                evac_store(o16d[r, 0:128, :], R("o16_dram", r, r + 1))

            checkpoint(3)
            for j in range(NT):
                tsl = slice(j * 128, (j + 1) * 128)
                dma_sp(xb2.ap, x_d[tsl, :], [], [xb2.reg()], sem="x2")
                dma_sp(o4n.ap.rearrange("p h d -> p (h d)"), o4_dram[tsl, :], [R("o4_dram", 0, 16)], [o4n.reg()], sem="on")
                dma_sp(o16n.ap.rearrange("p h d -> p (h d)"), o16_dram[tsl, :], [R("o16_dram", 0, 16)], [o16n.reg()], sem="on")
                for kvh in range(2):
                    jks = ([j - 1] if j > 0 else []) + [j]
                    pa = pT[2 * kvh].ap if False else None
                    pt0 = pT[(pcount[0]) % 4]
                    pt1 = pT[(pcount[0] + 1) % 4]
                    pcount[0] += 2
                    pts = [pt0, pt1]
                    for n_, jk in enumerate(jks):
                        sbk = 4 + n_
                        S.op("pe", lambda e, kvh=kvh, jk=jk, sbk=sbk, tsl=tsl: e.matmul(
                            banks[sbk][:].rearrange("p (i t) -> p i t", i=4), lhsT=kTa.ap[64 * kvh:64 * kvh + 64, jk * 128:(jk + 1) * 128],
                            rhs=qTa.ap[64 * kvh:64 * kvh + 64, :, tsl], start=True, stop=True),
                            reads=[kTa.reg(), qTa.reg()], writes=[bank_reg(sbk)])
                        pt = pts[n_]
                        S.op("act", lambda e, pt=pt, sbk=sbk: e.activation(out=pt.ap, in_=banks[sbk][:], func=ACTF.Exp),
                             reads=[bank_reg(sbk)], writes=[pt.reg()])
                        midx = 1 if jk == j else 0
                        S.op("dve", lambda e, pt=pt, midx=midx: e.tensor_tensor(out=pt.ap, in0=pt.ap, in1=maskA2.ap[:, midx, :, :].rearrange("p h k -> p (h k)"), op=ALU.mult),
                             reads=[pt.reg(), maskA2.reg()], writes=[pt.reg()])
                    ob = 6 + kvh
                    for i in range(4):
                        for n_, jk in enumerate(jks):
                            pt = pts[n_]
                            S.op("pe", lambda e, pt=pt, i=i, jk=jk, kvh=kvh, ob=ob, n_=n_, last=(n_ == len(jks) - 1): e.matmul(
                                banks[ob][:, i * 65:(i + 1) * 65], lhsT=pt.ap[:, i * 128:(i + 1) * 128], rhs=vnat.ap[:, jk, kvh, :],
                                start=(n_ == 0), stop=last),
                                reads=[pt.reg(), vnat.reg()], writes=[bank_reg(ob, i * 260, (i + 1) * 260)])
                ks = ([(slice((j - 1) * 128, j * 128), j - 1, True)] if j > 0 else []) + [(tsl, j, False)]
                if j == 0:
                    attn_b_tile(tsl, ks, vnat, 2, (2, 3))
                else:
                    attn_b_tile(tsl, ks, vnat, 2, (2, 3))
                den = att_s.ap[:, 0:8]
                for kvh in range(2):
                    S.op("dve", lambda e, kvh=kvh: e.tensor_tensor(out=att_s.ap[:, kvh * 4:kvh * 4 + 4], in0=banks[6 + kvh][:, 0:260].rearrange("p (h d) -> p h d", h=4)[:, :, 64],
                                                                  in1=esink.ap[:, kvh * 4:kvh * 4 + 4], op=ALU.add),
                         reads=[bank_reg(6 + kvh), esink.reg()], writes=[att_s.reg()])
                S.op("dve", lambda e: e.reciprocal(out=att_s.ap[:, 8:16], in_=att_s.ap[:, 0:8]), reads=[att_s.reg()], writes=[att_s.reg()])
                for kvh in range(2):
                    S.op("dve", lambda e, kvh=kvh: e.tensor_tensor(out=ocat.ap[:, kvh * 256:(kvh + 1) * 256].rearrange("p (h d) -> p h d", h=4),
                                                                  in0=banks[6 + kvh][:, 0:260].rearrange("p (h d) -> p h d", h=4)[:, :, 0:64],
                                                                  in1=att_s.ap[:, 8 + kvh * 4:12 + kvh * 4].unsqueeze(2).to_broadcast([128, 4, 64]), op=ALU.mult),
                         reads=[bank_reg(6 + kvh), att_s.reg()], writes=[ocat.reg(kvh * 1024, (kvh + 1) * 1024)])
                S.op("dve", lambda e: e.tensor_tensor(out=o4n.ap, in0=o4n.ap, in1=o16n.ap, op=ALU.add), reads=[o4n.reg(), o16n.reg()], writes=[o4n.reg()])
                for half in range(2):
                    S.op("dve", lambda e, half=half: e.tensor_tensor(out=o4n.ap[:, half * 4:half * 4 + 4, :], in0=o4n.ap[:, half * 4:half * 4 + 4, :],
                                                                    in1=banks[2 + half][:, 0:260].rearrange("p (h d) -> p h d", h=4), op=ALU.add),
                         reads=[o4n.reg(), bank_reg(2 + half)], writes=[o4n.reg()])
                S.op("dve", lambda e: e.reciprocal(out=att_s.ap[:, 16:24], in_=o4n.ap[:, :, 64]), reads=[o4n.reg()], writes=[att_s.reg()])
                S.op("dve", lambda e: e.tensor_tensor(out=ocat.ap[:, 512:1024].rearrange("p (h d) -> p h d", h=8), in0=o4n.ap[:, :, 0:64],
                                                      in1=att_s.ap[:, 16:24].unsqueeze(2).to_broadcast([128, 8, 64]), op=ALU.mult),
                     reads=[o4n.reg(), att_s.reg()], writes=[ocat.reg(2048, 4096)])
                for gi in range(2):
                    gsl = slice(gi * 512, (gi + 1) * 512)
                    S.op("act", lambda e, gsl=gsl, gi=gi: e.activation(out=junk.ap[:, 0:512], in_=ocat.ap[:, gsl], func=ACTF.Square, accum_out=att_s.ap[:, 24 + gi:25 + gi]),
                         reads=[ocat.reg(gi * 2048, (gi + 1) * 2048)], writes=[junk.reg(), att_s.reg()])
                S.op("dve", lambda e: e.tensor_scalar(out=att_s.ap[:, 26:28], in0=att_s.ap[:, 24:26], scalar1=1.0 / 512, scalar2=EPS, op0=ALU.mult, op1=ALU.add),
                     reads=[att_s.reg()], writes=[att_s.reg()])
                S.op("pool", lambda e: e.tensor_tensor(out=att_s.ap[:, 28:30], in0=att_s.ap[:, 26:28], in1=neghalf.ap[:, 0:2], op=ALU.pow),
                     reads=[att_s.reg(), neghalf.reg()], writes=[att_s.reg()])
                for gi in range(2):
                    gsl = slice(gi * 512, (gi + 1) * 512)
                    S.op("dve", lambda e, gsl=gsl, gi=gi: e.scalar_tensor_tensor(out=ocat.ap[:, gsl], in0=ocat.ap[:, gsl], scalar=att_s.ap[:, 28 + gi:29 + gi],
                                                                               in1=goutb.ap[:, gsl], op0=ALU.mult, op1=ALU.mult),
                         reads=[ocat.reg(gi * 2048, (gi + 1) * 2048), att_s.reg(), goutb.reg()], writes=[ocat.reg(gi * 2048, (gi + 1) * 2048)])
                if debug:
                    dma_sp(dbg["ocat"][tsl, :], ocat.ap, [ocat.reg()], [R("dbg_ocat", j, j + 1)], sem="st")
                transpose8(ocat.ap, ocat.reg(), ocT, 0, 1)
                for nh_ in range(2):
                    bk = 4 + nh_
                    for k in range(8):
                        S.op("pe", lambda e, k=k, bk=bk, nh_=nh_: e.matmul(banks[bk][:], lhsT=ocT.ap[:, k, :].bitcast(F32R),
                                                                         rhs=w_out.ap[:, k, nh_ * 512:(nh_ + 1) * 512].bitcast(F32R), start=(k == 0), stop=(k == 7)),
                             reads=[ocT.reg(), w_out.reg()], writes=[bank_reg(bk)])
                for nh_ in range(2):
                    csl = slice(nh_ * 512, (nh_ + 1) * 512)
                    S.op("dve", lambda e, nh_=nh_, csl=csl: e.tensor_tensor(out=x1b.ap[:, csl], in0=banks[4 + nh_][:], in1=modg.ap[:, csl], op=ALU.mult),
                         reads=[bank_reg(4 + nh_), modg.reg()], writes=[x1b.reg(nh_ * 2048, (nh_ + 1) * 2048)])
                S.op("dve", lambda e: e.tensor_tensor(out=x1b.ap, in0=x1b.ap, in1=xb2.ap, op=ALU.add), reads=[x1b.reg(), xb2.reg()], writes=[x1b.reg()])
                dma_sp(x1_dram[tsl, :], x1b.ap, [x1b.reg()], [R("x1_dram", j, j + 1)], sem="st")
                if debug:
                    dma_sp(dbg["x1"][tsl, :], x1b.ap, [x1b.reg()], [R("dbg_x1", j, j + 1)], sem="st")
            A.top = PERSIST_TOP

            checkpoint(4)
            modsC = A.get("modsC", 3072 * 4, F32)
            widx = A.get("widx", NOV * 4, I32)
            CKEEP = A.top
            wr = A.get("wr", 8 * 256 * 4, F32, "p (k n) -> p k n", k=8)
            wgus = A.get("wgus", 8 * 512 * 4, F32, "p (k n) -> p k n", k=8)
            wds = A.get("wds", 2 * 1024 * 4, F32, "p (c n) -> p c n", c=2)
            rbias = A.get("rbias", 1024, F32)
            mcum = A.get("mcum", 1024, F32)
            x1t = [A.get(f"x1t{i}", 4096, F32) for i in range(2)]
            h2 = A.get("h2", 4096, F32)
            h2T = A.get("h2T", 4096, F32, "p (k t) -> p k t", k=8)
            sc2 = [A.get(f"sc{i}", 1024, F32) for i in range(2)]
            rs2 = A.get("rs2", 64, F32)
            junk2 = A.get("junk2", 2048, F32)
            bi_ = A.get("bi", 1024, F32)
            msk = A.get("msk", 1024, F32)
            eqt = A.get("eqt", 1024, F32)
            offm = A.get("offm", 1024, F32)
            rs = A.get("rs", 128 * 4, F32)
            off8f = A.get("off8f", 32, F32)
            sc8 = A.get("sc8", 32, F32)
            mx8 = A.get("mx8", 32, F32)
            idx8 = A.get("idx8", 32, U32)
            idx8f = A.get("idx8f", 32, F32)
            shs = A.get("shs", 1024, F32)
            shh = A.get("shh", 1024, F32)
            shT = A.get("shT", 1024, F32, "p (c t) -> p c t", c=2)
            baseb = A.get("baseb", 4096, F32)
            zrow = A.get("zrow", 4096, F32)
            listinit = A.get("listinit", (NE + NOV) * 2 * 4, I32)
            posall = A.get("posall", 16 * 256 * 4, F32, "p (j e) -> p j e", j=16)
            idxall = A.get("idxall", 16 * 8 * 4, F32, "p (j k) -> p j k", j=16)
            ovT = A.get("ovT", 32, F32)
            rankT = A.get("rankT", 32, F32)
            diagR = A.get("diagR", 512, F32)
            ovb = A.get("ovb", 1024, F32)
            diffr = A.get("diffr", 1024, F32)
            selT = A.get("selT", 2 * NOV * 4, F32, "p (c o) -> p c o", c=2)
            widxf = A.get("widxf", NOV * 4, F32)
            CWIDE = A.top
            dma_sp(modsC.ap, mods_dram[:, 3072:6144], [R("mods_dram")], [modsC.reg()])
            dma_sp(wr.ap, wr_d.rearrange("(p k) n -> p k n", k=8), [], [wr.reg()])
            dma_pool(wgus.ap[:, :, 0:256].bitcast(F32R), wgs_d.rearrange("(p k) n -> p k n", k=8), [], [wgus.reg()], sem="wgus")
            dma_pool(wgus.ap[:, :, 256:512].bitcast(F32R), wus_d.rearrange("(p k) n -> p k n", k=8), [], [wgus.reg()], sem="wgus")
            dma_pool(wds.ap.bitcast(F32R), wds_d.rearrange("(p c) n -> p c n", c=2), [], [wds.reg()], sem="wds")
            dma_pool(rbias.ap, rb_d.partition_broadcast(128), [], [rbias.reg()])
            S.op("pool", lambda e: e.memset(mcum.ap, 0.0), writes=[mcum.reg()])
            S.op("pool", lambda e: e.memset(zrow.ap, 0.0), writes=[zrow.reg()])
            dma_sp(h2_dram[2048:2049, :], zrow.ap[0:1, :], [zrow.reg()], [R("h2_dram", 16, 17)], sem="st")
            S.op("pool", lambda e: e.memset(listinit.ap, 2049), writes=[listinit.reg()])
            dma_sp(list_dram.rearrange("(p n) o -> p (n o)", p=128), listinit.ap[:, 0:(NE + NOV) * 2], [listinit.reg()], [R("list_dram", 0, 1000)], sem="st_listinit")

            NGRP = 8
            def c1_A1(j):
                tsl = slice(j * 128, (j + 1) * 128)
                xb = x1t[j % 2]
                scj = sc2[j % 2]
                dma_sp(xb.ap, x1_dram[tsl, :], [R("x1_dram", j, j + 1)], [xb.reg()], sem="x1%d" % (j % 2))
                rmsnorm_mod(xb.ap, xb.reg(), h2.ap, h2.reg(), modsC.ap[:, 1024:2048], modsC.ap[:, 0:1024], modsC.reg(), rs2, "c1")
                dma_sp(h2_dram[tsl, :], h2.ap, [h2.reg()], [R("h2_dram", j, j + 1)], sem="st")
                if debug:
                    dma_sp(dbg["h2"][tsl, :], h2.ap, [h2.reg()], [R("dbg_h2", j, j + 1)], sem="st")
                transpose8(h2.ap, h2.reg(), h2T, 0, 1)
                for k in range(8):
                    S.op("pe", lambda e, k=k: e.matmul(banks[2][:, 0:256], lhsT=h2T.ap[:, k, :], rhs=wr.ap[:, k, :], start=(k == 0), stop=(k == 7)),
                         reads=[h2T.reg(), wr.reg()], writes=[bank_reg(2)])
                for k in range(8):
                    S.op("pe", lambda e, k=k: e.matmul(banks[4][:], lhsT=h2T.ap[:, k, :].bitcast(F32R), rhs=wgus.ap[:, k, :].bitcast(F32R), start=(k == 0), stop=(k == 7)),
                         reads=[h2T.reg(), wgus.reg()], writes=[bank_reg(4)])
                S.op("act", lambda e, scj=scj: e.activation(out=scj.ap, in_=banks[2][:, 0:256], func=ACTF.Sigmoid), reads=[bank_reg(2)], writes=[scj.reg()])
                S.op("act", lambda e: e.activation(out=shs.ap, in_=banks[4][:, 0:256], func=ACTF.Silu), reads=[bank_reg(4)], writes=[shs.reg()])
            def c1_A2(j):
                tsl = slice(j * 128, (j + 1) * 128)
                xb = x1t[j % 2]
                scj = sc2[j % 2]
                S.op("dve", lambda e: e.tensor_tensor(out=shh.ap, in0=shs.ap, in1=banks[4][:, 256:512], op=ALU.mult), reads=[shs.reg(), bank_reg(4)], writes=[shh.reg()])
                shv = shh.ap.rearrange("t (p c) -> t c p", c=2)
                for c in range(2):
                    S.op("pe", lambda e, c=c: e.transpose(banks[5][:, c * 128:(c + 1) * 128], shv[:, c, :], ident),
                         reads=[shh.reg(), R("consts")], writes=[bank_reg(5, c * 512, c * 512 + 512)])
                S.op("act", lambda e: e.activation(out=shT.ap.bitcast(F32R), in_=banks[5][:, 0:256].rearrange("p (c t) -> p c t", c=2), func=ACTF.Copy),
                     reads=[bank_reg(5)], writes=[shT.reg()])
                for nh_ in range(2):
                    for c in range(2):
                        S.op("pe", lambda e, c=c, nh_=nh_: e.matmul(banks[6 + nh_][:], lhsT=shT.ap[:, c, :].bitcast(F32R),
                                                                  rhs=wds.ap[:, c, nh_ * 512:(nh_ + 1) * 512].bitcast(F32R), start=(c == 0), stop=(c == 1)),
                             reads=[shT.reg(), wds.reg()], writes=[bank_reg(6 + nh_)])
                for nh_ in range(2):
                    csl = slice(nh_ * 512, (nh_ + 1) * 512)
                    S.op("dve", lambda e, nh_=nh_, csl=csl: e.tensor_tensor(out=baseb.ap[:, csl], in0=banks[6 + nh_][:], in1=modsC.ap[:, 2048 + nh_ * 512:2048 + (nh_ + 1) * 512], op=ALU.mult),
                         reads=[bank_reg(6 + nh_), modsC.reg()], writes=[baseb.reg(nh_ * 2048, (nh_ + 1) * 2048)])
                S.op("dve", lambda e, xb=xb: e.tensor_tensor(out=baseb.ap, in0=baseb.ap, in1=xb.ap, op=ALU.add), reads=[baseb.reg(), xb.reg()], writes=[baseb.reg()])
                dma_sp(base_dram[tsl, :], baseb.ap, [baseb.reg()], [R("base_dram", j, j + 1)], sem="st")
                if debug:
                    dma_sp(dbg["base"][tsl, :], baseb.ap, [baseb.reg()], [R("dbg_base", j, j + 1)], sem="st")
            def c1_B(j):
                tsl = slice(j * 128, (j + 1) * 128)
                xb = x1t[j % 2]
                scj = sc2[j % 2]
                S.op("dve", lambda e, scj=scj: e.tensor_tensor(out=bi_.ap, in0=scj.ap, in1=rbias.ap, op=ALU.add), reads=[scj.reg(), rbias.reg()], writes=[bi_.reg()])
                bv = bi_.ap.rearrange("p (g e) -> p g e", g=NGRP)
                m1 = rs.ap[:, 0:8]
                m2 = rs.ap[:, 8:16]
                S.op("dve", lambda e: e.tensor_reduce(out=m1, in_=bv, axis=AX.X, op=ALU.max), reads=[bi_.reg()], writes=[rs.reg()])
                ev = eqt.ap.rearrange("p (g e) -> p g e", g=NGRP)
                S.op("dve", lambda e: e.tensor_tensor(out=ev, in0=bv, in1=m1.unsqueeze(2).to_broadcast([128, 8, 32]), op=ALU.is_equal),
                     reads=[bi_.reg(), rs.reg()], writes=[eqt.reg()])
                S.op("dve", lambda e: e.scalar_tensor_tensor(out=eqt.ap, in0=eqt.ap, scalar=-1e9, in1=bi_.ap, op0=ALU.mult, op1=ALU.add),
                     reads=[eqt.reg(), bi_.reg()], writes=[eqt.reg()])
                S.op("dve", lambda e: e.tensor_reduce(out=m2, in_=ev, axis=AX.X, op=ALU.max), reads=[eqt.reg()], writes=[rs.reg()])
                gs = rs.ap[:, 16:24]
                S.op("dve", lambda e: e.tensor_tensor(out=gs, in0=m1, in1=m2, op=ALU.add), reads=[rs.reg()], writes=[rs.reg()])
                cmp = rs.ap[:, 32:96].rearrange("p (a b) -> p a b", a=8)
                S.op("dve", lambda e: e.tensor_tensor(out=cmp, in0=gs.unsqueeze(1).to_broadcast([128, 8, 8]), in1=gs.unsqueeze(2).to_broadcast([128, 8, 8]), op=ALU.is_gt),
                     reads=[rs.reg()], writes=[rs.reg()])
                cntg = rs.ap[:, 24:32]
                S.op("dve", lambda e: e.tensor_reduce(out=cntg, in_=cmp, axis=AX.X, op=ALU.add), reads=[rs.reg()], writes=[rs.reg()])
                S.op("dve", lambda e: e.tensor_scalar(out=cntg, in0=cntg, scalar1=3.5, scalar2=-1e9, op0=ALU.is_gt, op1=ALU.mult),
                     reads=[rs.reg()], writes=[rs.reg()])
                mv = msk.ap.rearrange("p (g e) -> p g e", g=NGRP)
                S.op("dve", lambda e: e.tensor_tensor(out=mv, in0=bv, in1=cntg.unsqueeze(2).to_broadcast([128, 8, 32]), op=ALU.add),
                     reads=[bi_.reg(), rs.reg()], writes=[msk.reg()])
                S.op("dve", lambda e: e.max(out=mx8.ap, in_=msk.ap), reads=[msk.reg()], writes=[mx8.reg()])
                S.op("dve", lambda e: e.max_index(out=idx8.ap, in_max=mx8.ap, in_values=msk.ap), reads=[msk.reg(), mx8.reg()], writes=[idx8.reg()])
                S.op("dve", lambda e: e.tensor_copy(out=idx8f.ap, in_=idx8.ap), reads=[idx8.reg()], writes=[idx8f.reg()])
                S.op("dve", lambda e: e.tensor_scalar(out=eqt.ap, in0=msk.ap, scalar1=mx8.ap[:, 7:8], scalar2=None, op0=ALU.is_ge),
                     reads=[msk.reg(), mx8.reg()], writes=[eqt.reg()])
                S.op("pe", lambda e: e.matmul(banks[3][:, 0:256], lhsT=tri, rhs=eqt.ap, start=True, stop=False),
                     reads=[eqt.reg(), R("consts")], writes=[bank_reg(3)])
                S.op("pe", lambda e: e.matmul(banks[3][:, 0:256], lhsT=ones, rhs=mcum.ap, start=False, stop=True),
                     reads=[mcum.reg(), R("consts")], writes=[bank_reg(3)])
                S.op("dve", lambda e: e.tensor_tensor(out=mcum.ap, in0=mcum.ap, in1=eqt.ap, op=ALU.add), reads=[mcum.reg(), eqt.reg()], writes=[mcum.reg()])
                S.op("dve", lambda e, j=j: e.tensor_copy(out=posall.ap[:, j, :], in_=banks[3][:, 0:256]), reads=[bank_reg(3)], writes=[posall.reg(j * 1024, (j + 1) * 1024)])
                S.op("dve", lambda e, j=j: e.tensor_copy(out=idxall.ap[:, j, :], in_=idx8f.ap), reads=[idx8f.reg()], writes=[idxall.reg(j * 32, j * 32 + 32)])
                for k in range(8):
                    S.op("dve", lambda e, k=k, scj=scj: e.scalar_tensor_tensor(out=junk2.ap[:, 256:512], in0=iota_e, scalar=idx8f.ap[:, k:k + 1], in1=scj.ap,
                                                                     op0=ALU.is_equal, op1=ALU.mult, accum_out=sc8.ap[:, k:k + 1]),
                         reads=[idx8f.reg(), scj.reg(), R("consts")], writes=[junk2.reg(1024, 2048), sc8.reg(k * 4, k * 4 + 4)])
                ssum = rs.ap[:, 96:97]
                S.op("dve", lambda e: e.tensor_reduce(out=ssum, in_=sc8.ap, axis=AX.X, op=ALU.add), reads=[sc8.reg()], writes=[rs.reg()])
                S.op("dve", lambda e: e.reciprocal(out=rs.ap[:, 97:98], in_=ssum), reads=[rs.reg()], writes=[rs.reg()])
                S.op("dve", lambda e, j=j: e.tensor_scalar(out=gate8.ap[:, j, :], in0=sc8.ap, scalar1=rs.ap[:, 97:98], scalar2=2.5, op0=ALU.mult, op1=ALU.mult),
                     reads=[sc8.reg(), rs.reg()], writes=[gate8.reg(j * 32, j * 32 + 32)])

            c1_A1(0)
            c1_A2(0)
            for j in range(NT):
                if j + 1 < NT:
                    c1_A1(j + 1)
                c1_B(j)
                if j + 1 < NT:
                    c1_A2(j + 1)

            for c in range(2):
                S.op("pe", lambda e, c=c: e.matmul(banks[3][:, c:c + 1], lhsT=mcum.ap[:, c * 128:(c + 1) * 128], rhs=ones[:, 0:1], start=True, stop=True),
                     reads=[mcum.reg(), R("consts")], writes=[bank_reg(3)])
            S.op("dve", lambda e: e.tensor_scalar(out=ovT.ap[:, 0:2], in0=banks[3][:, 0:2], scalar1=float(CAP) + 0.5, scalar2=None, op0=ALU.is_gt),
                 reads=[bank_reg(3)], writes=[ovT.reg()])
            S.op("pe", lambda e: e.matmul(banks[2][:, 0:2], lhsT=tri, rhs=ovT.ap[:, 0:2], start=True, stop=False),
                 reads=[ovT.reg(), R("consts")], writes=[bank_reg(2)])
            S.op("pe", lambda e: e.matmul(banks[2][:, 1:2], lhsT=ones, rhs=ovT.ap[:, 0:1], start=False, stop=True),
                 reads=[ovT.reg(), R("consts")], writes=[bank_reg(2)])
            S.op("dve", lambda e: e.tensor_copy(out=rankT.ap[:, 0:2], in_=banks[2][:, 0:2]), reads=[bank_reg(2)], writes=[rankT.reg()])
            for c in range(2):
                S.op("dve", lambda e, c=c: e.tensor_scalar(out=diagR.ap, in0=ident, scalar1=rankT.ap[:, c:c + 1], scalar2=None, op0=ALU.mult),
                     reads=[rankT.reg(), R("consts")], writes=[diagR.reg()])
                S.op("pe", lambda e, c=c: e.matmul(banks[3][:, c * 128:(c + 1) * 128], lhsT=ones, rhs=diagR.ap, start=True, stop=True),
                     reads=[diagR.reg(), R("consts")], writes=[bank_reg(3)])
            S.op("dve", lambda e: e.tensor_scalar(out=ovb.ap, in0=banks[3][:, 0:256], scalar1=float(NOV) - 0.5, scalar2=1e7, op0=ALU.is_gt, op1=ALU.mult),
                 reads=[bank_reg(3)], writes=[ovb.reg()])
            S.op("dve", lambda e: e.scalar_tensor_tensor(out=ovb.ap, in0=banks[3][:, 0:256], scalar=float(CAP), in1=ovb.ap, op0=ALU.mult, op1=ALU.add),
                 reads=[bank_reg(3), ovb.reg()], writes=[ovb.reg()])
            S.op("dve", lambda e: e.tensor_scalar(out=ovb.ap, in0=ovb.ap, scalar1=float((NE - 1) * CAP), scalar2=None, op0=ALU.add),
                 reads=[ovb.reg()], writes=[ovb.reg()])
            S.op("dve", lambda e: e.scalar_tensor_tensor(out=diffr.ap, in0=iota_e, scalar=float(CAP), in1=ovb.ap, op0=ALU.mult, op1=ALU.subtract),
                 reads=[ovb.reg(), R("consts")], writes=[diffr.reg()])
            for c in range(2):
                S.op("dve", lambda e, c=c: e.tensor_scalar(out=selT.ap[:, c, :], in0=iota_e[:, 0:NOV], scalar1=rankT.ap[:, c:c + 1], scalar2=ovT.ap[:, c:c + 1],
                                                           op0=ALU.is_equal, op1=ALU.mult),
                     reads=[rankT.reg(), ovT.reg(), R("consts")], writes=[selT.reg()])
            for c in range(2):
                S.op("pe", lambda e, c=c: e.matmul(banks[2][:, 0:NOV], lhsT=(eid0 if c == 0 else eid1), rhs=selT.ap[:, c, :], start=(c == 0), stop=(c == 1)),
                     reads=[selT.reg(), R("consts")], writes=[bank_reg(2)])
            S.op("dve", lambda e: e.tensor_scalar(out=widxf.ap, in0=banks[2][:, 0:NOV], scalar1=128.0, scalar2=tokf[:, 0:1], op0=ALU.mult, op1=ALU.add),
                 reads=[bank_reg(2), R("consts")], writes=[widxf.reg()])
            S.op("dve", lambda e: e.tensor_copy(out=widx.ap, in_=widxf.ap), reads=[widxf.reg()], writes=[widx.reg()])

            for j in range(NT):
                pj = posall.ap[:, j, :]
                S.op("dve", lambda e, pj=pj: e.tensor_scalar(out=eqt.ap, in0=pj, scalar1=float(CAP) - 0.5, scalar2=None, op0=ALU.is_lt),
                     reads=[posall.reg(j * 1024, (j + 1) * 1024)], writes=[eqt.reg()])
                S.op("dve", lambda e: e.tensor_tensor(out=offm.ap, in0=eqt.ap, in1=diffr.ap, op=ALU.mult), reads=[eqt.reg(), diffr.reg()], writes=[offm.reg()])
                S.op("dve", lambda e: e.tensor_tensor(out=offm.ap, in0=offm.ap, in1=ovb.ap, op=ALU.add), reads=[offm.reg(), ovb.reg()], writes=[offm.reg()])
                S.op("dve", lambda e, pj=pj: e.tensor_tensor(out=offm.ap, in0=offm.ap, in1=pj, op=ALU.add), reads=[offm.reg(), posall.reg(j * 1024, (j + 1) * 1024)], writes=[offm.reg()])
                S.op("dve", lambda e, pj=pj: e.tensor_scalar(out=eqt.ap, in0=pj, scalar1=2.0 * CAP - 0.5, scalar2=1e7, op0=ALU.is_gt, op1=ALU.mult),
                     reads=[posall.reg(j * 1024, (j + 1) * 1024)], writes=[eqt.reg()])
                S.op("dve", lambda e: e.tensor_tensor(out=offm.ap, in0=offm.ap, in1=eqt.ap, op=ALU.add), reads=[offm.reg(), eqt.reg()], writes=[offm.reg()])
                for k in range(8):
                    S.op("dve", lambda e, k=k, j=j: e.scalar_tensor_tensor(out=junk2.ap[:, 0:256], in0=iota_e, scalar=idxall.ap[:, j, k:k + 1], in1=offm.ap,
                                                                          op0=ALU.is_equal, op1=ALU.mult, accum_out=off8f.ap[:, k:k + 1]),
                         reads=[idxall.reg(j * 32, j * 32 + 32), offm.reg(), R("consts")], writes=[junk2.reg(0, 1024), off8f.reg(k * 4, k * 4 + 4)])
                S.op("dve", lambda e, j=j: e.tensor_copy(out=off8.ap[:, j, :], in_=off8f.ap), reads=[off8f.reg()], writes=[off8.reg(j * 32, j * 32 + 32)])
                for k in range(8):
                    S.op("pool", lambda e, j=j, k=k: e.indirect_dma_start(out=list_dram[:, :], out_offset=bass.IndirectOffsetOnAxis(ap=off8.ap[:, j, k:k + 1], axis=0),
                                                                         in_=tok_i.ap[:, j, :], in_offset=None, bounds_check=breg(e, (NE + NOV) * CAP - 1), oob_is_err=False),
                         reads=[off8.reg(j * 32, j * 32 + 32), tok_i.reg()], writes=[R("list_dram", j * 8 + k + 1, j * 8 + k + 2)], dsem="lsc")
            if debug:
                dma_sp(dbg["off8"], off8.ap.rearrange("p j k -> p (j k)"), [off8.reg()], [R("dbg_off8")], sem="st")
                dma_sp(dbg["gate8"], gate8.ap.rearrange("p j k -> p (j k)"), [gate8.reg()], [R("dbg_gate8")], sem="st")

            checkpoint(5)
            A.top = CKEEP
            NW = 3
            wg_ = [A.get(f"wg{i}", 8 * 256 * 4, F32, "p (k n) -> p k n", k=8) for i in range(NW)]
            wu_ = [A.get(f"wu{i}", 8 * 256 * 4, F32, "p (k n) -> p k n", k=8) for i in range(NW)]
            wdn = [A.get(f"wdn{i}", 2 * 1024 * 4, F32, "p (c n) -> p c n", c=2) for i in range(NW)]
            xe = [A.get(f"xe{i}", 4096, F32) for i in range(NW)]
            lidx = [A.get(f"lidx{i}", 8, I32) for i in range(NW)]
            xeT = [A.get(f"xeT{i}", 4096, F32, "p (k t) -> p k t", k=8) for i in range(2)]
            es = [A.get(f"es{i}", 1024, F32) for i in range(2)]
            eh = [A.get(f"eh{i}", 1024, F32) for i in range(2)]
            ehT = [A.get(f"ehT{i}", 1024, F32, "p (c t) -> p c t", c=2) for i in range(2)]
            ysb = [A.get(f"ysb{i}", 4096, F32) for i in range(2)]

            def load_expert(e_):
                s_ = e_ % NW
                dma_pool(wg_[s_].ap.bitcast(F32R), wge_d[e_].rearrange("(p k) n -> p k n", k=8), [], [wg_[s_].reg()], sem="wg%d" % s_)
                dma_pool(wu_[s_].ap.bitcast(F32R), wue_d[e_].rearrange("(p k) n -> p k n", k=8), [], [wu_[s_].reg()], sem="wu%d" % s_)
                dma_pool(wdn[s_].ap.bitcast(F32R), wde_d[e_].rearrange("(p c) n -> p c n", c=2), [], [wdn[s_].reg()], sem="wd%d" % s_)
                dma_sp(lidx[s_].ap[:, 0:2], list_dram[e_ * CAP:(e_ + 1) * CAP, :], [R("list_dram", 0, 1000)], [lidx[s_].reg()], sem="li%d" % s_)
                S.op("pool", lambda e, s_=s_: e.indirect_dma_start(out=xe[s_].ap, out_offset=None, in_=h2_dram[:, :],
                                                                   in_offset=bass.IndirectOffsetOnAxis(ap=lidx[s_].ap[:, 0:1], axis=0),
                                                                   bounds_check=breg(e, 2048), oob_is_err=False),
                     reads=[lidx[s_].reg(), R("h2_dram", 0, 17)], writes=[xe[s_].reg()], dsem="xg%d" % s_)

            for i_ in range(NW):
                S.op("pool", lambda e, i_=i_: e.memset(xe[i_].ap, 0.0), writes=[xe[i_].reg()])
            for e_ in range(min(NW - 1, NE)):
                load_expert(e_)
            for e_ in range(NE):
                if e_ + NW - 1 < NE:
                    load_expert(e_ + NW - 1)
                s_ = e_ % NW
                d_ = e_ % 2
                b0, b1 = (0, 1) if d_ == 0 else (2, 3)
                transpose8(xe[s_].ap, xe[s_].reg(), xeT[d_], b0, b1)
                for (wbuf, c0) in ((wg_[s_], 0), (wu_[s_], 256)):
                    for k in range(8):
                        S.op("pe", lambda e, k=k, wbuf=wbuf, c0=c0, d_=d_: e.matmul(banks[4][:, c0:c0 + 256], lhsT=xeT[d_].ap[:, k, :].bitcast(F32R),
                                                                                 rhs=wbuf.ap[:, k, :].bitcast(F32R), start=(k == 0), stop=(k == 7)),
                             reads=[xeT[d_].reg(), wbuf.reg()], writes=[bank_reg(4)])
                S.op("act", lambda e, d_=d_: e.activation(out=es[d_].ap, in_=banks[4][:, 0:256], func=ACTF.Silu), reads=[bank_reg(4)], writes=[es[d_].reg()])
                S.op("dve", lambda e, d_=d_: e.tensor_tensor(out=eh[d_].ap, in0=es[d_].ap, in1=banks[4][:, 256:512], op=ALU.mult),
                     reads=[es[d_].reg(), bank_reg(4)], writes=[eh[d_].reg()])
                ehv = eh[d_].ap.rearrange("t (p c) -> t c p", c=2)
                for c in range(2):
                    S.op("pe", lambda e, c=c, ehv=ehv, d_=d_: e.transpose(banks[5][:, c * 128:(c + 1) * 128], ehv[:, c, :], ident),
                         reads=[eh[d_].reg(), R("consts")], writes=[bank_reg(5, c * 512, c * 512 + 512)])
                S.op("act", lambda e, d_=d_: e.activation(out=ehT[d_].ap.bitcast(F32R), in_=banks[5][:, 0:256].rearrange("p (c t) -> p c t", c=2), func=ACTF.Copy),
                     reads=[bank_reg(5)], writes=[ehT[d_].reg()])
                for nh_ in range(2):
                    for c in range(2):
                        S.op("pe", lambda e, c=c, nh_=nh_, s_=s_, d_=d_: e.matmul(banks[6 + nh_][:], lhsT=ehT[d_].ap[:, c, :].bitcast(F32R),
                                                                               rhs=wdn[s_].ap[:, c, nh_ * 512:(nh_ + 1) * 512].bitcast(F32R), start=(c == 0), stop=(c == 1)),
                             reads=[ehT[d_].reg(), wdn[s_].reg()], writes=[bank_reg(6 + nh_)])
                S.op("dve", lambda e, d_=d_: e.tensor_copy(out=ysb[d_].ap[:, 0:512], in_=banks[6][:]), reads=[bank_reg(6)], writes=[ysb[d_].reg(0, 2048)])
                S.op("act", lambda e, d_=d_: e.activation(out=ysb[d_].ap[:, 512:1024], in_=banks[7][:], func=ACTF.Copy), reads=[bank_reg(7)], writes=[ysb[d_].reg(2048, 4096)])
                dma_sp(y_dram[e_ * CAP:(e_ + 1) * CAP, :], ysb[d_].ap, [ysb[d_].reg()], [R("y_dram", e_, e_ + 1)], sem="yst%d" % d_)

            A.top = CKEEP
            wgo = [A.get(f"wgo{i}", 8 * 256 * 4, F32, "p (k n) -> p k n", k=8) for i in range(2)]
            wuo = [A.get(f"wuo{i}", 8 * 256 * 4, F32, "p (k n) -> p k n", k=8) for i in range(2)]
            wdo = [A.get(f"wdo{i}", 2 * 1024 * 4, F32, "p (c n) -> p c n", c=2) for i in range(2)]
            wge_rows = wge_d.rearrange("e (p k) n -> (e p) (k n)", k=8)
            wue_rows = wue_d.rearrange("e (p k) n -> (e p) (k n)", k=8)
            wde_rows = wde_d.rearrange("e (p c) n -> (e p) (c n)", c=2)
            for ob in range(NOV):
                d_ = ob % 2
                s_ = ob % NW
                for (dst, src, nm) in ((wgo[d_], wge_rows, "og"), (wuo[d_], wue_rows, "ou"), (wdo[d_], wde_rows, "od")):
                    S.op("pool", lambda e, dst=dst, src=src, ob=ob: e.indirect_dma_start(
                        out=dst.ap.rearrange("p a b -> p (a b)"), out_offset=None, in_=src[:, :],
                        in_offset=bass.IndirectOffsetOnAxis(ap=widx.ap[:, ob:ob + 1], axis=0), bounds_check=breg(e, NE * 128 - 1), oob_is_err=False),
                        reads=[widx.reg()], writes=[dst.reg()], dsem="%s%d" % (nm, d_))
                dma_sp(lidx[s_].ap[:, 0:2], list_dram[(NE + ob) * CAP:(NE + ob + 1) * CAP, :], [R("list_dram", 0, 1000)], [lidx[s_].reg()], sem="li%d" % s_)
                S.op("pool", lambda e, s_=s_: e.indirect_dma_start(out=xe[s_].ap, out_offset=None, in_=h2_dram[:, :],
                                                                   in_offset=bass.IndirectOffsetOnAxis(ap=lidx[s_].ap[:, 0:1], axis=0),
                                                                   bounds_check=breg(e, 2048), oob_is_err=False),
                     reads=[lidx[s_].reg(), R("h2_dram", 0, 17)], writes=[xe[s_].reg()], dsem="xg%d" % s_)
                b0, b1 = (0, 1) if d_ == 0 else (2, 3)
                transpose8(xe[s_].ap, xe[s_].reg(), xeT[d_], b0, b1)
                for (wbuf, c0) in ((wgo[d_], 0), (wuo[d_], 256)):
                    for k in range(8):
                        S.op("pe", lambda e, k=k, wbuf=wbuf, c0=c0, d_=d_: e.matmul(banks[4][:, c0:c0 + 256], lhsT=xeT[d_].ap[:, k, :], rhs=wbuf.ap[:, k, :],
                                                                                 start=(k == 0), stop=(k == 7)),
                             reads=[xeT[d_].reg(), wbuf.reg()], writes=[bank_reg(4)])
                S.op("act", lambda e, d_=d_: e.activation(out=es[d_].ap, in_=banks[4][:, 0:256], func=ACTF.Silu), reads=[bank_reg(4)], writes=[es[d_].reg()])
                S.op("dve", lambda e, d_=d_: e.tensor_tensor(out=eh[d_].ap, in0=es[d_].ap, in1=banks[4][:, 256:512], op=ALU.mult),
                     reads=[es[d_].reg(), bank_reg(4)], writes=[eh[d_].reg()])
                ehv = eh[d_].ap.rearrange("t (p c) -> t c p", c=2)
                for c in range(2):
                    S.op("pe", lambda e, c=c, ehv=ehv, d_=d_: e.transpose(banks[5][:, c * 128:(c + 1) * 128], ehv[:, c, :], ident),
                         reads=[eh[d_].reg(), R("consts")], writes=[bank_reg(5, c * 512, c * 512 + 512)])
                S.op("act", lambda e, d_=d_: e.activation(out=ehT[d_].ap.bitcast(F32R), in_=banks[5][:, 0:256].rearrange("p (c t) -> p c t", c=2), func=ACTF.Copy),
                     reads=[bank_reg(5)], writes=[ehT[d_].reg()])
                for nh_ in range(2):
                    for c in range(2):
                        S.op("pe", lambda e, c=c, nh_=nh_, d_=d_: e.matmul(banks[6 + nh_][:], lhsT=ehT[d_].ap[:, c, :], rhs=wdo[d_].ap[:, c, nh_ * 512:(nh_ + 1) * 512],
                                                                        start=(c == 0), stop=(c == 1)),
                             reads=[ehT[d_].reg(), wdo[d_].reg()], writes=[bank_reg(6 + nh_)])
                S.op("dve", lambda e, d_=d_: e.tensor_copy(out=ysb[d_].ap[:, 0:512], in_=banks[6][:]), reads=[bank_reg(6)], writes=[ysb[d_].reg(0, 2048)])
                S.op("act", lambda e, d_=d_: e.activation(out=ysb[d_].ap[:, 512:1024], in_=banks[7][:], func=ACTF.Copy), reads=[bank_reg(7)], writes=[ysb[d_].reg(2048, 4096)])
                dma_sp(y_dram[(NE + ob) * CAP:(NE + ob + 1) * CAP, :], ysb[d_].ap, [ysb[d_].reg()], [R("y_dram", NE + ob, NE + ob + 1)], sem="yst%d" % d_)

            checkpoint(6)
            A.top = CKEEP
            yg = [A.get(f"yg{i}", 4096, F32) for i in range(4)]
            acc = [A.get(f"acc{i}", 4096, F32) for i in range(2)]
            bs = [A.get(f"bs{i}", 4096, F32) for i in range(2)]
            gi_ = 0
            last_tok = None
            for g in yg:
                S.op("pool", lambda e, g=g: e.memset(g.ap, 0.0), writes=[g.reg()])
            offv = A.get("offv", 128 * 4, F32, "p (j k) -> p j k", j=16)
            S.op("dve", lambda e: e.tensor_copy(out=offv.ap, in_=off8.ap), reads=[off8.reg()], writes=[offv.reg()])
            S.op("dve", lambda e: e.tensor_scalar(out=offv.ap, in0=offv.ap, scalar1=float((NE + NOV) * CAP) - 0.5, scalar2=None, op0=ALU.is_lt),
                 reads=[offv.reg()], writes=[offv.reg()])
            S.op("dve", lambda e: e.tensor_tensor(out=gate8.ap, in0=gate8.ap, in1=offv.ap, op=ALU.mult), reads=[gate8.reg(), offv.reg()], writes=[gate8.reg()])
            for j in range(NT):
                tsl = slice(j * 128, (j + 1) * 128)
                ac = acc[j % 2]
                bsb = bs[j % 2]
                dma_sp(bsb.ap, base_dram[tsl, :], [R("base_dram", j, j + 1)], [bsb.reg()], sem="bs%d" % (j % 2))
                for k in range(8):
                    g = yg[gi_ % 4]
                    gs_ = gi_ % 4
                    gi_ += 1
                    S.op("pool", lambda e, g=g, j=j, k=k: e.indirect_dma_start(out=g.ap, out_offset=None, in_=y_dram[:, :],
                                                                               in_offset=bass.IndirectOffsetOnAxis(ap=off8.ap[:, j, k:k + 1], axis=0),
                                                                               bounds_check=breg(e, (NE + NOV) * CAP - 1), oob_is_err=False),
                         reads=[off8.reg(j * 32, j * 32 + 32), R("y_dram", 0, 1000)], writes=[g.reg()], dsem="yg%d" % gs_)
                    if k == 0:
                        S.op("dve", lambda e, g=g, ac=ac, j=j, k=k: e.tensor_scalar(out=ac.ap, in0=g.ap, scalar1=gate8.ap[:, j, k:k + 1], scalar2=None, op0=ALU.mult),
                             reads=[g.reg(), gate8.reg()], writes=[ac.reg()])
                    else:
                        S.op("dve", lambda e, g=g, ac=ac, j=j, k=k: e.scalar_tensor_tensor(out=ac.ap, in0=g.ap, scalar=gate8.ap[:, j, k:k + 1], in1=ac.ap, op0=ALU.mult, op1=ALU.add),
                             reads=[g.reg(), gate8.reg(), ac.reg()], writes=[ac.reg()])
                S.op("dve", lambda e, ac=ac: e.tensor_tensor(out=ac.ap, in0=ac.ap, in1=modsC.ap[:, 2048:3072], op=ALU.mult), reads=[ac.reg(), modsC.reg()], writes=[ac.reg()])
                S.op("dve", lambda e, ac=ac, bsb=bsb: e.tensor_tensor(out=ac.ap, in0=ac.ap, in1=bsb.ap, op=ALU.add), reads=[ac.reg(), bsb.reg()], writes=[ac.reg()])
                last_tok = dma_sp(out_d[tsl, :], ac.ap, [ac.reg()], [R("out_d", j, j + 1)], sem="ost%d" % (j % 2))
        except _Stop:
            pass
        S.wait_all("sp", [(k, v) for k, v in S.cnt.items() if k not in COMPUTE])
        S.emit()
    return nc


_NC_CACHE = {}


def make_in_maps(inputs, ncores=8):
    f = lambda a: np.ascontiguousarray(np.asarray(a))
    x = f(inputs["x"]); c = f(inputs["c"]); pos = f(inputs["positions"])
    shared = dict(
        w_ada=f(inputs["w_ada"][0]), b_ada=f(inputs["b_ada"][0]).reshape(1, 6144),
        g_mix=f(inputs["g_norm_mix"][0]).reshape(1, 1024), g_ffn=f(inputs["g_norm_ffn"][0]).reshape(1, 1024),
        w_in=f(inputs["w_in"][0]),
        gqk=np.concatenate([f(inputs["g_q_a"][0]), f(inputs["g_k_a"][0]), f(inputs["g_q_b"][0]), f(inputs["g_k_b"][0])]).reshape(1, 256),
        sinks=f(inputs["sinks_a"][0]).reshape(1, 8),
        g_out=np.concatenate([f(inputs["g_out_a"][0]), f(inputs["g_out_b"][0])]).reshape(1, 1024),
        w_out=f(inputs["w_out"][0]), w_router=f(inputs["w_router"][0]), rbias=f(inputs["router_bias"][0]).reshape(1, 256),
        w_gate_e=f(inputs["w_gate_e"][0]), w_up_e=f(inputs["w_up_e"][0]), w_down_e=f(inputs["w_down_e"][0]),
        w_gate_s=f(inputs["w_gate_s"][0]), w_up_s=f(inputs["w_up_s"][0]), w_down_s=f(inputs["w_down_s"][0]),
        consts=make_consts(),
    )
    maps = []
    for b in range(ncores):
        m = dict(shared)
        m["x"] = x[b]
        m["cT"] = np.ascontiguousarray(c[b].reshape(128, 8))
        m["posT"] = np.ascontiguousarray(pos[b].reshape(16, 128).T.astype(np.int32))
        maps.append(m)
    return maps


def kernel(**inputs):
    if "nc" not in _NC_CACHE:
        _NC_CACHE["nc"] = build_nc()
    nc = _NC_CACHE["nc"]
    maps = make_in_maps(inputs, 8)
    res = run_bass_kernel_spmd(nc, maps, core_ids=list(range(8)))
    out = np.stack([np.asarray(r["out"]) for r in res.results], axis=0)
    return out.astype(np.float32)
```

```python
import contextlib
import numpy as np
import concourse.bass as bass
import concourse.mybir as mybir
from concourse.bass_utils import run_bass_kernel_spmd

F32 = mybir.dt.float32
F32R = mybir.dt.float32r
BF16 = mybir.dt.bfloat16
I32 = mybir.dt.int32
U32 = mybir.dt.uint32
ALU = mybir.AluOpType
ACTF = mybir.ActivationFunctionType
AX = mybir.AxisListType

COMPUTE = ("pe", "act", "dve", "pool")
ENGS = ("pe", "act", "dve", "pool", "sp")
NT = 16
CAP = 128
NE = 256
EPS = 1e-6
NCONST = 1048 + 256
NOV = 24


class Sched:
    def __init__(self, nc, stack, same_engine_sync=True):
        self.nc = nc
        self.stack = stack
        self.same = same_engine_sync
        self.ops = {e: [] for e in ENGS}
        self.cnt = {}
        self.sems = {}
        self.recs = {}
        self.bank_last = {}
        self.waited = {e: {} for e in ENGS}
        for e in COMPUTE:
            self._sem(e)

    def _sem(self, key):
        if key not in self.sems:
            self.sems[key] = self.stack.enter_context(self.nc.semaphore("s_" + key))
            self.cnt[key] = 0
        return self.sems[key]

    def _deps(self, reads, writes):
        deps = []
        for (sp, lo, hi) in reads:
            if sp.startswith("bank"):
                continue
            for r in self.recs.get(sp, ()):
                if r[2] == "w" and r[0] < hi and lo < r[1]:
                    deps.append(r[3])
        for (sp, lo, hi) in writes:
            if sp.startswith("bank"):
                continue
            for r in self.recs.get(sp, ()):
                if r[0] < hi and lo < r[1]:
                    deps.append(r[3])
        for (sp, lo, hi) in list(reads) + list(writes):
            if sp.startswith("bank") and sp in self.bank_last:
                deps.append(self.bank_last[sp])
        return deps

    def _record(self, reads, writes, tok):
        for (sp, lo, hi) in list(reads) + list(writes):
            if sp.startswith("bank"):
                self.bank_last[sp] = tok
        for (sp, lo, hi) in writes:
            if sp.startswith("bank"):
                continue
            lst = self.recs.setdefault(sp, [])
            lst[:] = [r for r in lst if not (lo <= r[0] and r[1] <= hi)]
            lst.append([lo, hi, "w", tok])
        for (sp, lo, hi) in reads:
            if sp.startswith("bank"):
                continue
            lst = self.recs.setdefault(sp, [])
            lst[:] = [r for r in lst if not (r[2] == "r" and r[3][0] == tok[0]
                                             and lo <= r[0] and r[1] <= hi)]
            lst.append([lo, hi, "r", tok])

    def op(self, eng, emit, reads=(), writes=(), dsem=None):
        deps = self._deps(reads, writes)
        if dsem is None:
            key, amt = eng, 1
        else:
            key, amt = dsem, 16
            self._sem(key)
        waits = {}
        for (k, v) in deps:
            if k == eng and dsem is None and (eng == "pe" or not self.same):
                continue
            if self.waited[eng].get(k, 0) >= v:
                continue
            waits[k] = max(waits.get(k, 0), v)
        for k, v in waits.items():
            self.waited[eng][k] = v
        self.cnt[key] += amt
        tok = (key, self.cnt[key])
        self.ops[eng].append((sorted(waits.items()), emit, key, amt))
        self._record(reads, writes, tok)
        return tok

    def wait_all(self, eng, toks):
        waits = {}
        for (k, v) in toks:
            waits[k] = max(waits.get(k, 0), v)
        self.ops[eng].append((sorted(waits.items()), None, None, 0))

    def emit(self):
        nc = self.nc
        with nc.Block() as block:
            def run(engname):
                def body(eng):
                    for waits, emit, key, amt in self.ops[engname]:
                        for (k, v) in waits:
                            eng.wait_ge(self.sems[k], v)
                        if emit is not None:
                            ins = emit(eng)
                            ins.then_inc(self.sems[key], amt)
                return body
            block.tensor(run("pe"))
            block.scalar(run("act"))
            block.vector(run("dve"))
            block.gpsimd(run("pool"))
            block.sync(run("sp"))


def R(name, lo=0, hi=1):
    return (name, lo, hi)


def make_consts():
    c = np.zeros((128, NCONST), np.float32)
    p = np.arange(128)
    c[:, 0:128] = np.eye(128, dtype=np.float32)
    c[:, 128:256] = (p[:, None] < p[None, :]).astype(np.float32)
    c[:, 256:384] = 1.0
    c[:, 384:640] = np.arange(256, dtype=np.float32)[None, :]
    c[:, 640:768] = (p[:, None] <= p[None, :]).astype(np.float32)
    c[:, 768:896] = (p[:, None] > p[None, :]).astype(np.float32)
    c[:, 896:1024] = (p[:, None] >= p[None, :]).astype(np.float32)
    inv = (np.float32(500000.0) ** (-np.arange(0, 16, 2, dtype=np.float32) / np.float32(16))).astype(np.float32)
    c[:, 1024:1032] = inv[None, :]
    c[:, 1032:1048] = (128 * np.arange(16)[None, :] + p[:, None]).astype(np.float32)
    c[:, 1048:1176] = p[:, None].astype(np.float32)
    c[:, 1176:1304] = (128 + p[:, None]).astype(np.float32)
    return c


class _Stop(Exception):
    pass


def build_nc(debug=False, stop_at=None):
    nc = bass.Bass("TRN2", target_bir_lowering=False)

    def din(name, shape, dt=F32):
        return nc.dram_tensor(name, shape, dt, kind="ExternalInput").ap()

    x_d = din("x", [2048, 1024])
    cT_d = din("cT", [128, 8])
    pos_d = din("posT", [128, 16], I32)
    w_ada_d = din("w_ada", [1024, 6144])
    b_ada_d = din("b_ada", [1, 6144])
    gmix_d = din("g_mix", [1, 1024])
    gffn_d = din("g_ffn", [1, 1024])
    w_in_d = din("w_in", [1024, 2304])
    gqk_d = din("gqk", [1, 256])
    sinks_d = din("sinks", [1, 8])
    gout_d = din("g_out", [1, 1024])
    w_out_d = din("w_out", [1024, 1024])
    wr_d = din("w_router", [1024, 256])
    rb_d = din("rbias", [1, 256])
    wge_d = din("w_gate_e", [256, 1024, 256])
    wue_d = din("w_up_e", [256, 1024, 256])
    wde_d = din("w_down_e", [256, 256, 1024])
    wgs_d = din("w_gate_s", [1024, 256])
    wus_d = din("w_up_s", [1024, 256])
    wds_d = din("w_down_s", [256, 1024])
    consts_d = din("consts", [128, NCONST])
    out_d = nc.dram_tensor("out", [2048, 1024], F32, kind="ExternalOutput").ap()

    mods_dram = nc.dram_tensor("mods_scr", [128, 6144], F32).ap()
    v_dram = nc.dram_tensor("v_scr", [2048, 520], BF16).ap()
    o4_dram = nc.dram_tensor("o4_scr", [2048, 520], F32).ap()
    o16_dram = nc.dram_tensor("o16_scr", [2048, 520], F32).ap()
    x1_dram = nc.dram_tensor("x1_scr", [2048, 1024], F32).ap()
    base_dram = nc.dram_tensor("base_scr", [2048, 1024], F32).ap()
    h2_dram = nc.dram_tensor("h2_scr", [2049, 1024], F32).ap()
    list_dram = nc.dram_tensor("list_scr", [(NE + NOV) * CAP, 2], I32).ap()
    y_dram = nc.dram_tensor("y_scr", [(NE + NOV) * CAP, 1024], F32).ap()

    dbg = {}
    if debug:
        dbg["x1"] = nc.dram_tensor("dbg_x1", [2048, 1024], F32, kind="ExternalOutput").ap()
        dbg["h2"] = nc.dram_tensor("dbg_h2", [2048, 1024], F32, kind="ExternalOutput").ap()
        dbg["off8"] = nc.dram_tensor("dbg_off8", [128, 128], I32, kind="ExternalOutput").ap()
        dbg["gate8"] = nc.dram_tensor("dbg_gate8", [128, 128], F32, kind="ExternalOutput").ap()
        dbg["base"] = nc.dram_tensor("dbg_base", [2048, 1024], F32, kind="ExternalOutput").ap()
        dbg["mods"] = nc.dram_tensor("dbg_mods", [128, 6144], F32, kind="ExternalOutput").ap()
        dbg["ocat"] = nc.dram_tensor("dbg_ocat", [2048, 1024], F32, kind="ExternalOutput").ap()
        dbg["qk"] = nc.dram_tensor("dbg_qk", [2048, 1664], BF16, kind="ExternalOutput").ap()
        dbg["kTa"] = nc.dram_tensor("dbg_kTa", [128, 2048], BF16, kind="ExternalOutput").ap()
        dbg["kTb"] = nc.dram_tensor("dbg_kTb", [128, 4 * 2048], BF16, kind="ExternalOutput").ap()
        dbg["qTa"] = nc.dram_tensor("dbg_qTa", [128, 4 * 2048], BF16, kind="ExternalOutput").ap()
        dbg["qTb"] = nc.dram_tensor("dbg_qTb", [128, 4 * 2048], BF16, kind="ExternalOutput").ap()
        dbg["v"] = nc.dram_tensor("dbg_v", [2048, 650], BF16, kind="ExternalOutput").ap()

    with contextlib.ExitStack() as st:
        S = Sched(nc, st)
        ARENA_F = 52100
        consts = nc.alloc_sbuf_tensor_at("consts_sb", [128, NCONST], F32, offset=16512)
        banks = [st.enter_context(nc.psum_tensor(f"bank{i}", [128, 512], F32)) for i in range(8)]

        uid = [0]
        ABASE = 21760

        class Buf:
            def __init__(self, name, off_b, nbytes, dt, shape_str=None, **kw):
                self.name = name
                self.off = off_b
                self.nbytes = nbytes
                isz = 2 if dt == BF16 else 4
                uid[0] += 1
                t = nc.alloc_sbuf_tensor_at("%s_%d" % (name, uid[0]), [128, nbytes // isz], dt, offset=ABASE + off_b)
                ap = t[:]
                if shape_str:
                    ap = ap.rearrange(shape_str, **kw)
                self.ap = ap

            def reg(self, lo=None, hi=None):
                if lo is None:
                    return ("arena", self.off, self.off + self.nbytes)
                return ("arena", self.off + lo, self.off + hi)

        class Alloc:
            def __init__(self):
                self.top = 0

            def get(self, name, nbytes, dt=F32, shape_str=None, **kw):
                nbytes = (nbytes + 31) // 32 * 32
                b = Buf(name, self.top, nbytes, dt, shape_str, **kw)
                self.top += nbytes
                assert self.top <= ARENA_F * 4, (name, self.top)
                return b

        def checkpoint(n):
            if stop_at == n:
                raise _Stop()

        last_tok = None
        try:
            A = Alloc()
            ident = consts[:, 0:128]
            tri = consts[:, 128:256]
            ones = consts[:, 256:384]
            iota_e = consts[:, 384:640]
            invf = consts[:, 1024:1032]
            tokf = consts[:, 1032:1048]
            eid0 = consts[:, 1048:1176]
            eid1 = consts[:, 1176:1304]
            ident_b = A.get("ident_b", 256, BF16)
            masks_b = A.get("masks_b", 3 * 256, BF16, "p (m k) -> p m k", m=3)
            maskA2 = A.get("maskA2", 2 * 4 * 256, BF16, "p (j h k) -> p j h k", j=2, h=4)
            maskB4 = A.get("maskB4", 4 * 256, BF16, "p (b k) -> p b k", b=4)
            cosb = A.get("cos", 16 * 8 * 4, F32, "p (j f) -> p j f", j=16)
            sinb = A.get("sin", 16 * 8 * 4, F32, "p (j f) -> p j f", j=16)
            off8 = A.get("off8", 128 * 4, I32, "p (j k) -> p j k", j=16)
            gate8 = A.get("gate8", 128 * 4, F32, "p (j k) -> p j k", j=16)
            tok_i = A.get("tok_i", 32 * 4, I32, "p (j t) -> p j t", t=2)
            small = A.get("small", 64 * 4, F32)
            neghalf = A.get("neghalf", 32 * 4, F32)
            esink = A.get("esink", 8 * 4, F32)
            junk = A.get("junk", 4096, F32)
            PERSIST_TOP = A.top

            ld = [0]

            def _autosem(reads, writes, sem):
                if sem not in (None, "st"):
                    return sem
                regs = reads if sem == "st" else writes
                r0 = regs[0]
                return ("s" if sem == "st" else "l") + "_%s_%s" % (r0[0], r0[1])

            def dma_sp(out, in_, reads, writes, sem=None):
                return S.op("sp", lambda e: e.dma_start(out=out, in_=in_), reads=reads, writes=writes, dsem=_autosem(reads, writes, sem))

            def dma_pool(out, in_, reads, writes, sem=None):
                return S.op("pool", lambda e: e.dma_start(out=out, in_=in_), reads=reads, writes=writes, dsem=_autosem(reads, writes, sem))

            _regs = {}

            def breg(e, val):
                if val not in _regs:
                    _regs[val] = e.to_reg(val)
                return _regs[val]

            def bank_reg(i, lo=0, hi=2048):
                return ("bank%d" % i, lo, hi)

            dma_sp(consts[:], consts_d, [], [R("consts")])
            S.op("dve", lambda e: e.tensor_copy(out=ident_b.ap, in_=ident), reads=[R("consts")], writes=[ident_b.reg()])
            for m in range(3):
                S.op("dve", lambda e, m=m: e.tensor_copy(out=masks_b.ap[:, m, :], in_=consts[:, 640 + 128 * m:768 + 128 * m]),
                     reads=[R("consts")], writes=[masks_b.reg()])
            for h in range(4):
                S.op("dve", lambda e, h=h: e.tensor_copy(out=maskA2.ap[:, 0, h, :], in_=consts[:, 768:896]),
                     reads=[R("consts")], writes=[maskA2.reg()])
                S.op("dve", lambda e, h=h: e.tensor_copy(out=maskA2.ap[:, 1, h, :], in_=consts[:, 640:768]),
                     reads=[R("consts")], writes=[maskA2.reg()])
                S.op("dve", lambda e, h=h: e.tensor_copy(out=maskB4.ap[:, h, :], in_=(consts[:, 896:1024] if h % 2 == 0 else consts[:, 640:768])),
                     reads=[R("consts")], writes=[maskB4.reg()])
            S.op("pool", lambda e: e.memset(neghalf.ap, -0.5), writes=[neghalf.reg()])
            S.op("dve", lambda e: e.tensor_copy(out=tok_i.ap, in_=tokf.unsqueeze(2).to_broadcast([128, 16, 2])), reads=[R("consts")], writes=[tok_i.reg()])

            TWO_PI = float(2 * np.pi)
            pos_i = A.get("pos_i", 64, I32)
            pos_f = A.get("pos_f", 64, F32)
            ang = A.get("ang", 512, F32, "p (j f) -> p j f", j=16)
            nrot = A.get("nrot", 512, F32, "p (j f) -> p j f", j=16)
            nrot_i = A.get("nrot_i", 512, I32, "p (j f) -> p j f", j=16)
            tmp_r = A.get("tmp_r", 512, F32, "p (j f) -> p j f", j=16)
            dma_sp(pos_i.ap, pos_d, [], [pos_i.reg()])
            S.op("dve", lambda e: e.tensor_copy(out=pos_f.ap, in_=pos_i.ap), reads=[pos_i.reg()], writes=[pos_f.reg()])
            S.op("dve", lambda e: e.tensor_tensor(out=ang.ap, in0=pos_f.ap.unsqueeze(2).to_broadcast([128, 16, 8]),
                                                  in1=invf.unsqueeze(1).to_broadcast([128, 16, 8]), op=ALU.mult),
                 reads=[pos_f.reg(), R("consts")], writes=[ang.reg()])
            S.op("dve", lambda e: e.tensor_scalar(out=nrot.ap, in0=ang.ap, scalar1=1.0 / TWO_PI, scalar2=None, op0=ALU.mult),
                 reads=[ang.reg()], writes=[nrot.reg()])
            S.op("dve", lambda e: e.tensor_copy(out=nrot_i.ap, in_=nrot.ap), reads=[nrot.reg()], writes=[nrot_i.reg()])
            S.op("dve", lambda e: e.tensor_copy(out=nrot.ap, in_=nrot_i.ap), reads=[nrot_i.reg()], writes=[nrot.reg()])
            S.op("dve", lambda e: e.scalar_tensor_tensor(out=ang.ap, in0=nrot.ap, scalar=-6.28125, in1=ang.ap, op0=ALU.mult, op1=ALU.add),
                 reads=[nrot.reg(), ang.reg()], writes=[ang.reg()])
            S.op("dve", lambda e: e.scalar_tensor_tensor(out=ang.ap, in0=nrot.ap, scalar=-(TWO_PI - 6.28125), in1=ang.ap, op0=ALU.mult, op1=ALU.add),
                 reads=[nrot.reg(), ang.reg()], writes=[ang.reg()])
            PI = float(np.pi)
            S.op("dve", lambda e: e.tensor_scalar(out=tmp_r.ap, in0=ang.ap, scalar1=PI, scalar2=-TWO_PI, op0=ALU.is_gt, op1=ALU.mult),
                 reads=[ang.reg()], writes=[tmp_r.reg()])
            S.op("dve", lambda e: e.tensor_tensor(out=ang.ap, in0=ang.ap, in1=tmp_r.ap, op=ALU.add),
                 reads=[ang.reg(), tmp_r.reg()], writes=[ang.reg()])
            S.op("dve", lambda e: e.tensor_scalar(out=tmp_r.ap, in0=ang.ap, scalar1=-PI, scalar2=TWO_PI, op0=ALU.is_lt, op1=ALU.mult),
                 reads=[ang.reg()], writes=[tmp_r.reg()])
            S.op("dve", lambda e: e.tensor_tensor(out=ang.ap, in0=ang.ap, in1=tmp_r.ap, op=ALU.add),
                 reads=[ang.reg(), tmp_r.reg()], writes=[ang.reg()])
            S.op("act", lambda e: e.activation(out=sinb.ap, in_=ang.ap, func=ACTF.Sin), reads=[ang.reg()], writes=[sinb.reg()])
            S.op("dve", lambda e: e.tensor_scalar(out=tmp_r.ap, in0=ang.ap, scalar1=-1.0, scalar2=None, op0=ALU.mult),
                 reads=[ang.reg()], writes=[tmp_r.reg()])
            S.op("dve", lambda e: e.tensor_tensor(out=tmp_r.ap, in0=tmp_r.ap, in1=ang.ap, op=ALU.max),
                 reads=[ang.reg(), tmp_r.reg()], writes=[tmp_r.reg()])
            S.op("dve", lambda e: e.tensor_scalar(out=tmp_r.ap, in0=tmp_r.ap, scalar1=-1.0, scalar2=PI / 2, op0=ALU.mult, op1=ALU.add),
                 reads=[tmp_r.reg()], writes=[tmp_r.reg()])
            S.op("act", lambda e: e.activation(out=cosb.ap, in_=tmp_r.ap, func=ACTF.Sin), reads=[tmp_r.reg()], writes=[cosb.reg()])
            A.top = PERSIST_TOP

            wada = [A.get(f"wada{i}", 8 * 512 * 4, F32, "p (k n) -> p k n", k=8) for i in range(2)]
            csb = A.get("csb", 8 * 128 * 4, F32, "p (k m) -> p k m", k=8)
            cs = A.get("cs", 32, F32)
            bbc = A.get("bbc", 6144 * 4, F32)
            gmix = A.get("gmix", 4096, F32)
            gffn = A.get("gffn", 4096, F32)
            mods = A.get("mods", 6144 * 4, F32)
            dma_sp(cs.ap, cT_d, [], [cs.reg()])
            dma_pool(bbc.ap, b_ada_d.partition_broadcast(128), [], [bbc.reg()])
            dma_pool(gmix.ap, gmix_d.partition_broadcast(128), [], [gmix.reg()])
            dma_pool(gffn.ap, gffn_d.partition_broadcast(128), [], [gffn.reg()])
            S.op("act", lambda e: e.activation(out=cs.ap, in_=cs.ap, func=ACTF.Silu), reads=[cs.reg()], writes=[cs.reg()])
            S.op("dve", lambda e: e.tensor_copy(out=csb.ap.bitcast(F32R), in_=cs.ap.unsqueeze(2).to_broadcast([128, 8, 128])),
                 reads=[cs.reg()], writes=[csb.reg()])
            wada_v = w_ada_d.rearrange("(p k) n -> p k n", k=8)
            for c in range(12):
                wb = wada[c % 2]
                dma_pool(wb.ap.bitcast(F32R), wada_v[:, :, c * 512:(c + 1) * 512], [], [wb.reg()], sem="wada%d" % (c % 2))
                bk = c % 2
                for k in range(8):
                    S.op("pe", lambda e, k=k, wb=wb, bk=bk: e.matmul(banks[bk][:], lhsT=csb.ap[:, k, :].bitcast(F32R),
                                                                   rhs=wb.ap[:, k, :].bitcast(F32R), start=(k == 0), stop=(k == 7)),
                         reads=[csb.reg(), wb.reg()], writes=[bank_reg(bk)])
                mi = c // 2
                cols = slice(c * 512, (c + 1) * 512)
                S.op("dve", lambda e, bk=bk, cols=cols: e.tensor_tensor(out=mods.ap[:, cols], in0=banks[bk][:], in1=bbc.ap[:, cols], op=ALU.add),
                     reads=[bank_reg(bk), bbc.reg()], writes=[mods.reg(c * 2048, (c + 1) * 2048)])
                if mi in (1, 4):
                    g = gmix if mi == 1 else gffn
                    gc = slice((c % 2) * 512, (c % 2) * 512 + 512)
                    S.op("dve", lambda e, cols=cols, g=g, gc=gc: e.scalar_tensor_tensor(out=mods.ap[:, cols], in0=mods.ap[:, cols], scalar=1.0,
                                                                                      in1=g.ap[:, gc], op0=ALU.add, op1=ALU.mult),
                         reads=[mods.reg(c * 2048, (c + 1) * 2048), g.reg()], writes=[mods.reg(c * 2048, (c + 1) * 2048)])
            dma_sp(mods_dram, mods.ap, [mods.reg()], [R("mods_dram")], sem="st")
            if debug:
                dma_sp(dbg["mods"], mods.ap, [mods.reg()], [R("dbg_mods")], sem="st")
            A.top = PERSIST_TOP

            checkpoint(1)
            modsB = A.get("modsB", 2048 * 4, F32)
            gqk = A.get("gqk", 26 * 64 * 4, F32, "p (h d) -> p h d", h=26)
            qTa = A.get("qTa", 4 * 2048 * 2, BF16, "p (i t) -> p i t", i=4)
            kTa = A.get("kTa", 2048 * 2, BF16)
            qTb = A.get("qTb", 4 * 2048 * 2, BF16, "p (i t) -> p i t", i=4)
            kTb = A.get("kTb", 4 * 2048 * 2, BF16, "p (i t) -> p i t", i=4)
            vnat = A.get("vnat", 16 * 650 * 2, BF16, "p (j h d) -> p j h d", j=16, h=10)
            BWIDE_TOP = A.top
            dma_sp(modsB.ap, mods_dram[:, 0:2048], [R("mods_dram")], [modsB.reg()])
            gq_tmp = A.get("gq_tmp", 1024, F32)
            dma_pool(gq_tmp.ap, gqk_d.partition_broadcast(128), [], [gq_tmp.reg()])
            for h in range(26):
                if h < 8:
                    src, sc = 0, 0.125
                elif h < 10:
                    src, sc = 1, 1.0
                elif h < 18:
                    src, sc = 2, 0.125
                else:
                    src, sc = 3, 1.0
                S.op("dve", lambda e, h=h, src=src, sc=sc: e.tensor_scalar(out=gqk.ap[:, h, :], in0=gq_tmp.ap[:, src * 64:src * 64 + 64],
                                                                            scalar1=sc, scalar2=None, op0=ALU.mult),
                     reads=[gq_tmp.reg()], writes=[gqk.reg()])
            S.op("pool", lambda e: e.memset(vnat.ap, 1.0), writes=[vnat.reg()])

            w_in = A.get("w_in", 8 * 2304 * 4, F32, "p (k n) -> p k n", k=8)
            xt = [A.get(f"xt{i}", 4096, F32) for i in range(1)]
            hbuf = A.get("h", 4096, F32)
            hT = [A.get(f"hT{i}", 4096, F32, "p (k t) -> p k t", k=8) for i in range(1)]
            sq = [A.get(f"sq{i}", 2048, F32) for i in range(2)]
            qkf = [A.get(f"qkf{i}", 2048, F32) for i in range(2)]
            qkb = A.get("qkb", 1664 * 2, BF16)
            rope_t = A.get("rope_t", 4 * 8 * 8 * 4, F32, "p (a h f) -> p a h f", a=4, h=8)
            st_small = A.get("st_small", 64 * 4, F32)
            st_rms = A.get("st_rms", 32, F32)
            dma_pool(w_in.ap.bitcast(F32R), w_in_d.rearrange("(p k) n -> p k n", k=8), [], [w_in.reg()], sem="win")

            def rmsnorm_mod(xin, xin_reg, out_ap, out_reg, a_ap, s_ap, mod_reg, scr, tagsem):
                ss = scr.ap[:, 0:1]
                S.op("act", lambda e: e.activation(out=junk.ap,
                                                   in_=xin, func=ACTF.Square, accum_out=ss),
                     reads=[xin_reg], writes=[junk.reg(), scr.reg()])
                S.op("dve", lambda e: e.tensor_scalar(out=scr.ap[:, 1:2], in0=ss, scalar1=1.0 / 1024, scalar2=EPS, op0=ALU.mult, op1=ALU.add),
                     reads=[scr.reg()], writes=[scr.reg()])
                S.op("pool", lambda e: e.tensor_tensor(out=scr.ap[:, 2:3], in0=scr.ap[:, 1:2], in1=neghalf.ap[:, 0:1], op=ALU.pow),
                     reads=[scr.reg(), neghalf.reg()], writes=[scr.reg()])
                S.op("dve", lambda e: e.scalar_tensor_tensor(out=out_ap, in0=xin, scalar=scr.ap[:, 2:3], in1=a_ap, op0=ALU.mult, op1=ALU.mult),
                     reads=[xin_reg, scr.reg(), mod_reg], writes=[out_reg])
                S.op("dve", lambda e: e.tensor_tensor(out=out_ap, in0=out_ap, in1=s_ap, op=ALU.add),
                     reads=[out_reg, mod_reg], writes=[out_reg])

            def transpose8(src_ap, src_reg, dst, bk0=0, bk1=1):
                v = src_ap.rearrange("t (p k) -> t k p", k=8)
                for k in range(8):
                    bk = bk0 if k < 4 else bk1
                    S.op("pe", lambda e, k=k, bk=bk: e.transpose(banks[bk][:, (k % 4) * 128:(k % 4) * 128 + 128], v[:, k, :], ident),
                         reads=[src_reg, R("consts")], writes=[bank_reg(bk, (k % 4) * 512, (k % 4) * 512 + 512)])
                S.op("act", lambda e: e.activation(out=dst.ap[:, 0:4, :].bitcast(F32R), in_=banks[bk0][:].rearrange("p (k t) -> p k t", k=4), func=ACTF.Copy),
                     reads=[bank_reg(bk0)], writes=[dst.reg(0, 2048)])
                S.op("act", lambda e: e.activation(out=dst.ap[:, 4:8, :].bitcast(F32R), in_=banks[bk1][:].rearrange("p (k t) -> p k t", k=4), func=ACTF.Copy),
                     reads=[bank_reg(bk1)], writes=[dst.reg(2048, 4096)])

            chunks = [(0, 512, 8, 0), (512, 256, 2, 8), (768, 512, 8, 10), (1280, 512, 8, 18), (1792, 512, 0, 0)]
            def b1_A1(j):
                xb = xt[0]
                hTb = hT[0]
                dma_sp(xb.ap, x_d[j * 128:(j + 1) * 128, :], [], [xb.reg()], sem="x0")
                rmsnorm_mod(xb.ap, xb.reg(), hbuf.ap, hbuf.reg(), modsB.ap[:, 1024:2048], modsB.ap[:, 0:1024], modsB.reg(), st_rms, "b1")
                transpose8(hbuf.ap, hbuf.reg(), hTb)
            def b1_A2(j):
                xb = xt[0]
                hTb = hT[0]
                for ci, (c0, ncol, nh, h0) in enumerate(chunks):
                    bk = 2 + ci
                    for k in range(8):
                        S.op("pe", lambda e, k=k, bk=bk, c0=c0, ncol=ncol, hTb=hTb: e.matmul(
                            banks[bk][:, 0:ncol], lhsT=hTb.ap[:, k, :].bitcast(F32R), rhs=w_in.ap[:, k, c0:c0 + ncol].bitcast(F32R),
                            start=(k == 0), stop=(k == 7)),
                            reads=[hTb.reg(), w_in.reg()], writes=[bank_reg(bk)])
            def b1_B(j):
                xb = xt[0]
                hTb = hT[0]
                for ci, (c0, ncol, nh, h0) in enumerate(chunks):
                    bk = 2 + ci
                    if nh > 0:
                        sqb = sq[ci % 2]
                        qf = qkf[ci % 2]
                        nq = nh * 64
                        S.op("act", lambda e, bk=bk, sqb=sqb, nq=nq: e.activation(out=sqb.ap[:, 0:nq], in_=banks[bk][:, 0:nq], func=ACTF.Square),
                             reads=[bank_reg(bk)], writes=[sqb.reg()])
                        ssum = st_small.ap[:, 8:8 + nh]
                        S.op("dve", lambda e, sqb=sqb, nh=nh, nq=nq, ssum=ssum: e.tensor_reduce(out=ssum, in_=sqb.ap[:, 0:nq].rearrange("p (h d) -> p h d", h=nh),
                                                                                             axis=AX.X, op=ALU.add),
                             reads=[sqb.reg()], writes=[st_small.reg()])
                        S.op("dve", lambda e, ssum=ssum: e.tensor_scalar(out=ssum, in0=ssum, scalar1=1.0 / 64, scalar2=EPS, op0=ALU.mult, op1=ALU.add),
                             reads=[st_small.reg()], writes=[st_small.reg()])
                        rstd = st_small.ap[:, 16:16 + nh]
                        S.op("pool", lambda e, ssum=ssum, rstd=rstd, nh=nh: e.tensor_tensor(out=rstd, in0=ssum, in1=neghalf.ap[:, 0:nh], op=ALU.pow),
                             reads=[st_small.reg(), neghalf.reg()], writes=[st_small.reg()])
                        qv = qf.ap[:, 0:nq].rearrange("p (h d) -> p h d", h=nh)
                        S.op("dve", lambda e, bk=bk, qv=qv, rstd=rstd, nh=nh, nq=nq: e.tensor_tensor(
                            out=qv, in0=banks[bk][:, 0:nq].rearrange("p (h d) -> p h d", h=nh),
                            in1=rstd.unsqueeze(2).to_broadcast([128, nh, 64]), op=ALU.mult),
                            reads=[bank_reg(bk), st_small.reg()], writes=[qf.reg()])
                        S.op("dve", lambda e, qv=qv, h0=h0, nh=nh: e.tensor_tensor(out=qv, in0=qv, in1=gqk.ap[:, h0:h0 + nh, :], op=ALU.mult),
                             reads=[qf.reg(), gqk.reg()], writes=[qf.reg()])
                        cb = cosb.ap[:, j, :].unsqueeze(1).to_broadcast([128, nh, 8])
                        sb_ = sinb.ap[:, j, :].unsqueeze(1).to_broadcast([128, nh, 8])
                        t1 = qv[:, :, 0:8]
                        t2 = qv[:, :, 8:16]
                        ra, rb_, rc, rd = (rope_t.ap[:, a, 0:nh, :] for a in range(4))
                        for (o_, i0, i1) in ((ra, t1, cb), (rb_, t2, sb_), (rc, t2, cb), (rd, t1, sb_)):
                            S.op("dve", lambda e, o_=o_, i0=i0, i1=i1: e.tensor_tensor(out=o_, in0=i0, in1=i1, op=ALU.mult),
                                 reads=[qf.reg(), cosb.reg(), sinb.reg()], writes=[rope_t.reg()])
                        S.op("dve", lambda e, t1=t1, ra=ra, rb_=rb_: e.tensor_tensor(out=t1, in0=ra, in1=rb_, op=ALU.subtract),
                             reads=[rope_t.reg()], writes=[qf.reg()])
                        S.op("dve", lambda e, t2=t2, rc=rc, rd=rd: e.tensor_tensor(out=t2, in0=rc, in1=rd, op=ALU.add),
                             reads=[rope_t.reg()], writes=[qf.reg()])
                        qoff = {0: 0, 1: 512, 2: 640, 3: 1152}[ci]
                        if ci == 0:
                            S.op("act", lambda e, qf=qf: e.activation(out=qkb.ap[:, 0:512].rearrange("p (i g d) -> p g i d", i=4, g=2),
                                                                      in_=qf.ap[:, 0:512].rearrange("p (g i d) -> p g i d", g=2, i=4), func=ACTF.Copy),
                                 reads=[qf.reg()], writes=[qkb.reg(0, 1024)])
                        else:
                            S.op("act", lambda e, qf=qf, nq=nq, qoff=qoff: e.activation(out=qkb.ap[:, qoff:qoff + nq], in_=qf.ap[:, 0:nq], func=ACTF.Copy),
                                 reads=[qf.reg()], writes=[qkb.reg(qoff * 2, (qoff + nq) * 2)])
                    if ci == 1:
                        S.op("act", lambda e, bk=bk, j=j: e.activation(out=vnat.ap[:, j, 0:2, 0:64], in_=banks[bk][:, 128:256].rearrange("p (h d) -> p h d", h=2), func=ACTF.Copy),
                             reads=[bank_reg(bk)], writes=[vnat.reg(j * 1300, j * 1300 + 260)])
                    if ci == 4:
                        S.op("act", lambda e, bk=bk, j=j: e.activation(out=vnat.ap[:, j, 2:10, 0:64], in_=banks[bk][:, 0:512].rearrange("p (h d) -> p h d", h=8), func=ACTF.Copy),
                             reads=[bank_reg(bk)], writes=[vnat.reg(j * 1300 + 260, (j + 1) * 1300)])
                        dma_sp(v_dram[j * 128:(j + 1) * 128, :], vnat.ap[:, j, 2:10, :].rearrange("p h d -> p (h d)"),
                               [vnat.reg(j * 1300 + 260, (j + 1) * 1300)], [R("v_dram", j, j + 1), R("vnat_chain")], sem="st_vnat")
                if debug:
                    dma_sp(dbg["qk"][j * 128:(j + 1) * 128, :], qkb.ap, [qkb.reg()], [R("dbg_qk", j, j + 1)], sem="st")
                    dma_sp(dbg["v"][j * 128:(j + 1) * 128, :], vnat.ap[:, j, :, :].rearrange("p h d -> p (h d)"), [vnat.reg(j * 1300, (j + 1) * 1300)], [R("dbg_v", j, j + 1)], sem="st_dbgv")
                pb7 = banks[7][:].bitcast(BF16).rearrange("p (s t) -> p s t", s=8)
                pb0 = banks[0][:].bitcast(BF16).rearrange("p (s t) -> p s t", s=8)
                tsl = slice(j * 128, (j + 1) * 128)
                for i in range(4):
                    S.op("pe", lambda e, i=i: e.transpose(pb7[:, i, :], qkb.ap[:, i * 128:(i + 1) * 128], ident_b.ap),
                         reads=[qkb.reg(0, 1024), ident_b.reg()], writes=[bank_reg(7, i * 256, i * 256 + 256)])
                for i in range(4):
                    S.op("pe", lambda e, i=i: e.transpose(pb7[:, 4 + i, :], qkb.ap[:, 640 + i * 128:640 + i * 128 + 128], ident_b.ap),
                         reads=[qkb.reg(1280, 2304), ident_b.reg()], writes=[bank_reg(7, (4 + i) * 256, (4 + i) * 256 + 256)])
                S.op("act", lambda e, tsl=tsl: e.activation(out=qTa.ap[:, :, tsl], in_=pb7[:, 0:4, :], func=ACTF.Copy),
                     reads=[bank_reg(7)], writes=[qTa.reg()])
                S.op("dve", lambda e, tsl=tsl: e.tensor_copy(out=qTb.ap[:, :, tsl], in_=pb7[:, 4:8, :]),
                     reads=[bank_reg(7)], writes=[qTb.reg()])
                for i in range(4):
                    S.op("pe", lambda e, i=i: e.transpose(pb7[:, i, :], qkb.ap[:, 1152 + i * 128:1152 + i * 128 + 128], ident_b.ap),
                         reads=[qkb.reg(2304, 3328), ident_b.reg()], writes=[bank_reg(7, i * 256, i * 256 + 256)])
                S.op("pe", lambda e: e.transpose(pb7[:, 4, :], qkb.ap[:, 512:640], ident_b.ap),
                     reads=[qkb.reg(1024, 1280), ident_b.reg()], writes=[bank_reg(7, 1024, 1280)])
                S.op("act", lambda e, tsl=tsl: e.activation(out=kTb.ap[:, :, tsl], in_=pb7[:, 0:4, :], func=ACTF.Copy),
                     reads=[bank_reg(7)], writes=[kTb.reg()])
                S.op("dve", lambda e, tsl=tsl: e.tensor_copy(out=kTa.ap[:, tsl], in_=pb7[:, 4, :]),
                     reads=[bank_reg(7)], writes=[kTa.reg()])
            b1_A1(0)
            b1_A2(0)
            for j in range(NT):
                if j + 1 < NT:
                    b1_A1(j + 1)
                b1_B(j)
                if j + 1 < NT:
                    b1_A2(j + 1)
            A.top = BWIDE_TOP
            if debug:
                dma_sp(dbg["kTa"], kTa.ap, [kTa.reg()], [R("dbg_kTa")], sem="st_d1")
                dma_sp(dbg["kTb"], kTb.ap.rearrange("p i t -> p (i t)"), [kTb.reg()], [R("dbg_kTb")], sem="st_d2")
                dma_sp(dbg["qTa"], qTa.ap.rearrange("p i t -> p (i t)"), [qTa.reg()], [R("dbg_qTa")], sem="st_d3")
                dma_sp(dbg["qTb"], qTb.ap.rearrange("p i t -> p (i t)"), [qTb.reg()], [R("dbg_qTb")], sem="st_d4")

            checkpoint(2)
            w_out = A.get("w_out", 8 * 1024 * 4, F32, "p (k n) -> p k n", k=8)
            v4 = A.get("v4", 16 * 520 * 2, BF16, "p (j h d) -> p j h d", j=16, h=8)
            v16 = A.get("v16", 16 * 520 * 2, BF16, "p (j h d) -> p j h d", j=16, h=8)
            modg = A.get("modg", 4096, F32)
            goutb = A.get("goutb", 4096, F32)
            pT = [A.get(f"pT{i}", 512 * 2, BF16) for i in range(4)]
            osb = [A.get(f"osb{i}", 520 * 4, F32, "p (h d) -> p h d", h=8) for i in range(2)]
            o4n = A.get("o4n", 520 * 4, F32, "p (h d) -> p h d", h=8)
            o16n = A.get("o16n", 520 * 4, F32, "p (h d) -> p h d", h=8)
            ocat = A.get("ocat", 4096, F32)
            ocT = A.get("ocT", 4096, F32, "p (k t) -> p k t", k=8)
            xb2 = A.get("xb2", 4096, F32)
            x1b = ocat
            att_s = A.get("att_s", 64 * 4, F32)
            dma_pool(w_out.ap.bitcast(F32R), w_out_d.rearrange("(p k) n -> p k n", k=8), [], [w_out.reg()], sem="wout")
            dma_sp(modg.ap, mods_dram[:, 2048:3072], [R("mods_dram")], [modg.reg()])
            dma_pool(goutb.ap, gout_d.partition_broadcast(128), [], [goutb.reg()])
            dma_pool(esink.ap, sinks_d.partition_broadcast(128), [], [esink.reg()])
            S.op("act", lambda e: e.activation(out=esink.ap, in_=esink.ap, func=ACTF.Exp), reads=[esink.reg()], writes=[esink.reg()])
            v4d = v_dram.rearrange("(i d) c -> d i c", d=4)
            v16d = v_dram.rearrange("(i d) c -> d i c", d=16)
            for r in range(4):
                for m in range(4):
                    dma_sp(v4.ap[:, r * 4 + m, :, :].rearrange("p h d -> p (h d)"), v4d[r, m * 128:(m + 1) * 128, :],
                           [R("v_dram", 0, 16)], [v4.reg((r * 4 + m) * 1040, (r * 4 + m + 1) * 1040)], sem="l_v4")
            for r in range(16):
                dma_sp(v16.ap[:, r, :, :].rearrange("p h d -> p (h d)"), v16d[r, 0:128, :],
                       [R("v_dram", 0, 16)], [v16.reg(r * 1040, (r + 1) * 1040)], sem="l_v16")

            pcount = [0]

            def attn_b_tile(q_sel, k_sels, vbuf, vidx, out_banks):
                nk = len(k_sels)
                for hp2 in range(2):
                    pts = [pT[(pcount[0]) % 4], pT[(pcount[0] + 1) % 4]]
                    pcount[0] += 2
                    blocks = []
                    for hp in (2 * hp2, 2 * hp2 + 1):
                        for (ks, vt, isprev) in k_sels:
                            blocks.append((hp, ks, vt))
                    nb = len(blocks)
                    for s in range(2):
                        sbk = s
                        pt = pts[s]
                        for bi, (hp, ks, vt) in enumerate(blocks):
                            S.op("pe", lambda e, s=s, ks=ks, hp=hp, bi=bi, sbk=sbk: e.matmul(
                                banks[sbk][:, bi * 128:(bi + 1) * 128], lhsT=kTb.ap[64 * s:64 * s + 64, hp, ks],
                                rhs=qTb.ap[64 * s:64 * s + 64, hp, q_sel], start=True, stop=True),
                                reads=[kTb.reg(), qTb.reg()], writes=[bank_reg(sbk, bi * 512, (bi + 1) * 512)])
                        S.op("act", lambda e, pt=pt, sbk=sbk, nb=nb: e.activation(out=pt.ap[:, 0:nb * 128], in_=banks[sbk][:, 0:nb * 128], func=ACTF.Exp),
                             reads=[bank_reg(sbk, 0, nb * 512)], writes=[pt.reg()])
                        if nk == 2:
                            S.op("dve", lambda e, pt=pt: e.tensor_tensor(out=pt.ap[:, 0:512], in0=pt.ap[:, 0:512],
                                                                        in1=maskB4.ap[:, :, :].rearrange("p b k -> p (b k)"), op=ALU.mult),
                                 reads=[pt.reg(), maskB4.reg()], writes=[pt.reg()])
                        else:
                            S.op("dve", lambda e, pt=pt, nb=nb: e.tensor_tensor(out=pt.ap[:, 0:nb * 128].rearrange("p (b k) -> p b k", b=nb),
                                                                               in0=pt.ap[:, 0:nb * 128].rearrange("p (b k) -> p b k", b=nb),
                                                                               in1=masks_b.ap[:, 0:1, :].to_broadcast([128, nb, 128]), op=ALU.mult),
                                 reads=[pt.reg(), masks_b.reg()], writes=[pt.reg()])
                    for s in range(2):
                        pt = pts[s]
                        for hp in (2 * hp2, 2 * hp2 + 1):
                            hh = 2 * hp + s
                            ob = out_banks[hh // 4]
                            col = (hh % 4) * 65
                            mine = [(bi, b) for bi, b in enumerate(blocks) if b[0] == hp]
                            for n_, (bi, (hp_, ks, vt)) in enumerate(mine):
                                S.op("pe", lambda e, pt=pt, bi=bi, vt=vt, hh=hh, ob=ob, col=col, n_=n_, last=(n_ == len(mine) - 1): e.matmul(
                                    banks[ob][:, col:col + 65], lhsT=pt.ap[:, bi * 128:(bi + 1) * 128], rhs=vbuf.ap[:, vt, vidx + hh, :],
                                    start=(n_ == 0), stop=last),
                                    reads=[pt.reg(), vbuf.reg()], writes=[bank_reg(ob, col * 4, col * 4 + 260)])

            o4d = o4_dram.rearrange("(i d) c -> d i c", d=4)
            o16d = o16_dram.rearrange("(i d) c -> d i c", d=16)
            oi = [0]

            def evac_store(dst_ap, dst_reg):
                ob = osb[oi[0] % 2]
                oi[0] += 1
                S.op("act", lambda e, ob=ob: e.activation(out=ob.ap[:, 0:4, :], in_=banks[2][:, 0:260].rearrange("p (h d) -> p h d", h=4), func=ACTF.Copy),
                     reads=[bank_reg(2)], writes=[ob.reg(0, 1040)])
                S.op("dve", lambda e, ob=ob: e.tensor_copy(out=ob.ap[:, 4:8, :], in_=banks[3][:, 0:260].rearrange("p (h d) -> p h d", h=4)),
                     reads=[bank_reg(3)], writes=[ob.reg(1040, 2080)])
                dma_sp(dst_ap, ob.ap.rearrange("p h d -> p (h d)"), [ob.reg()], [dst_reg], sem="st")

            for r in range(4):
                for m in range(4):
                    def sel(mm, r=r):
                        st_ = r + 512 * mm
                        return slice(st_, st_ + 4 * 127 + 1, 4)
                    ks = []
                    if m > 0:
                        ks.append((sel(m - 1), r * 4 + m - 1, True))
                    ks.append((sel(m), r * 4 + m, False))
                    attn_b_tile(sel(m), ks, v4, 0, (2, 3))
                    evac_store(o4d[r, m * 128:(m + 1) * 128, :], R("o4_dram", r * 4 + m, r * 4 + m + 1))
            for r in range(16):
                sl = slice(r, r + 16 * 127 + 1, 16)
                attn_b_tile(sl, [(sl, r, False)], v16, 0, (2, 3))
                evac_store(o16d[r, 0:128, :], R("o16_dram", r, r + 1))

            checkpoint(3)
            for j in range(NT):
                tsl = slice(j * 128, (j + 1) * 128)
                dma_sp(xb2.ap, x_d[tsl, :], [], [xb2.reg()], sem="x2")
                dma_sp(o4n.ap.rearrange("p h d -> p (h d)"), o4_dram[tsl, :], [R("o4_dram", 0, 16)], [o4n.reg()], sem="on")
                dma_sp(o16n.ap.rearrange("p h d -> p (h d)"), o16_dram[tsl, :], [R("o16_dram", 0, 16)], [o16n.reg()], sem="on")
                for kvh in range(2):
                    jks = ([j - 1] if j > 0 else []) + [j]
                    pa = pT[2 * kvh].ap if False else None
                    pt0 = pT[(pcount[0]) % 4]
                    pt1 = pT[(pcount[0] + 1) % 4]
                    pcount[0] += 2
                    pts = [pt0, pt1]
                    for n_, jk in enumerate(jks):
                        sbk = 4 + n_
                        S.op("pe", lambda e, kvh=kvh, jk=jk, sbk=sbk, tsl=tsl: e.matmul(
                            banks[sbk][:].rearrange("p (i t) -> p i t", i=4), lhsT=kTa.ap[64 * kvh:64 * kvh + 64, jk * 128:(jk + 1) * 128],
                            rhs=qTa.ap[64 * kvh:64 * kvh + 64, :, tsl], start=True, stop=True),
                            reads=[kTa.reg(), qTa.reg()], writes=[bank_reg(sbk)])
                        pt = pts[n_]
                        S.op("act", lambda e, pt=pt, sbk=sbk: e.activation(out=pt.ap, in_=banks[sbk][:], func=ACTF.Exp),
                             reads=[bank_reg(sbk)], writes=[pt.reg()])
                        midx = 1 if jk == j else 0
                        S.op("dve", lambda e, pt=pt, midx=midx: e.tensor_tensor(out=pt.ap, in0=pt.ap, in1=maskA2.ap[:, midx, :, :].rearrange("p h k -> p (h k)"), op=ALU.mult),
                             reads=[pt.reg(), maskA2.reg()], writes=[pt.reg()])
                    ob = 6 + kvh
                    for i in range(4):
                        for n_, jk in enumerate(jks):
                            pt = pts[n_]
                            S.op("pe", lambda e, pt=pt, i=i, jk=jk, kvh=kvh, ob=ob, n_=n_, last=(n_ == len(jks) - 1): e.matmul(
                                banks[ob][:, i * 65:(i + 1) * 65], lhsT=pt.ap[:, i * 128:(i + 1) * 128], rhs=vnat.ap[:, jk, kvh, :],
                                start=(n_ == 0), stop=last),
                                reads=[pt.reg(), vnat.reg()], writes=[bank_reg(ob, i * 260, (i + 1) * 260)])
                ks = ([(slice((j - 1) * 128, j * 128), j - 1, True)] if j > 0 else []) + [(tsl, j, False)]
                if j == 0:
                    attn_b_tile(tsl, ks, vnat, 2, (2, 3))
                else:
                    attn_b_tile(tsl, ks, vnat, 2, (2, 3))
                den = att_s.ap[:, 0:8]
                for kvh in range(2):
                    S.op("dve", lambda e, kvh=kvh: e.tensor_tensor(out=att_s.ap[:, kvh * 4:kvh * 4 + 4], in0=banks[6 + kvh][:, 0:260].rearrange("p (h d) -> p h d", h=4)[:, :, 64],
                                                                  in1=esink.ap[:, kvh * 4:kvh * 4 + 4], op=ALU.add),
                         reads=[bank_reg(6 + kvh), esink.reg()], writes=[att_s.reg()])
                S.op("dve", lambda e: e.reciprocal(out=att_s.ap[:, 8:16], in_=att_s.ap[:, 0:8]), reads=[att_s.reg()], writes=[att_s.reg()])
                for kvh in range(2):
                    S.op("dve", lambda e, kvh=kvh: e.tensor_tensor(out=ocat.ap[:, kvh * 256:(kvh + 1) * 256].rearrange("p (h d) -> p h d", h=4),
                                                                  in0=banks[6 + kvh][:, 0:260].rearrange("p (h d) -> p h d", h=4)[:, :, 0:64],
                                                                  in1=att_s.ap[:, 8 + kvh * 4:12 + kvh * 4].unsqueeze(2).to_broadcast([128, 4, 64]), op=ALU.mult),
                         reads=[bank_reg(6 + kvh), att_s.reg()], writes=[ocat.reg(kvh * 1024, (kvh + 1) * 1024)])
                S.op("dve", lambda e: e.tensor_tensor(out=o4n.ap, in0=o4n.ap, in1=o16n.ap, op=ALU.add), reads=[o4n.reg(), o16n.reg()], writes=[o4n.reg()])
                for half in range(2):
                    S.op("dve", lambda e, half=half: e.tensor_tensor(out=o4n.ap[:, half * 4:half * 4 + 4, :], in0=o4n.ap[:, half * 4:half * 4 + 4, :],
                                                                    in1=banks[2 + half][:, 0:260].rearrange("p (h d) -> p h d", h=4), op=ALU.add),
                         reads=[o4n.reg(), bank_reg(2 + half)], writes=[o4n.reg()])
                S.op("dve", lambda e: e.reciprocal(out=att_s.ap[:, 16:24], in_=o4n.ap[:, :, 64]), reads=[o4n.reg()], writes=[att_s.reg()])
                S.op("dve", lambda e: e.tensor_tensor(out=ocat.ap[:, 512:1024].rearrange("p (h d) -> p h d", h=8), in0=o4n.ap[:, :, 0:64],
                                                      in1=att_s.ap[:, 16:24].unsqueeze(2).to_broadcast([128, 8, 64]), op=ALU.mult),
                     reads=[o4n.reg(), att_s.reg()], writes=[ocat.reg(2048, 4096)])
                for gi in range(2):
                    gsl = slice(gi * 512, (gi + 1) * 512)
                    S.op("act", lambda e, gsl=gsl, gi=gi: e.activation(out=junk.ap[:, 0:512], in_=ocat.ap[:, gsl], func=ACTF.Square, accum_out=att_s.ap[:, 24 + gi:25 + gi]),
                         reads=[ocat.reg(gi * 2048, (gi + 1) * 2048)], writes=[junk.reg(), att_s.reg()])
                S.op("dve", lambda e: e.tensor_scalar(out=att_s.ap[:, 26:28], in0=att_s.ap[:, 24:26], scalar1=1.0 / 512, scalar2=EPS, op0=ALU.mult, op1=ALU.add),
                     reads=[att_s.reg()], writes=[att_s.reg()])
                S.op("pool", lambda e: e.tensor_tensor(out=att_s.ap[:, 28:30], in0=att_s.ap[:, 26:28], in1=neghalf.ap[:, 0:2], op=ALU.pow),
                     reads=[att_s.reg(), neghalf.reg()], writes=[att_s.reg()])
                for gi in range(2):
                    gsl = slice(gi * 512, (gi + 1) * 512)
                    S.op("dve", lambda e, gsl=gsl, gi=gi: e.scalar_tensor_tensor(out=ocat.ap[:, gsl], in0=ocat.ap[:, gsl], scalar=att_s.ap[:, 28 + gi:29 + gi],
                                                                               in1=goutb.ap[:, gsl], op0=ALU.mult, op1=ALU.mult),
                         reads=[ocat.reg(gi * 2048, (gi + 1) * 2048), att_s.reg(), goutb.reg()], writes=[ocat.reg(gi * 2048, (gi + 1) * 2048)])
                if debug:
                    dma_sp(dbg["ocat"][tsl, :], ocat.ap, [ocat.reg()], [R("dbg_ocat", j, j + 1)], sem="st")
                transpose8(ocat.ap, ocat.reg(), ocT, 0, 1)
                for nh_ in range(2):
                    bk = 4 + nh_
                    for k in range(8):
                        S.op("pe", lambda e, k=k, bk=bk, nh_=nh_: e.matmul(banks[bk][:], lhsT=ocT.ap[:, k, :].bitcast(F32R),
                                                                         rhs=w_out.ap[:, k, nh_ * 512:(nh_ + 1) * 512].bitcast(F32R), start=(k == 0), stop=(k == 7)),
                             reads=[ocT.reg(), w_out.reg()], writes=[bank_reg(bk)])
                for nh_ in range(2):
                    csl = slice(nh_ * 512, (nh_ + 1) * 512)
                    S.op("dve", lambda e, nh_=nh_, csl=csl: e.tensor_tensor(out=x1b.ap[:, csl], in0=banks[4 + nh_][:], in1=modg.ap[:, csl], op=ALU.mult),
                         reads=[bank_reg(4 + nh_), modg.reg()], writes=[x1b.reg(nh_ * 2048, (nh_ + 1) * 2048)])
                S.op("dve", lambda e: e.tensor_tensor(out=x1b.ap, in0=x1b.ap, in1=xb2.ap, op=ALU.add), reads=[x1b.reg(), xb2.reg()], writes=[x1b.reg()])
                dma_sp(x1_dram[tsl, :], x1b.ap, [x1b.reg()], [R("x1_dram", j, j + 1)], sem="st")
                if debug:
                    dma_sp(dbg["x1"][tsl, :], x1b.ap, [x1b.reg()], [R("dbg_x1", j, j + 1)], sem="st")
            A.top = PERSIST_TOP

            checkpoint(4)
            modsC = A.get("modsC", 3072 * 4, F32)
            widx = A.get("widx", NOV * 4, I32)
            CKEEP = A.top
            wr = A.get("wr", 8 * 256 * 4, F32, "p (k n) -> p k n", k=8)
            wgus = A.get("wgus", 8 * 512 * 4, F32, "p (k n) -> p k n", k=8)
            wds = A.get("wds", 2 * 1024 * 4, F32, "p (c n) -> p c n", c=2)
            rbias = A.get("rbias", 1024, F32)
            mcum = A.get("mcum", 1024, F32)
            x1t = [A.get(f"x1t{i}", 4096, F32) for i in range(2)]
            h2 = A.get("h2", 4096, F32)
            h2T = A.get("h2T", 4096, F32, "p (k t) -> p k t", k=8)
            sc2 = [A.get(f"sc{i}", 1024, F32) for i in range(2)]
            rs2 = A.get("rs2", 64, F32)
            junk2 = A.get("junk2", 2048, F32)
            bi_ = A.get("bi", 1024, F32)
            msk = A.get("msk", 1024, F32)
            eqt = A.get("eqt", 1024, F32)
            offm = A.get("offm", 1024, F32)
            rs = A.get("rs", 128 * 4, F32)
            off8f = A.get("off8f", 32, F32)
            sc8 = A.get("sc8", 32, F32)
            mx8 = A.get("mx8", 32, F32)
            idx8 = A.get("idx8", 32, U32)
            idx8f = A.get("idx8f", 32, F32)
            shs = A.get("shs", 1024, F32)
            shh = A.get("shh", 1024, F32)
            shT = A.get("shT", 1024, F32, "p (c t) -> p c t", c=2)
            baseb = A.get("baseb", 4096, F32)
            zrow = A.get("zrow", 4096, F32)
            listinit = A.get("listinit", (NE + NOV) * 2 * 4, I32)
            posall = A.get("posall", 16 * 256 * 4, F32, "p (j e) -> p j e", j=16)
            idxall = A.get("idxall", 16 * 8 * 4, F32, "p (j k) -> p j k", j=16)
            ovT = A.get("ovT", 32, F32)
            rankT = A.get("rankT", 32, F32)
            diagR = A.get("diagR", 512, F32)
            ovb = A.get("ovb", 1024, F32)
            diffr = A.get("diffr", 1024, F32)
            selT = A.get("selT", 2 * NOV * 4, F32, "p (c o) -> p c o", c=2)
            widxf = A.get("widxf", NOV * 4, F32)
            CWIDE = A.top
            dma_sp(modsC.ap, mods_dram[:, 3072:6144], [R("mods_dram")], [modsC.reg()])
            dma_sp(wr.ap, wr_d.rearrange("(p k) n -> p k n", k=8), [], [wr.reg()])
            dma_pool(wgus.ap[:, :, 0:256].bitcast(F32R), wgs_d.rearrange("(p k) n -> p k n", k=8), [], [wgus.reg()], sem="wgus")
            dma_pool(wgus.ap[:, :, 256:512].bitcast(F32R), wus_d.rearrange("(p k) n -> p k n", k=8), [], [wgus.reg()], sem="wgus")
            dma_pool(wds.ap.bitcast(F32R), wds_d.rearrange("(p c) n -> p c n", c=2), [], [wds.reg()], sem="wds")
            dma_pool(rbias.ap, rb_d.partition_broadcast(128), [], [rbias.reg()])
            S.op("pool", lambda e: e.memset(mcum.ap, 0.0), writes=[mcum.reg()])
            S.op("pool", lambda e: e.memset(zrow.ap, 0.0), writes=[zrow.reg()])
            dma_sp(h2_dram[2048:2049, :], zrow.ap[0:1, :], [zrow.reg()], [R("h2_dram", 16, 17)], sem="st")
            S.op("pool", lambda e: e.memset(listinit.ap, 2049), writes=[listinit.reg()])
            dma_sp(list_dram.rearrange("(p n) o -> p (n o)", p=128), listinit.ap[:, 0:(NE + NOV) * 2], [listinit.reg()], [R("list_dram", 0, 1000)], sem="st_listinit")

            NGRP = 8
            def c1_A1(j):
                tsl = slice(j * 128, (j + 1) * 128)
                xb = x1t[j % 2]
                scj = sc2[j % 2]
                dma_sp(xb.ap, x1_dram[tsl, :], [R("x1_dram", j, j + 1)], [xb.reg()], sem="x1%d" % (j % 2))
                rmsnorm_mod(xb.ap, xb.reg(), h2.ap, h2.reg(), modsC.ap[:, 1024:2048], modsC.ap[:, 0:1024], modsC.reg(), rs2, "c1")
                dma_sp(h2_dram[tsl, :], h2.ap, [h2.reg()], [R("h2_dram", j, j + 1)], sem="st")
                if debug:
                    dma_sp(dbg["h2"][tsl, :], h2.ap, [h2.reg()], [R("dbg_h2", j, j + 1)], sem="st")
                transpose8(h2.ap, h2.reg(), h2T, 0, 1)
                for k in range(8):
                    S.op("pe", lambda e, k=k: e.matmul(banks[2][:, 0:256], lhsT=h2T.ap[:, k, :], rhs=wr.ap[:, k, :], start=(k == 0), stop=(k == 7)),
                         reads=[h2T.reg(), wr.reg()], writes=[bank_reg(2)])
                for k in range(8):
                    S.op("pe", lambda e, k=k: e.matmul(banks[4][:], lhsT=h2T.ap[:, k, :].bitcast(F32R), rhs=wgus.ap[:, k, :].bitcast(F32R), start=(k == 0), stop=(k == 7)),
                         reads=[h2T.reg(), wgus.reg()], writes=[bank_reg(4)])
                S.op("act", lambda e, scj=scj: e.activation(out=scj.ap, in_=banks[2][:, 0:256], func=ACTF.Sigmoid), reads=[bank_reg(2)], writes=[scj.reg()])
                S.op("act", lambda e: e.activation(out=shs.ap, in_=banks[4][:, 0:256], func=ACTF.Silu), reads=[bank_reg(4)], writes=[shs.reg()])
            def c1_A2(j):
                tsl = slice(j * 128, (j + 1) * 128)
                xb = x1t[j % 2]
                scj = sc2[j % 2]
                S.op("dve", lambda e: e.tensor_tensor(out=shh.ap, in0=shs.ap, in1=banks[4][:, 256:512], op=ALU.mult), reads=[shs.reg(), bank_reg(4)], writes=[shh.reg()])
                shv = shh.ap.rearrange("t (p c) -> t c p", c=2)
                for c in range(2):
                    S.op("pe", lambda e, c=c: e.transpose(banks[5][:, c * 128:(c + 1) * 128], shv[:, c, :], ident),
                         reads=[shh.reg(), R("consts")], writes=[bank_reg(5, c * 512, c * 512 + 512)])
                S.op("act", lambda e: e.activation(out=shT.ap.bitcast(F32R), in_=banks[5][:, 0:256].rearrange("p (c t) -> p c t", c=2), func=ACTF.Copy),
                     reads=[bank_reg(5)], writes=[shT.reg()])
                for nh_ in range(2):
                    for c in range(2):
                        S.op("pe", lambda e, c=c, nh_=nh_: e.matmul(banks[6 + nh_][:], lhsT=shT.ap[:, c, :].bitcast(F32R),
                                                                  rhs=wds.ap[:, c, nh_ * 512:(nh_ + 1) * 512].bitcast(F32R), start=(c == 0), stop=(c == 1)),
                             reads=[shT.reg(), wds.reg()], writes=[bank_reg(6 + nh_)])
                for nh_ in range(2):
                    csl = slice(nh_ * 512, (nh_ + 1) * 512)
                    S.op("dve", lambda e, nh_=nh_, csl=csl: e.tensor_tensor(out=baseb.ap[:, csl], in0=banks[6 + nh_][:], in1=modsC.ap[:, 2048 + nh_ * 512:2048 + (nh_ + 1) * 512], op=ALU.mult),
                         reads=[bank_reg(6 + nh_), modsC.reg()], writes=[baseb.reg(nh_ * 2048, (nh_ + 1) * 2048)])
                S.op("dve", lambda e, xb=xb: e.tensor_tensor(out=baseb.ap, in0=baseb.ap, in1=xb.ap, op=ALU.add), reads=[baseb.reg(), xb.reg()], writes=[baseb.reg()])
                dma_sp(base_dram[tsl, :], baseb.ap, [baseb.reg()], [R("base_dram", j, j + 1)], sem="st")
                if debug:
                    dma_sp(dbg["base"][tsl, :], baseb.ap, [baseb.reg()], [R("dbg_base", j, j + 1)], sem="st")
            def c1_B(j):
                tsl = slice(j * 128, (j + 1) * 128)
                xb = x1t[j % 2]
                scj = sc2[j % 2]
                S.op("dve", lambda e, scj=scj: e.tensor_tensor(out=bi_.ap, in0=scj.ap, in1=rbias.ap, op=ALU.add), reads=[scj.reg(), rbias.reg()], writes=[bi_.reg()])
                bv = bi_.ap.rearrange("p (g e) -> p g e", g=NGRP)
                m1 = rs.ap[:, 0:8]
                m2 = rs.ap[:, 8:16]
                S.op("dve", lambda e: e.tensor_reduce(out=m1, in_=bv, axis=AX.X, op=ALU.max), reads=[bi_.reg()], writes=[rs.reg()])
                ev = eqt.ap.rearrange("p (g e) -> p g e", g=NGRP)
                S.op("dve", lambda e: e.tensor_tensor(out=ev, in0=bv, in1=m1.unsqueeze(2).to_broadcast([128, 8, 32]), op=ALU.is_equal),
                     reads=[bi_.reg(), rs.reg()], writes=[eqt.reg()])
                S.op("dve", lambda e: e.scalar_tensor_tensor(out=eqt.ap, in0=eqt.ap, scalar=-1e9, in1=bi_.ap, op0=ALU.mult, op1=ALU.add),
                     reads=[eqt.reg(), bi_.reg()], writes=[eqt.reg()])
                S.op("dve", lambda e: e.tensor_reduce(out=m2, in_=ev, axis=AX.X, op=ALU.max), reads=[eqt.reg()], writes=[rs.reg()])
                gs = rs.ap[:, 16:24]
                S.op("dve", lambda e: e.tensor_tensor(out=gs, in0=m1, in1=m2, op=ALU.add), reads=[rs.reg()], writes=[rs.reg()])
                cmp = rs.ap[:, 32:96].rearrange("p (a b) -> p a b", a=8)
                S.op("dve", lambda e: e.tensor_tensor(out=cmp, in0=gs.unsqueeze(1).to_broadcast([128, 8, 8]), in1=gs.unsqueeze(2).to_broadcast([128, 8, 8]), op=ALU.is_gt),
                     reads=[rs.reg()], writes=[rs.reg()])
                cntg = rs.ap[:, 24:32]
                S.op("dve", lambda e: e.tensor_reduce(out=cntg, in_=cmp, axis=AX.X, op=ALU.add), reads=[rs.reg()], writes=[rs.reg()])
                S.op("dve", lambda e: e.tensor_scalar(out=cntg, in0=cntg, scalar1=3.5, scalar2=-1e9, op0=ALU.is_gt, op1=ALU.mult),
                     reads=[rs.reg()], writes=[rs.reg()])
                mv = msk.ap.rearrange("p (g e) -> p g e", g=NGRP)
                S.op("dve", lambda e: e.tensor_tensor(out=mv, in0=bv, in1=cntg.unsqueeze(2).to_broadcast([128, 8, 32]), op=ALU.add),
                     reads=[bi_.reg(), rs.reg()], writes=[msk.reg()])
                S.op("dve", lambda e: e.max(out=mx8.ap, in_=msk.ap), reads=[msk.reg()], writes=[mx8.reg()])
                S.op("dve", lambda e: e.max_index(out=idx8.ap, in_max=mx8.ap, in_values=msk.ap), reads=[msk.reg(), mx8.reg()], writes=[idx8.reg()])
                S.op("dve", lambda e: e.tensor_copy(out=idx8f.ap, in_=idx8.ap), reads=[idx8.reg()], writes=[idx8f.reg()])
                S.op("dve", lambda e: e.tensor_scalar(out=eqt.ap, in0=msk.ap, scalar1=mx8.ap[:, 7:8], scalar2=None, op0=ALU.is_ge),
                     reads=[msk.reg(), mx8.reg()], writes=[eqt.reg()])
                S.op("pe", lambda e: e.matmul(banks[3][:, 0:256], lhsT=tri, rhs=eqt.ap, start=True, stop=False),
                     reads=[eqt.reg(), R("consts")], writes=[bank_reg(3)])
                S.op("pe", lambda e: e.matmul(banks[3][:, 0:256], lhsT=ones, rhs=mcum.ap, start=False, stop=True),
                     reads=[mcum.reg(), R("consts")], writes=[bank_reg(3)])
                S.op("dve", lambda e: e.tensor_tensor(out=mcum.ap, in0=mcum.ap, in1=eqt.ap, op=ALU.add), reads=[mcum.reg(), eqt.reg()], writes=[mcum.reg()])
                S.op("dve", lambda e, j=j: e.tensor_copy(out=posall.ap[:, j, :], in_=banks[3][:, 0:256]), reads=[bank_reg(3)], writes=[posall.reg(j * 1024, (j + 1) * 1024)])
                S.op("dve", lambda e, j=j: e.tensor_copy(out=idxall.ap[:, j, :], in_=idx8f.ap), reads=[idx8f.reg()], writes=[idxall.reg(j * 32, j * 32 + 32)])
                for k in range(8):
                    S.op("dve", lambda e, k=k, scj=scj: e.scalar_tensor_tensor(out=junk2.ap[:, 256:512], in0=iota_e, scalar=idx8f.ap[:, k:k + 1], in1=scj.ap,
                                                                     op0=ALU.is_equal, op1=ALU.mult, accum_out=sc8.ap[:, k:k + 1]),
                         reads=[idx8f.reg(), scj.reg(), R("consts")], writes=[junk2.reg(1024, 2048), sc8.reg(k * 4, k * 4 + 4)])
                ssum = rs.ap[:, 96:97]
                S.op("dve", lambda e: e.tensor_reduce(out=ssum, in_=sc8.ap, axis=AX.X, op=ALU.add), reads=[sc8.reg()], writes=[rs.reg()])
                S.op("dve", lambda e: e.reciprocal(out=rs.ap[:, 97:98], in_=ssum), reads=[rs.reg()], writes=[rs.reg()])
                S.op("dve", lambda e, j=j: e.tensor_scalar(out=gate8.ap[:, j, :], in0=sc8.ap, scalar1=rs.ap[:, 97:98], scalar2=2.5, op0=ALU.mult, op1=ALU.mult),
                     reads=[sc8.reg(), rs.reg()], writes=[gate8.reg(j * 32, j * 32 + 32)])

            c1_A1(0)
            c1_A2(0)
            for j in range(NT):
                if j + 1 < NT:
                    c1_A1(j + 1)
                c1_B(j)
                if j + 1 < NT:
                    c1_A2(j + 1)

            for c in range(2):
                S.op("pe", lambda e, c=c: e.matmul(banks[3][:, c:c + 1], lhsT=mcum.ap[:, c * 128:(c + 1) * 128], rhs=ones[:, 0:1], start=True, stop=True),
                     reads=[mcum.reg(), R("consts")], writes=[bank_reg(3)])
            S.op("dve", lambda e: e.tensor_scalar(out=ovT.ap[:, 0:2], in0=banks[3][:, 0:2], scalar1=float(CAP) + 0.5, scalar2=None, op0=ALU.is_gt),
                 reads=[bank_reg(3)], writes=[ovT.reg()])
            S.op("pe", lambda e: e.matmul(banks[2][:, 0:2], lhsT=tri, rhs=ovT.ap[:, 0:2], start=True, stop=False),
                 reads=[ovT.reg(), R("consts")], writes=[bank_reg(2)])
            S.op("pe", lambda e: e.matmul(banks[2][:, 1:2], lhsT=ones, rhs=ovT.ap[:, 0:1], start=False, stop=True),
                 reads=[ovT.reg(), R("consts")], writes=[bank_reg(2)])
            S.op("dve", lambda e: e.tensor_copy(out=rankT.ap[:, 0:2], in_=banks[2][:, 0:2]), reads=[bank_reg(2)], writes=[rankT.reg()])
            for c in range(2):
                S.op("dve", lambda e, c=c: e.tensor_scalar(out=diagR.ap, in0=ident, scalar1=rankT.ap[:, c:c + 1], scalar2=None, op0=ALU.mult),
                     reads=[rankT.reg(), R("consts")], writes=[diagR.reg()])
                S.op("pe", lambda e, c=c: e.matmul(banks[3][:, c * 128:(c + 1) * 128], lhsT=ones, rhs=diagR.ap, start=True, stop=True),
                     reads=[diagR.reg(), R("consts")], writes=[bank_reg(3)])
            S.op("dve", lambda e: e.tensor_scalar(out=ovb.ap, in0=banks[3][:, 0:256], scalar1=float(NOV) - 0.5, scalar2=1e7, op0=ALU.is_gt, op1=ALU.mult),
                 reads=[bank_reg(3)], writes=[ovb.reg()])
            S.op("dve", lambda e: e.scalar_tensor_tensor(out=ovb.ap, in0=banks[3][:, 0:256], scalar=float(CAP), in1=ovb.ap, op0=ALU.mult, op1=ALU.add),
                 reads=[bank_reg(3), ovb.reg()], writes=[ovb.reg()])
            S.op("dve", lambda e: e.tensor_scalar(out=ovb.ap, in0=ovb.ap, scalar1=float((NE - 1) * CAP), scalar2=None, op0=ALU.add),
                 reads=[ovb.reg()], writes=[ovb.reg()])
            S.op("dve", lambda e: e.scalar_tensor_tensor(out=diffr.ap, in0=iota_e, scalar=float(CAP), in1=ovb.ap, op0=ALU.mult, op1=ALU.subtract),
                 reads=[ovb.reg(), R("consts")], writes=[diffr.reg()])
            for c in range(2):
                S.op("dve", lambda e, c=c: e.tensor_scalar(out=selT.ap[:, c, :], in0=iota_e[:, 0:NOV], scalar1=rankT.ap[:, c:c + 1], scalar2=ovT.ap[:, c:c + 1],
                                                           op0=ALU.is_equal, op1=ALU.mult),
                     reads=[rankT.reg(), ovT.reg(), R("consts")], writes=[selT.reg()])
            for c in range(2):
                S.op("pe", lambda e, c=c: e.matmul(banks[2][:, 0:NOV], lhsT=(eid0 if c == 0 else eid1), rhs=selT.ap[:, c, :], start=(c == 0), stop=(c == 1)),
                     reads=[selT.reg(), R("consts")], writes=[bank_reg(2)])
            S.op("dve", lambda e: e.tensor_scalar(out=widxf.ap, in0=banks[2][:, 0:NOV], scalar1=128.0, scalar2=tokf[:, 0:1], op0=ALU.mult, op1=ALU.add),
                 reads=[bank_reg(2), R("consts")], writes=[widxf.reg()])
            S.op("dve", lambda e: e.tensor_copy(out=widx.ap, in_=widxf.ap), reads=[widxf.reg()], writes=[widx.reg()])

            for j in range(NT):
                pj = posall.ap[:, j, :]
                S.op("dve", lambda e, pj=pj: e.tensor_scalar(out=eqt.ap, in0=pj, scalar1=float(CAP) - 0.5, scalar2=None, op0=ALU.is_lt),
                     reads=[posall.reg(j * 1024, (j + 1) * 1024)], writes=[eqt.reg()])
                S.op("dve", lambda e: e.tensor_tensor(out=offm.ap, in0=eqt.ap, in1=diffr.ap, op=ALU.mult), reads=[eqt.reg(), diffr.reg()], writes=[offm.reg()])
                S.op("dve", lambda e: e.tensor_tensor(out=offm.ap, in0=offm.ap, in1=ovb.ap, op=ALU.add), reads=[offm.reg(), ovb.reg()], writes=[offm.reg()])
                S.op("dve", lambda e, pj=pj: e.tensor_tensor(out=offm.ap, in0=offm.ap, in1=pj, op=ALU.add), reads=[offm.reg(), posall.reg(j * 1024, (j + 1) * 1024)], writes=[offm.reg()])
                S.op("dve", lambda e, pj=pj: e.tensor_scalar(out=eqt.ap, in0=pj, scalar1=2.0 * CAP - 0.5, scalar2=1e7, op0=ALU.is_gt, op1=ALU.mult),
                     reads=[posall.reg(j * 1024, (j + 1) * 1024)], writes=[eqt.reg()])
                S.op("dve", lambda e: e.tensor_tensor(out=offm.ap, in0=offm.ap, in1=eqt.ap, op=ALU.add), reads=[offm.reg(), eqt.reg()], writes=[offm.reg()])
                for k in range(8):
                    S.op("dve", lambda e, k=k, j=j: e.scalar_tensor_tensor(out=junk2.ap[:, 0:256], in0=iota_e, scalar=idxall.ap[:, j, k:k + 1], in1=offm.ap,
                                                                          op0=ALU.is_equal, op1=ALU.mult, accum_out=off8f.ap[:, k:k + 1]),
                         reads=[idxall.reg(j * 32, j * 32 + 32), offm.reg(), R("consts")], writes=[junk2.reg(0, 1024), off8f.reg(k * 4, k * 4 + 4)])
                S.op("dve", lambda e, j=j: e.tensor_copy(out=off8.ap[:, j, :], in_=off8f.ap), reads=[off8f.reg()], writes=[off8.reg(j * 32, j * 32 + 32)])
                for k in range(8):
                    S.op("pool", lambda e, j=j, k=k: e.indirect_dma_start(out=list_dram[:, :], out_offset=bass.IndirectOffsetOnAxis(ap=off8.ap[:, j, k:k + 1], axis=0),
                                                                         in_=tok_i.ap[:, j, :], in_offset=None, bounds_check=breg(e, (NE + NOV) * CAP - 1), oob_is_err=False),
                         reads=[off8.reg(j * 32, j * 32 + 32), tok_i.reg()], writes=[R("list_dram", j * 8 + k + 1, j * 8 + k + 2)], dsem="lsc")
            if debug:
                dma_sp(dbg["off8"], off8.ap.rearrange("p j k -> p (j k)"), [off8.reg()], [R("dbg_off8")], sem="st")
                dma_sp(dbg["gate8"], gate8.ap.rearrange("p j k -> p (j k)"), [gate8.reg()], [R("dbg_gate8")], sem="st")

            checkpoint(5)
            A.top = CKEEP
            NW = 3
            wg_ = [A.get(f"wg{i}", 8 * 256 * 4, F32, "p (k n) -> p k n", k=8) for i in range(NW)]
            wu_ = [A.get(f"wu{i}", 8 * 256 * 4, F32, "p (k n) -> p k n", k=8) for i in range(NW)]
            wdn = [A.get(f"wdn{i}", 2 * 1024 * 4, F32, "p (c n) -> p c n", c=2) for i in range(NW)]
            xe = [A.get(f"xe{i}", 4096, F32) for i in range(NW)]
            lidx = [A.get(f"lidx{i}", 8, I32) for i in range(NW)]
            xeT = [A.get(f"xeT{i}", 4096, F32, "p (k t) -> p k t", k=8) for i in range(2)]
            es = [A.get(f"es{i}", 1024, F32) for i in range(2)]
            eh = [A.get(f"eh{i}", 1024, F32) for i in range(2)]
            ehT = [A.get(f"ehT{i}", 1024, F32, "p (c t) -> p c t", c=2) for i in range(2)]
            ysb = [A.get(f"ysb{i}", 4096, F32) for i in range(2)]

            def load_expert(e_):
                s_ = e_ % NW
                dma_pool(wg_[s_].ap.bitcast(F32R), wge_d[e_].rearrange("(p k) n -> p k n", k=8), [], [wg_[s_].reg()], sem="wg%d" % s_)
                dma_pool(wu_[s_].ap.bitcast(F32R), wue_d[e_].rearrange("(p k) n -> p k n", k=8), [], [wu_[s_].reg()], sem="wu%d" % s_)
                dma_pool(wdn[s_].ap.bitcast(F32R), wde_d[e_].rearrange("(p c) n -> p c n", c=2), [], [wdn[s_].reg()], sem="wd%d" % s_)
                dma_sp(lidx[s_].ap[:, 0:2], list_dram[e_ * CAP:(e_ + 1) * CAP, :], [R("list_dram", 0, 1000)], [lidx[s_].reg()], sem="li%d" % s_)
                S.op("pool", lambda e, s_=s_: e.indirect_dma_start(out=xe[s_].ap, out_offset=None, in_=h2_dram[:, :],
                                                                   in_offset=bass.IndirectOffsetOnAxis(ap=lidx[s_].ap[:, 0:1], axis=0),
                                                                   bounds_check=breg(e, 2048), oob_is_err=False),
                     reads=[lidx[s_].reg(), R("h2_dram", 0, 17)], writes=[xe[s_].reg()], dsem="xg%d" % s_)

            for i_ in range(NW):
                S.op("pool", lambda e, i_=i_: e.memset(xe[i_].ap, 0.0), writes=[xe[i_].reg()])
            for e_ in range(min(NW - 1, NE)):
                load_expert(e_)
            for e_ in range(NE):
                if e_ + NW - 1 < NE:
                    load_expert(e_ + NW - 1)
                s_ = e_ % NW
                d_ = e_ % 2
                b0, b1 = (0, 1) if d_ == 0 else (2, 3)
                transpose8(xe[s_].ap, xe[s_].reg(), xeT[d_], b0, b1)
                for (wbuf, c0) in ((wg_[s_], 0), (wu_[s_], 256)):
                    for k in range(8):
                        S.op("pe", lambda e, k=k, wbuf=wbuf, c0=c0, d_=d_: e.matmul(banks[4][:, c0:c0 + 256], lhsT=xeT[d_].ap[:, k, :].bitcast(F32R),
                                                                                 rhs=wbuf.ap[:, k, :].bitcast(F32R), start=(k == 0), stop=(k == 7)),
                             reads=[xeT[d_].reg(), wbuf.reg()], writes=[bank_reg(4)])
                S.op("act", lambda e, d_=d_: e.activation(out=es[d_].ap, in_=banks[4][:, 0:256], func=ACTF.Silu), reads=[bank_reg(4)], writes=[es[d_].reg()])
                S.op("dve", lambda e, d_=d_: e.tensor_tensor(out=eh[d_].ap, in0=es[d_].ap, in1=banks[4][:, 256:512], op=ALU.mult),
                     reads=[es[d_].reg(), bank_reg(4)], writes=[eh[d_].reg()])
                ehv = eh[d_].ap.rearrange("t (p c) -> t c p", c=2)
                for c in range(2):
                    S.op("pe", lambda e, c=c, ehv=ehv, d_=d_: e.transpose(banks[5][:, c * 128:(c + 1) * 128], ehv[:, c, :], ident),
                         reads=[eh[d_].reg(), R("consts")], writes=[bank_reg(5, c * 512, c * 512 + 512)])
                S.op("act", lambda e, d_=d_: e.activation(out=ehT[d_].ap.bitcast(F32R), in_=banks[5][:, 0:256].rearrange("p (c t) -> p c t", c=2), func=ACTF.Copy),
                     reads=[bank_reg(5)], writes=[ehT[d_].reg()])
                for nh_ in range(2):
                    for c in range(2):
                        S.op("pe", lambda e, c=c, nh_=nh_, s_=s_, d_=d_: e.matmul(banks[6 + nh_][:], lhsT=ehT[d_].ap[:, c, :].bitcast(F32R),
                                                                               rhs=wdn[s_].ap[:, c, nh_ * 512:(nh_ + 1) * 512].bitcast(F32R), start=(c == 0), stop=(c == 1)),
                             reads=[ehT[d_].reg(), wdn[s_].reg()], writes=[bank_reg(6 + nh_)])
                S.op("dve", lambda e, d_=d_: e.tensor_copy(out=ysb[d_].ap[:, 0:512], in_=banks[6][:]), reads=[bank_reg(6)], writes=[ysb[d_].reg(0, 2048)])
                S.op("act", lambda e, d_=d_: e.activation(out=ysb[d_].ap[:, 512:1024], in_=banks[7][:], func=ACTF.Copy), reads=[bank_reg(7)], writes=[ysb[d_].reg(2048, 4096)])
                dma_sp(y_dram[e_ * CAP:(e_ + 1) * CAP, :], ysb[d_].ap, [ysb[d_].reg()], [R("y_dram", e_, e_ + 1)], sem="yst%d" % d_)

            A.top = CKEEP
            wgo = [A.get(f"wgo{i}", 8 * 256 * 4, F32, "p (k n) -> p k n", k=8) for i in range(2)]
            wuo = [A.get(f"wuo{i}", 8 * 256 * 4, F32, "p (k n) -> p k n", k=8) for i in range(2)]
            wdo = [A.get(f"wdo{i}", 2 * 1024 * 4, F32, "p (c n) -> p c n", c=2) for i in range(2)]
            wge_rows = wge_d.rearrange("e (p k) n -> (e p) (k n)", k=8)
            wue_rows = wue_d.rearrange("e (p k) n -> (e p) (k n)", k=8)
            wde_rows = wde_d.rearrange("e (p c) n -> (e p) (c n)", c=2)
            for ob in range(NOV):
                d_ = ob % 2
                s_ = ob % NW
                for (dst, src, nm) in ((wgo[d_], wge_rows, "og"), (wuo[d_], wue_rows, "ou"), (wdo[d_], wde_rows, "od")):
                    S.op("pool", lambda e, dst=dst, src=src, ob=ob: e.indirect_dma_start(
                        out=dst.ap.rearrange("p a b -> p (a b)"), out_offset=None, in_=src[:, :],
                        in_offset=bass.IndirectOffsetOnAxis(ap=widx.ap[:, ob:ob + 1], axis=0), bounds_check=breg(e, NE * 128 - 1), oob_is_err=False),
                        reads=[widx.reg()], writes=[dst.reg()], dsem="%s%d" % (nm, d_))
                dma_sp(lidx[s_].ap[:, 0:2], list_dram[(NE + ob) * CAP:(NE + ob + 1) * CAP, :], [R("list_dram", 0, 1000)], [lidx[s_].reg()], sem="li%d" % s_)
                S.op("pool", lambda e, s_=s_: e.indirect_dma_start(out=xe[s_].ap, out_offset=None, in_=h2_dram[:, :],
                                                                   in_offset=bass.IndirectOffsetOnAxis(ap=lidx[s_].ap[:, 0:1], axis=0),
                                                                   bounds_check=breg(e, 2048), oob_is_err=False),
                     reads=[lidx[s_].reg(), R("h2_dram", 0, 17)], writes=[xe[s_].reg()], dsem="xg%d" % s_)
                b0, b1 = (0, 1) if d_ == 0 else (2, 3)
                transpose8(xe[s_].ap, xe[s_].reg(), xeT[d_], b0, b1)
                for (wbuf, c0) in ((wgo[d_], 0), (wuo[d_], 256)):
                    for k in range(8):
                        S.op("pe", lambda e, k=k, wbuf=wbuf, c0=c0, d_=d_: e.matmul(banks[4][:, c0:c0 + 256], lhsT=xeT[d_].ap[:, k, :], rhs=wbuf.ap[:, k, :],
                                                                                 start=(k == 0), stop=(k == 7)),
                             reads=[xeT[d_].reg(), wbuf.reg()], writes=[bank_reg(4)])
                S.op("act", lambda e, d_=d_: e.activation(out=es[d_].ap, in_=banks[4][:, 0:256], func=ACTF.Silu), reads=[bank_reg(4)], writes=[es[d_].reg()])
                S.op("dve", lambda e, d_=d_: e.tensor_tensor(out=eh[d_].ap, in0=es[d_].ap, in1=banks[4][:, 256:512], op=ALU.mult),
                     reads=[es[d_].reg(), bank_reg(4)], writes=[eh[d_].reg()])
                ehv = eh[d_].ap.rearrange("t (p c) -> t c p", c=2)
                for c in range(2):
                    S.op("pe", lambda e, c=c, ehv=ehv, d_=d_: e.transpose(banks[5][:, c * 128:(c + 1) * 128], ehv[:, c, :], ident),
                         reads=[eh[d_].reg(), R("consts")], writes=[bank_reg(5, c * 512, c * 512 + 512)])
                S.op("act", lambda e, d_=d_: e.activation(out=ehT[d_].ap.bitcast(F32R), in_=banks[5][:, 0:256].rearrange("p (c t) -> p c t", c=2), func=ACTF.Copy),
                     reads=[bank_reg(5)], writes=[ehT[d_].reg()])
                for nh_ in range(2):
                    for c in range(2):
                        S.op("pe", lambda e, c=c, nh_=nh_, d_=d_: e.matmul(banks[6 + nh_][:], lhsT=ehT[d_].ap[:, c, :], rhs=wdo[d_].ap[:, c, nh_ * 512:(nh_ + 1) * 512],
                                                                        start=(c == 0), stop=(c == 1)),
                             reads=[ehT[d_].reg(), wdo[d_].reg()], writes=[bank_reg(6 + nh_)])
                S.op("dve", lambda e, d_=d_: e.tensor_copy(out=ysb[d_].ap[:, 0:512], in_=banks[6][:]), reads=[bank_reg(6)], writes=[ysb[d_].reg(0, 2048)])
                S.op("act", lambda e, d_=d_: e.activation(out=ysb[d_].ap[:, 512:1024], in_=banks[7][:], func=ACTF.Copy), reads=[bank_reg(7)], writes=[ysb[d_].reg(2048, 4096)])
                dma_sp(y_dram[(NE + ob) * CAP:(NE + ob + 1) * CAP, :], ysb[d_].ap, [ysb[d_].reg()], [R("y_dram", NE + ob, NE + ob + 1)], sem="yst%d" % d_)

            checkpoint(6)
            A.top = CKEEP
            yg = [A.get(f"yg{i}", 4096, F32) for i in range(4)]
            acc = [A.get(f"acc{i}", 4096, F32) for i in range(2)]
            bs = [A.get(f"bs{i}", 4096, F32) for i in range(2)]
            gi_ = 0
            last_tok = None
            for g in yg:
                S.op("pool", lambda e, g=g: e.memset(g.ap, 0.0), writes=[g.reg()])
            offv = A.get("offv", 128 * 4, F32, "p (j k) -> p j k", j=16)
            S.op("dve", lambda e: e.tensor_copy(out=offv.ap, in_=off8.ap), reads=[off8.reg()], writes=[offv.reg()])
            S.op("dve", lambda e: e.tensor_scalar(out=offv.ap, in0=offv.ap, scalar1=float((NE + NOV) * CAP) - 0.5, scalar2=None, op0=ALU.is_lt),
                 reads=[offv.reg()], writes=[offv.reg()])
            S.op("dve", lambda e: e.tensor_tensor(out=gate8.ap, in0=gate8.ap, in1=offv.ap, op=ALU.mult), reads=[gate8.reg(), offv.reg()], writes=[gate8.reg()])
            for j in range(NT):
                tsl = slice(j * 128, (j + 1) * 128)
                ac = acc[j % 2]
                bsb = bs[j % 2]
                dma_sp(bsb.ap, base_dram[tsl, :], [R("base_dram", j, j + 1)], [bsb.reg()], sem="bs%d" % (j % 2))
                for k in range(8):
                    g = yg[gi_ % 4]
                    gs_ = gi_ % 4
                    gi_ += 1
                    S.op("pool", lambda e, g=g, j=j, k=k: e.indirect_dma_start(out=g.ap, out_offset=None, in_=y_dram[:, :],
                                                                               in_offset=bass.IndirectOffsetOnAxis(ap=off8.ap[:, j, k:k + 1], axis=0),
                                                                               bounds_check=breg(e, (NE + NOV) * CAP - 1), oob_is_err=False),
                         reads=[off8.reg(j * 32, j * 32 + 32), R("y_dram", 0, 1000)], writes=[g.reg()], dsem="yg%d" % gs_)
                    if k == 0:
                        S.op("dve", lambda e, g=g, ac=ac, j=j, k=k: e.tensor_scalar(out=ac.ap, in0=g.ap, scalar1=gate8.ap[:, j, k:k + 1], scalar2=None, op0=ALU.mult),
                             reads=[g.reg(), gate8.reg()], writes=[ac.reg()])
                    else:
                        S.op("dve", lambda e, g=g, ac=ac, j=j, k=k: e.scalar_tensor_tensor(out=ac.ap, in0=g.ap, scalar=gate8.ap[:, j, k:k + 1], in1=ac.ap, op0=ALU.mult, op1=ALU.add),
                             reads=[g.reg(), gate8.reg(), ac.reg()], writes=[ac.reg()])
                S.op("dve", lambda e, ac=ac: e.tensor_tensor(out=ac.ap, in0=ac.ap, in1=modsC.ap[:, 2048:3072], op=ALU.mult), reads=[ac.reg(), modsC.reg()], writes=[ac.reg()])
                S.op("dve", lambda e, ac=ac, bsb=bsb: e.tensor_tensor(out=ac.ap, in0=ac.ap, in1=bsb.ap, op=ALU.add), reads=[ac.reg(), bsb.reg()], writes=[ac.reg()])
                last_tok = dma_sp(out_d[tsl, :], ac.ap, [ac.reg()], [R("out_d", j, j + 1)], sem="ost%d" % (j % 2))
        except _Stop:
            pass
        S.wait_all("sp", [(k, v) for k, v in S.cnt.items() if k not in COMPUTE])
        S.emit()
    return nc


_NC_CACHE = {}


def make_in_maps(inputs, ncores=8):
    f = lambda a: np.ascontiguousarray(np.asarray(a))
    x = f(inputs["x"]); c = f(inputs["c"]); pos = f(inputs["positions"])
    shared = dict(
        w_ada=f(inputs["w_ada"][0]), b_ada=f(inputs["b_ada"][0]).reshape(1, 6144),
        g_mix=f(inputs["g_norm_mix"][0]).reshape(1, 1024), g_ffn=f(inputs["g_norm_ffn"][0]).reshape(1, 1024),
        w_in=f(inputs["w_in"][0]),
        gqk=np.concatenate([f(inputs["g_q_a"][0]), f(inputs["g_k_a"][0]), f(inputs["g_q_b"][0]), f(inputs["g_k_b"][0])]).reshape(1, 256),
        sinks=f(inputs["sinks_a"][0]).reshape(1, 8),
        g_out=np.concatenate([f(inputs["g_out_a"][0]), f(inputs["g_out_b"][0])]).reshape(1, 1024),
        w_out=f(inputs["w_out"][0]), w_router=f(inputs["w_router"][0]), rbias=f(inputs["router_bias"][0]).reshape(1, 256),
        w_gate_e=f(inputs["w_gate_e"][0]), w_up_e=f(inputs["w_up_e"][0]), w_down_e=f(inputs["w_down_e"][0]),
        w_gate_s=f(inputs["w_gate_s"][0]), w_up_s=f(inputs["w_up_s"][0]), w_down_s=f(inputs["w_down_s"][0]),
        consts=make_consts(),
    )
    maps = []
    for b in range(ncores):
        m = dict(shared)
        m["x"] = x[b]
        m["cT"] = np.ascontiguousarray(c[b].reshape(128, 8))
        m["posT"] = np.ascontiguousarray(pos[b].reshape(16, 128).T.astype(np.int32))
        maps.append(m)
    return maps


def kernel(**inputs):
    if "nc" not in _NC_CACHE:
        _NC_CACHE["nc"] = build_nc()
    nc = _NC_CACHE["nc"]
    maps = make_in_maps(inputs, 8)
    res = run_bass_kernel_spmd(nc, maps, core_ids=list(range(8)))
    out = np.stack([np.asarray(r["out"]) for r in res.results], axis=0)
    return out.astype(np.float32)
```
